# Optimizing a Trainium2 kernel written in Bass

```python
import math
import jax, jax.numpy as jnp
from jax import lax
import numpy as np

D_MODEL = 1024
BATCH = 4
SEQ = 8192
DEPTH = 4

N_MEM = 256
D_MIX = D_MODEL
N_MIXERS = 4
W_GROUP = D_MIX // N_MIXERS
EPS = 1e-6

S5_GROUP_CH = 16
S5_GROUPS = W_GROUP // S5_GROUP_CH
S5_STATE = 64
S5_DT_MIN = 1e-3
S5_DT_MAX = 1e-1

GLA_HEADS = 4
GLA_DV = W_GROUP // GLA_HEADS
GLA_DK = GLA_DV // 2
GLA_RANK = 16
GLA_TAU = 16.0
GLA_CHUNK = 16

RW_HEADS = 4
RW_N = W_GROUP // RW_HEADS
RW_W_RANK = 32
RW_A_RANK = 32
RW_G_RANK = 64
RW_LN_EPS = 64e-5

CONV_WIDTH = 31
CONV_LN_EPS = 1e-5

X_HEADS = 4
X_HEAD_DIM = D_MODEL // X_HEADS

N_GROUPS = 4
EXP_PER_GROUP = 8
N_EXPERTS = N_GROUPS * EXP_PER_GROUP
TOP_K = 2
D_EXPERT = 512
MOE_BLOCK = 256

S5_COLS = W_GROUP
GLA_COLS = 2 * GLA_HEADS * GLA_DK + 2 * W_GROUP + GLA_RANK
RW_COLS = 3 * W_GROUP + RW_W_RANK + RW_A_RANK + RW_G_RANK
CONV_COLS = 2 * W_GROUP
IN_COLS = S5_COLS + GLA_COLS + RW_COLS + CONV_COLS
COL_SPLITS = (S5_COLS, S5_COLS + GLA_COLS, S5_COLS + GLA_COLS + RW_COLS)

kernel_name = 'hybrid_parallel_headgroup_hmoe_block'


def rms_norm(x, g):
    xf = x.astype(jnp.float32)
    y = xf * lax.rsqrt(jnp.mean(xf * xf, axis=-1, keepdims=True) + EPS)
    return (y * g.astype(jnp.float32)).astype(x.dtype)


def standardize(x, eps):
    mu = jnp.mean(x, axis=-1, keepdims=True)
    xc = x - mu
    return xc * lax.rsqrt(jnp.mean(xc * xc, axis=-1, keepdims=True) + eps)


def layer_norm(x, g, b, eps):
    return standardize(x, eps) * g + b


def s5_mixer(u, lam_re, lam_im, b_re, b_im, c_re, c_im, d_skip, log_dt, glu_w, glu_b):
    f32 = jnp.float32
    bsz, seq, _ = u.shape
    u = u.astype(f32)
    lam = lax.complex(lam_re.astype(f32), lam_im.astype(f32))
    dt = jnp.exp(log_dt.astype(f32))[:, None]
    lam_bar = jnp.exp(lam * dt)
    b_bar = ((lam_bar - 1.0) / lam)[:, :, None] * lax.complex(b_re.astype(f32), b_im.astype(f32))
    ug = u.reshape(bsz, seq, S5_GROUPS, S5_GROUP_CH).astype(jnp.complex64)
    bu = jnp.einsum('gnp,blgp->blgn', b_bar, ug)
    decay = jnp.broadcast_to(lam_bar, bu.shape)

    def combine(left, right):
        a_l, b_l = left
        a_r, b_r = right
        return a_l * a_r, a_r * b_l + b_r

    _, states = lax.associative_scan(combine, (decay, bu), axis=1)
    c = lax.complex(c_re.astype(f32), c_im.astype(f32))
    y = jnp.einsum('gpn,blgn->blgp', c, states).real.reshape(bsz, seq, W_GROUP)
    y = jax.nn.gelu(y + d_skip * u)
    return y * jax.nn.sigmoid(y @ glu_w + glu_b)


def gla_mixer(p, w_up, b_up, norm_g):
    f32 = jnp.float32
    bsz, seq, _ = p.shape
    H, dk, dv, C = GLA_HEADS, GLA_DK, GLA_DV, GLA_CHUNK
    nc = seq // C
    q, k, v, g, z = jnp.split(p.astype(f32), [H * dk, 2 * H * dk, 2 * H * dk + W_GROUP, 2 * H * dk + 2 * W_GROUP], axis=-1)
    log_alpha = jax.nn.log_sigmoid(z @ w_up + b_up) / GLA_TAU

    def chunks(t, d):
        return t.reshape(bsz, nc, C, H, d).transpose(0, 3, 1, 2, 4)

    q = chunks(q * dk ** -0.5, dk)
    k = chunks(k, dk)
    v = chunks(v, dv)
    bcum = jnp.cumsum(chunks(log_alpha, dk), axis=3)
    causal = jnp.tril(jnp.ones((C, C), dtype=bool))[:, :, None]
    rel = jnp.where(causal, bcum[:, :, :, :, None, :] - bcum[:, :, :, None, :, :], -jnp.inf)
    scores = jnp.sum(q[:, :, :, :, None, :] * k[:, :, :, None, :, :] * jnp.exp(rel), axis=-1)
    o_intra = jnp.einsum('bhnij,bhnjv->bhniv', scores, v)
    b_last = bcum[:, :, :, -1:, :]
    chunk_upd = jnp.einsum('bhncd,bhncv->bhndv', k * jnp.exp(b_last - bcum), v)
    chunk_decay = jnp.exp(b_last[:, :, :, 0, :])

    def carry_state(state, inp):
        dec, upd = inp
        return dec[..., None] * state + upd, state

    s0 = jnp.zeros((bsz, H, dk, dv), f32)
    _, s_prev = lax.scan(carry_state, s0, (chunk_decay.transpose(2, 0, 1, 3), chunk_upd.transpose(2, 0, 1, 3, 4)))
    o_inter = jnp.einsum('bhncd,bhndv->bhncv', q * jnp.exp(bcum), s_prev.transpose(1, 2, 0, 3, 4))
    o = (o_intra + o_inter).transpose(0, 2, 3, 1, 4).reshape(bsz, seq, H, dv)
    o = o * lax.rsqrt(jnp.mean(o * o, axis=-1, keepdims=True) + EPS)
    return o.reshape(bsz, seq, W_GROUP) * norm_g * jax.nn.silu(g)


def rwkv7_mixer(p, mu, w0, w2, a0, a2, g2, k_k, k_a, r_k, ln_g, ln_b):
    f32 = jnp.float32
    bsz, seq, _ = p.shape
    p = p.astype(f32)
    p_prev = jnp.pad(p, ((0, 0), (1, 0), (0, 0)))[:, :-1]
    p = p + (p_prev - p) * mu
    r, k, v, zw, za, zg = jnp.split(p, [W_GROUP, 2 * W_GROUP, 3 * W_GROUP, 3 * W_GROUP + RW_W_RANK, 3 * W_GROUP + RW_W_RANK + RW_A_RANK], axis=-1)
    w = -jax.nn.softplus(-(w0 + jnp.tanh(zw) @ w2)) - 0.5
    decay = jnp.exp(-jnp.exp(w))
    a = jax.nn.sigmoid(a0 + za @ a2)
    g = jax.nn.sigmoid(zg) @ g2

    def heads(t):
        return t.reshape(bsz, seq, RW_HEADS, RW_N)

    kk = heads(k * k_k)
    kk = kk / jnp.maximum(jnp.sqrt(jnp.sum(kk * kk, axis=-1, keepdims=True)), 1e-12)
    k = k * (1.0 + (a - 1.0) * k_a)
    r_h, k_h, v_h = heads(r), heads(k), heads(v)

    def time_step(state, inp):
        r_t, w_t, k_t, v_t, a_t, b_t = inp
        sa = jnp.einsum('bhvk,bhk->bhv', state, a_t)
        state = state * w_t[:, :, None, :] + sa[..., None] * b_t[:, :, None, :] + v_t[..., None] * k_t[:, :, None, :]
        return state, jnp.einsum('bhvk,bhk->bhv', state, r_t)

    xs = tuple(t.transpose(1, 0, 2, 3) for t in (r_h, heads(decay), k_h, v_h, -kk, kk * heads(a)))
    s0 = jnp.zeros((bsz, RW_HEADS, RW_N, RW_N), f32)
    _, y = lax.scan(time_step, s0, xs)
    y = standardize(y.transpose(1, 0, 2, 3), RW_LN_EPS)
    y = y.reshape(bsz, seq, W_GROUP) * ln_g + ln_b
    bonus = jnp.sum(r_h * k_h * r_k, axis=-1, keepdims=True) * v_h
    return (y + bonus.reshape(bsz, seq, W_GROUP)) * g


def conv_mixer(p, dw_w, dw_b, ln_g, ln_b):
    pf = p.astype(jnp.float32)
    u = pf[..., :W_GROUP] * jax.nn.sigmoid(pf[..., W_GROUP:])
    y = lax.conv_general_dilated(u, dw_w.astype(jnp.float32)[:, None, :], window_strides=(1,),
                                 padding=[(CONV_WIDTH - 1, 0)], dimension_numbers=('NWC', 'WIO', 'NWC'),
                                 feature_group_count=W_GROUP)
    y = layer_norm(y + dw_b, ln_g, ln_b, CONV_LN_EPS)
    return jax.nn.silu(y)


def memory_xattn(hn, mn, wq, wk, wv, wo):
    bsz, seq, d = hn.shape
    n_mem = mn.shape[1]
    q = (hn @ wq).reshape(bsz, seq, X_HEADS, X_HEAD_DIM)
    k = (mn @ wk).reshape(bsz, n_mem, X_HEADS, X_HEAD_DIM)
    v = (mn @ wv).reshape(bsz, n_mem, X_HEADS, X_HEAD_DIM)
    s = jnp.einsum('blhd,bmhd->bhlm', q, k).astype(jnp.float32) * X_HEAD_DIM ** -0.5
    pr = jax.nn.softmax(s, axis=-1).astype(v.dtype)
    o = jnp.einsum('bhlm,bmhd->blhd', pr, v).reshape(bsz, seq, d)
    return (o @ wo).astype(hn.dtype)


def hier_moe(xn, group_w, group_b, expert_w, expert_b, w_gate, w_up, w_down):
    f32 = jnp.float32
    bsz, seq, d = xn.shape
    T = bsz * seq
    xt = xn.reshape(T, d)
    g_logits = (xt @ group_w).astype(f32) + group_b
    g_sel = jnp.argmax(g_logits, axis=-1)
    g_w = jnp.take_along_axis(jax.nn.softmax(g_logits, axis=-1), g_sel[:, None], axis=-1)
    e_logits = ((xt @ expert_w).astype(f32) + expert_b).reshape(T, N_GROUPS, EXP_PER_GROUP)
    e_in_group = jnp.take_along_axis(e_logits, g_sel[:, None, None], axis=1)[:, 0]
    top_v, top_i = lax.top_k(e_in_group, TOP_K)
    gate = (jax.nn.softmax(top_v, axis=-1) * g_w).reshape(-1)
    expert_id = (g_sel[:, None] * EXP_PER_GROUP + top_i).reshape(-1).astype(jnp.int32)
    token_id = jnp.repeat(jnp.arange(T, dtype=jnp.int32), TOP_K)
    order = jnp.argsort(expert_id)
    e_sorted = expert_id[order]
    counts = jnp.bincount(expert_id, length=N_EXPERTS).astype(jnp.int32)
    starts = jnp.cumsum(counts) - counts
    padded = (counts + MOE_BLOCK - 1) // MOE_BLOCK * MOE_BLOCK
    pad_ends = jnp.cumsum(padded)
    pad_starts = pad_ends - padded
    dest = pad_starts[e_sorted] + (jnp.arange(T * TOP_K, dtype=jnp.int32) - starts[e_sorted])
    n_slots = ((T * TOP_K + MOE_BLOCK - 1) // MOE_BLOCK + N_EXPERTS) * MOE_BLOCK
    n_blocks = n_slots // MOE_BLOCK
    slot_tok = jnp.full((n_slots,), T, jnp.int32).at[dest].set(token_id[order])
    slot_gate = jnp.zeros((n_slots,), f32).at[dest].set(gate[order])
    block_start = jnp.arange(n_blocks, dtype=jnp.int32) * MOE_BLOCK
    block_exp = jnp.minimum(jnp.searchsorted(pad_ends, block_start, side='right'), N_EXPERTS - 1)
    x_pad = jnp.concatenate([xt, jnp.zeros((1, d), xt.dtype)], axis=0)
    xb = x_pad[slot_tok].reshape(n_blocks, MOE_BLOCK, d)

    def expert_block(args):
        xblk, e = args
        hid = jax.nn.silu(xblk @ w_gate[e]) * (xblk @ w_up[e])
        return hid @ w_down[e]

    yb = lax.map(expert_block, (xb, block_exp)).reshape(n_slots, d).astype(f32)
    y = jnp.zeros((T + 1, d), f32).at[slot_tok].add(yb * slot_gate[:, None])
    return y[:T].reshape(bsz, seq, d).astype(xn.dtype)


def setup_inputs(seed: int = 0) -> dict:
    f32 = jnp.float32
    key = jax.random.key(seed)
    keys = jax.random.split(key, 64)
    count = [0]

    def nk():
        count[0] += 1
        return keys[count[0] - 1]

    def nrm(shape, scale):
        return jax.random.normal(nk(), shape, f32) * scale

    def unif(shape, lo, hi):
        return jax.random.uniform(nk(), shape, f32, lo, hi)

    def gain(shape):
        return 1.0 + nrm(shape, 0.02)

    L = DEPTH
    D = D_MODEL
    G, N, P = S5_GROUPS, S5_STATE, S5_GROUP_CH
    res_scale = (2.0 * DEPTH) ** -0.5
    return {
        'x': nrm((BATCH, SEQ, D), 1.0),
        'mem': nrm((BATCH, N_MEM, D), 1.0),
        'norm_mix_g': gain((L, D)),
        'w_in': nrm((L, D, IN_COLS), D ** -0.5),
        'w_out': nrm((L, D_MIX, D), D_MIX ** -0.5 * res_scale),
        'mix_beta': gain((L, D_MIX)),
        's5_lam_re': -0.5 + nrm((L, G, N), 0.01),
        's5_lam_im': jnp.pi * jnp.arange(N, dtype=f32) + nrm((L, G, N), 0.01),
        's5_b_re': nrm((L, G, N, P), (2.0 * P) ** -0.5),
        's5_b_im': nrm((L, G, N, P), (2.0 * P) ** -0.5),
        's5_c_re': nrm((L, G, P, N), N ** -0.5),
        's5_c_im': nrm((L, G, P, N), N ** -0.5),
        's5_d': nrm((L, W_GROUP), 1.0),
        's5_log_dt': unif((L, G), math.log(S5_DT_MIN), math.log(S5_DT_MAX)),
        's5_glu_w': nrm((L, W_GROUP, W_GROUP), W_GROUP ** -0.5),
        's5_glu_b': nrm((L, W_GROUP), 0.01),
        'gla_w_up': nrm((L, GLA_RANK, GLA_HEADS * GLA_DK), GLA_RANK ** -0.5),
        'gla_b_up': unif((L, GLA_HEADS * GLA_DK), 0.0, 4.0),
        'gla_norm_g': gain((L, W_GROUP)),
        'rw_mu': unif((L, RW_COLS), 0.0, 1.0),
        'rw_w0': unif((L, W_GROUP), -6.0, -1.0),
        'rw_w2': nrm((L, RW_W_RANK, W_GROUP), 0.1),
        'rw_a0': nrm((L, W_GROUP), 0.1),
        'rw_a2': nrm((L, RW_A_RANK, W_GROUP), 0.1),
        'rw_g2': nrm((L, RW_G_RANK, W_GROUP), RW_G_RANK ** -0.5),
        'rw_k_k': 0.85 + nrm((L, W_GROUP), 0.02),
        'rw_k_a': gain((L, W_GROUP)),
        'rw_r_k': nrm((L, RW_HEADS, RW_N), 0.1),
        'rw_ln_g': gain((L, W_GROUP)),
        'rw_ln_b': nrm((L, W_GROUP), 0.01),
        'conv_w': nrm((L, CONV_WIDTH, W_GROUP), CONV_WIDTH ** -0.5),
        'conv_b': nrm((L, W_GROUP), 0.01),
        'conv_ln_g': gain((L, W_GROUP)),
        'conv_ln_b': nrm((L, W_GROUP), 0.01),
        'norm_xattn_g': gain((L, D)),
        'norm_mem_g': gain((L, D)),
        'xa_wq': nrm((L, D, D), D ** -0.5),
        'xa_wk': nrm((L, D, D), D ** -0.5),
        'xa_wv': nrm((L, D, D), D ** -0.5),
        'xa_wo': nrm((L, D, D), D ** -0.5 * res_scale),
        'norm_ffn_g': gain((L, D)),
        'moe_group_w': nrm((L, D, N_GROUPS), D ** -0.5),
        'moe_group_b': nrm((L, N_GROUPS), 0.01),
        'moe_expert_w': nrm((L, D, N_EXPERTS), D ** -0.5),
        'moe_expert_b': nrm((L, N_EXPERTS), 0.01),
        'moe_w_gate': nrm((L, N_EXPERTS, D, D_EXPERT), D ** -0.5),
        'moe_w_up': nrm((L, N_EXPERTS, D, D_EXPERT), D ** -0.5),
        'moe_w_down': nrm((L, N_EXPERTS, D_EXPERT, D), D_EXPERT ** -0.5 * res_scale),
        'norm_final_g': gain((D,)),
    }


def reference(x, mem, norm_mix_g, w_in, w_out, mix_beta, s5_lam_re, s5_lam_im, s5_b_re, s5_b_im, s5_c_re, s5_c_im, s5_d, s5_log_dt, s5_glu_w, s5_glu_b, gla_w_up, gla_b_up, gla_norm_g, rw_mu, rw_w0, rw_w2, rw_a0, rw_a2, rw_g2, rw_k_k, rw_k_a, rw_r_k, rw_ln_g, rw_ln_b, conv_w, conv_b, conv_ln_g, conv_ln_b, norm_xattn_g, norm_mem_g, xa_wq, xa_wk, xa_wv, xa_wo, norm_ffn_g, moe_group_w, moe_group_b, moe_expert_w, moe_expert_b, moe_w_gate, moe_w_up, moe_w_down, norm_final_g):
    h = x
    for l in range(DEPTH):
        xn = rms_norm(h, norm_mix_g[l])
        p_s5, p_gla, p_rw, p_conv = jnp.split(xn @ w_in[l], COL_SPLITS, axis=-1)
        y_s5 = s5_mixer(p_s5, s5_lam_re[l], s5_lam_im[l], s5_b_re[l], s5_b_im[l], s5_c_re[l], s5_c_im[l],
                        s5_d[l], s5_log_dt[l], s5_glu_w[l], s5_glu_b[l])
        y_gla = gla_mixer(p_gla, gla_w_up[l], gla_b_up[l], gla_norm_g[l])
        y_rw = rwkv7_mixer(p_rw, rw_mu[l], rw_w0[l], rw_w2[l], rw_a0[l], rw_a2[l], rw_g2[l], rw_k_k[l],
                           rw_k_a[l], rw_r_k[l], rw_ln_g[l], rw_ln_b[l])
        y_conv = conv_mixer(p_conv, conv_w[l], conv_b[l], conv_ln_g[l], conv_ln_b[l])
        mixed = jnp.concatenate([y_s5, y_gla, y_rw, y_conv], axis=-1) * mix_beta[l]
        h = h + (mixed @ w_out[l]).astype(h.dtype)
        h = h + memory_xattn(rms_norm(h, norm_xattn_g[l]), rms_norm(mem, norm_mem_g[l]),
                             xa_wq[l], xa_wk[l], xa_wv[l], xa_wo[l])
        h = h + hier_moe(rms_norm(h, norm_ffn_g[l]), moe_group_w[l], moe_group_b[l], moe_expert_w[l],
                         moe_expert_b[l], moe_w_gate[l], moe_w_up[l], moe_w_down[l])
    return rms_norm(h, norm_final_g)
```

```python
import numpy as np
from contextlib import ExitStack
import concourse.bass as bass
import concourse.mybir as mybir
from concourse.bass_utils import run_bass_kernel_spmd

F32 = mybir.dt.float32
BF16 = mybir.dt.bfloat16
I32 = mybir.dt.int32
ALU = mybir.AluOpType
AF = mybir.ActivationFunctionType
AX = mybir.AxisListType

D = 1024
KT = D // 128
IN_COLS = 2448
NDMA = 16


class Prog:
    ENG = ['pe', 'act', 'dve', 'pool', 'sp']

    def __init__(self, nc, es):
        self.nc = nc
        self.es = es
        self.ops = []
        self.last_w = {}
        self.readers = {}
        self.eng_cnt = {e: 0 for e in self.ENG}
        self.dma_cnt = [0] * NDMA
        self.dma_rr = 0

    def op(self, eng, fn, reads=(), writes=(), dma=False):
        deps = set()
        for k in reads:
            if k in self.last_w:
                deps.add(self.last_w[k])
        for k in writes:
            if k in self.last_w:
                deps.add(self.last_w[k])
            deps.update(self.readers.get(k, ()))
        if dma:
            s = self.dma_rr
            self.dma_rr = (s + 1) % NDMA
            prev = self.dma_cnt[s]
            self.dma_cnt[s] += 16
            if prev > 0:
                deps.add(('d%d' % s, prev))
            tok = ('d%d' % s, prev + 16)
        else:
            self.eng_cnt[eng] += 1
            tok = (eng, self.eng_cnt[eng])
        if eng == 'pe':
            deps = {d for d in deps if d[0] != 'pe'}
        self.ops.append((eng, fn, deps, tok, dma))
        for k in writes:
            self.last_w[k] = tok
            self.readers[k] = []
        for k in reads:
            if k not in writes:
                self.readers.setdefault(k, []).append(tok)
        return tok

    def pe(self, fn, reads=(), writes=()):
        return self.op('pe', fn, reads, writes)

    def act(self, fn, reads=(), writes=()):
        return self.op('act', fn, reads, writes)

    def dve(self, fn, reads=(), writes=()):
        return self.op('dve', fn, reads, writes)

    def pool(self, fn, reads=(), writes=()):
        return self.op('pool', fn, reads, writes)

    def dma(self, fn, reads=(), writes=(), q='sp'):
        return self.op(q, fn, reads, writes, dma=True)

    def emit(self):
        nc = self.nc
        sems = {}
        for e in self.ENG:
            sems[e] = self.es.enter_context(nc.semaphore('s_' + e))
        for i in range(NDMA):
            sems['d%d' % i] = self.es.enter_context(nc.semaphore('s_d%d' % i))
        final_waits = [('d%d' % i, self.dma_cnt[i]) for i in range(NDMA) if self.dma_cnt[i] > 0]
        final_waits += [(e, self.eng_cnt[e]) for e in self.ENG if self.eng_cnt[e] > 0]
        by_eng = {e: [o for o in self.ops if o[0] == e] for e in self.ENG}

        class PEProxy:
            def __init__(self, eng, waited):
                self.eng, self.waited, self.last, self.before = eng, waited, None, 0

            def _sync(self, ap):
                rows = ap.partition_size()
                rg = (ap.base_partition(), 32 if rows <= 32 else (64 if rows <= 64 else 128))
                if self.last is not None and rg != self.last and self.before > 0 \
                        and self.waited.get('pe', 0) < self.before:
                    self.eng.wait_ge(sems['pe'], self.before)
                    self.waited['pe'] = self.before
                self.last = rg

            def matmul(self, **kw):
                self._sync(kw['lhsT'])
                return self.eng.matmul(**kw)

            def transpose(self, **kw):
                self._sync(kw['in_'])
                return self.eng.transpose(**kw)

        def run(eng_name, eng):
            waited = {}
            proxy = PEProxy(eng, waited) if eng_name == 'pe' else None
            for (_, fn, deps, tok, dma) in by_eng[eng_name]:
                for (s, v) in sorted(deps):
                    if waited.get(s, 0) < v:
                        eng.wait_ge(sems[s], v)
                        waited[s] = v
                if eng_name == 'pe':
                    proxy.before = tok[1] - 1
                    ins = fn(proxy)
                else:
                    ins = fn(eng)
                ins.then_inc(sems[tok[0]], 16 if dma else 1)
            if eng_name == 'sp':
                for (s, v) in final_waits:
                    if waited.get(s, 0) < v:
                        eng.wait_ge(sems[s], v)

        with nc.Block() as block:
            @block.tensor
            def _(e):
                run('pe', e)

            @block.scalar
            def _(e):
                run('act', e)

            @block.vector
            def _(e):
                run('dve', e)

            @block.gpsimd
            def _(e):
                run('pool', e)

            @block.sync
            def _(e):
                run('sp', e)


class Ctx:
    pass


def build(T, L, dbg=False, phases=('A',)):
    nc = bass.Bass("TRN2", target_bir_lowering=False)
    es = ExitStack()
    P = Prog(nc, es)
    NT = T // 128

    def din(name, shape, dt=F32):
        return nc.dram_tensor(name, list(shape), dt, kind="ExternalInput").ap()

    def dscr(name, shape, dt=F32):
        kind = "ExternalOutput" if dbg else "Internal"
        return nc.dram_tensor(name, list(shape), dt, kind=kind).ap()

    def sb(name, shape, dt=F32):
        return es.enter_context(nc.sbuf_tensor(name, list(shape), dt))

    def ps(name, shape, dt=F32):
        return es.enter_context(nc.psum_tensor(name, list(shape), dt))

    x = din("x", [T, D])
    consts = din("consts", [128, 2112])
    norm_mix_g = din("norm_mix_g", [L, D])
    w_in = din("w_in", [L, D, IN_COLS])
    out = nc.dram_tensor("out", [T, D], F32, kind="ExternalOutput").ap()
    pT = dscr("pT", [IN_COLS, T])
    mixT = (din if 'Ctest' in phases else dscr)("mixT", [D, T])
    memx = din("mem", [256, D])
    w_out = din("w_out", [L, D, D])
    beta_c = din("beta_c", [L, 128, KT])
    norm_xattn_g = din("norm_xattn_g", [L, D])
    norm_mem_g = din("norm_mem_g", [L, D])
    xa_wq = din("xa_wq", [L, D, D]); xa_wk = din("xa_wk", [L, D, D])
    xa_wv = din("xa_wv", [L, D, D]); xa_wo = din("xa_wo", [L, D, D])
    norm_ffn_g = din("norm_ffn_g", [L, D])
    moe_rw = din("moe_rw", [L, D, 36])
    moe_rb = din("moe_rb", [L, 36])
    moe_wg = din("moe_w_gate", [L, 32, D, 512]); moe_wu = din("moe_w_up", [L, 32, D, 512])
    moe_wd = din("moe_w_down", [L, 32, 512, D])
    norm_final_g = din("norm_final_g", [1, D])

    c_f32 = sb("c_f32", [128, 2112])
    ident = sb("ident", [128, 128], BF16)
    ones_bf = sb("ones_bf", [128, 128], BF16)
    P.dma(lambda e: e.dma_start(out=c_f32[:], in_=consts[:, :]), writes=['c_f32'])
    P.act(lambda e: e.copy(out=ident[:], in_=c_f32[:, 0:128]), reads=['c_f32'], writes=['ident'])
    P.dve(lambda e: e.memset(ones_bf[:], 1.0), writes=['ones_bf'])
    identf = c_f32[:, 0:128]

    hb = [sb("hb%d" % i, [128, D]) for i in range(2)]
    ss = [sb("ss%d" % i, [128, 1]) for i in range(2)]
    rstd = [sb("rstd%d" % i, [128, 1]) for i in range(2)]
    xn = [sb("xn%d" % i, [128, D], BF16) for i in range(2)]
    xnf = [sb("xnf%d" % i, [128, D]) for i in range(2)]
    gb = sb("gb", [128, D])
    NTC = min(4, NT)
    TC = NTC * 128
    XN = sb("XN", [128, max(2 * KT * TC, 8192)], BF16)
    xnT = [XN[:, i * KT * TC:(i + 1) * KT * TC].rearrange("p (k t) -> p k t", k=KT) for i in range(2)]
    psT = [ps("psT%d" % i, [128, KT, 128], BF16) for i in range(2)]
    B = [ps("B%d" % i, [128, 512]) for i in range(6)]
    wst = [sb("wst%d" % i, [128, 2560]) for i in range(2)]
    WB = sb("WB", [128, 20480], BF16)
    po = [sb("po%d" % i, [128, 512]) for i in range(2)]
    win_bf = WB[:, 0:KT * IN_COLS].rearrange("p (k n) -> p k n", k=KT)

    cnt = {'tile': 0, 'chunk': 0, 'mm': 0, 'w': 0}

    def load_gb(vec_row):
        P.dma(lambda e: e.dma_start(out=gb[:], in_=vec_row.partition_broadcast(128)), writes=['gb'])

    def rms_to_T(src_rows, dstT, dst_key, tcol, src_key=None, dstTf=None, dstf_key=None):
        i = cnt['tile'] % 2
        cnt['tile'] += 1
        P.dma(lambda e: e.dma_start(out=hb[i][:], in_=src_rows), reads=[src_key] if src_key else [],
              writes=['hb%d' % i])
        P.act(lambda e: e.activation(out=xn[i][:], in_=hb[i][:], func=AF.Square, accum_out=ss[i][:]),
              reads=['hb%d' % i], writes=['xn%d' % i, 'ss%d' % i])
        P.dve(lambda e: e.tensor_scalar(out=rstd[i][:], in0=ss[i][:], scalar1=1.0 / D, scalar2=1e-6,
                                        op0=ALU.mult, op1=ALU.add), reads=['ss%d' % i], writes=['rstd%d' % i])
        P.act(lambda e: e.sqrt(out=rstd[i][:], in_=rstd[i][:]), reads=['rstd%d' % i], writes=['rstd%d' % i])
        P.dve(lambda e: e.reciprocal(out=rstd[i][:], in_=rstd[i][:]), reads=['rstd%d' % i], writes=['rstd%d' % i])
        P.dve(lambda e: e.scalar_tensor_tensor(out=xn[i][:], in0=hb[i][:], scalar=rstd[i][:, 0:1], in1=gb[:],
                                               op0=ALU.mult, op1=ALU.mult),
              reads=['hb%d' % i, 'rstd%d' % i, 'gb'], writes=['xn%d' % i])
        for kt in range(KT):
            P.pe(lambda e, kt=kt: e.transpose(out=psT[i][:, kt, :], in_=xn[i][:, kt * 128:(kt + 1) * 128],
                                              identity=ident[:]),
                 reads=['xn%d' % i, 'ident'], writes=['psT%d' % i])
        P.act(lambda e: e.copy(out=dstT[:, :, tcol:tcol + 128], in_=psT[i][:]),
              reads=['psT%d' % i], writes=[dst_key])
        if dstTf is not None:
            P.dve(lambda e: e.scalar_tensor_tensor(out=xnf[i][:], in0=hb[i][:], scalar=rstd[i][:, 0:1], in1=gb[:],
                                                    op0=ALU.mult, op1=ALU.mult),
                   reads=['hb%d' % i, 'rstd%d' % i, 'gb'], writes=['xnf%d' % i])
            for half in range(2):
                bk = 4 + half
                for q in range(4):
                    kt = half * 4 + q
                    P.pe(lambda e, kt=kt, q=q, bk=bk: e.transpose(out=B[bk][:, q * 128:(q + 1) * 128],
                                                                 in_=xnf[i][:, kt * 128:(kt + 1) * 128],
                                                                 identity=identf),
                         reads=['xnf%d' % i, 'c_f32'], writes=['B%d' % bk])
                P.act(lambda e, half=half, bk=bk: e.copy(
                    out=dstTf[:, half * 4:half * 4 + 4, 0:128],
                    in_=B[bk][:].rearrange("p (q t) -> p q t", q=4)),
                    reads=['B%d' % bk], writes=[dstf_key])

    def load_w_bf(dst3, dst_key, src2d, nk, ncols, scale_col=None, scale_key=None):
        rows_per = max(1, 2560 // ncols)
        k = 0
        while k < nk:
            n = min(rows_per, nk - k)
            j = cnt['w'] % 2
            cnt['w'] += 1
            st = wst[j][:, 0:n * ncols].rearrange("p (k n) -> p k n", k=n)
            P.dma(lambda e, st=st, k=k, n=n: e.dma_start(
                out=st, in_=src2d[k * 128:(k + n) * 128, :].rearrange("(k p) n -> p k n", p=128)),
                writes=['wst%d' % j], q='pool' if (cnt['w'] % 4 < 2) else 'sp')
            if scale_col is None:
                P.pool(lambda e, st=st, k=k, n=n: e.tensor_copy(out=dst3[:, k:k + n, 0:ncols], in_=st),
                       reads=['wst%d' % j], writes=[dst_key])
            else:
                for kk in range(n):
                    P.pool(lambda e, st=st, k=k, kk=kk: e.tensor_scalar(
                        out=dst3[:, k + kk, 0:ncols], in0=st[:, kk, :], scalar1=scale_col[:, k + kk:k + kk + 1],
                        scalar2=None, op0=ALU.mult), reads=['wst%d' % j, scale_key], writes=[dst_key])
            k += n

    fz = sb("fz", [128, 1])
    ALIAS = ['xnT0', 'xnT1', 'xfT', 'mst', 'qT_bf', 'oT_bf', 'macc', 'mxb', 'kT_bf', 'v_bf', 'mnT', 'eT_bf', 'rden',
             'xfTf', 'hidT', 'sg0', 'sg1', 'btok', 'ktok2', 'vtok2'] + ['gm%d' % i for i in range(8)]

    def fence():
        P.dve(lambda e: e.memset(fz[:], 0.0), reads=[], writes=ALIAS + ['fz'])

    def phase_A(l, src):
        fence()
        load_w_bf(win_bf, 'WB', w_in[l], KT, IN_COLS)
        load_gb(norm_mix_g[l:l + 1, :])
        NF = (IN_COLS + 127) // 128
        for c in range(T // TC):
            ci = cnt['chunk'] % 2
            cnt['chunk'] += 1
            for tt in range(NTC):
                r0 = c * TC + tt * 128
                rms_to_T(src[r0:r0 + 128, :], xnT[ci], 'xnT%d' % ci, tt * 128, src_key=('h', r0 // 128))
            for f in range(NF):
                fw = min(128, IN_COLS - f * 128)
                m = cnt['mm'] % 2
                cnt['mm'] += 1
                for kt in range(KT):
                    P.pe(lambda e, f=f, fw=fw, kt=kt, m=m, ci=ci: e.matmul(
                        out=B[m][0:fw, 0:TC], lhsT=win_bf[:, kt, f * 128:f * 128 + fw],
                        rhs=xnT[ci][:, kt, :], start=(kt == 0), stop=(kt == KT - 1)),
                        reads=['WB', 'xnT%d' % ci], writes=['B%d' % m])
                P.act(lambda e, fw=fw, m=m: e.copy(out=po[m][0:fw, 0:TC], in_=B[m][0:fw, 0:TC]),
                      reads=['B%d' % m], writes=['po%d' % m])
                P.dma(lambda e, f=f, fw=fw, m=m, c=c: e.dma_start(
                    out=pT[f * 128:f * 128 + fw, c * TC:(c + 1) * TC], in_=po[m][0:fw, 0:TC]),
                    reads=['po%d' % m], writes=['pT'])

    colv = sb("colv", [128, 64])
    R32 = sb("R32", [128, 8192])
    mst = R32[:, 0:KT * TC].rearrange("p (k t) -> p k t", k=KT)
    PX = sb("PX", [128, 6144])
    mxb = PX[:, 0:2048].bitcast(BF16)[:, 0:KT * TC].rearrange("p (k t) -> p k t", k=KT)
    hacc = [sb("hacc%d" % i, [128, D]) for i in range(2)]
    W2 = WB[:, 0:2 * KT * D].rearrange("p (w k n) -> p w k n", w=2, k=KT)

    def add_to_h(l, src, tok_tile, banks, first_src_x):
        i = cnt['tile'] % 2
        cnt['tile'] += 1
        r0 = tok_tile * 128
        P.dma(lambda e: e.dma_start(out=hacc[i][:], in_=src[r0:r0 + 128, :]), reads=[('h', tok_tile)],
              writes=['hacc%d' % i])
        for half, bk in enumerate(banks):
            P.dve(lambda e, half=half, bk=bk: e.tensor_tensor(
                out=hacc[i][:, half * 512:(half + 1) * 512], in0=hacc[i][:, half * 512:(half + 1) * 512],
                in1=B[bk][:], op=ALU.add), reads=['B%d' % bk, 'hacc%d' % i], writes=['hacc%d' % i])
        P.dma(lambda e: e.dma_start(out=out[r0:r0 + 128, :], in_=hacc[i][:]), reads=['hacc%d' % i],
              writes=[('h', tok_tile)])

    def phase_outproj(l, src):
        fence()
        P.dma(lambda e: e.dma_start(out=colv[:, 0:KT], in_=beta_c[l]), writes=['colv'])
        load_w_bf(W2[:, 0], 'WB', w_out[l], KT, D, scale_col=colv, scale_key='colv')
        for c in range(T // TC):
            P.dma(lambda e, c=c: e.dma_start(
                out=mst[:], in_=mixT[:, c * TC:(c + 1) * TC].rearrange("(k p) t -> p k t", p=128)),
                reads=['mixT'], writes=['mst'])
            P.act(lambda e: e.copy(out=mxb[:], in_=mst[:]), reads=['mst'], writes=['mxb'])
            for tt in range(NTC):
                for half in range(2):
                    for kt in range(KT):
                        P.pe(lambda e, tt=tt, half=half, kt=kt: e.matmul(
                            out=B[half][:], lhsT=mxb[:, kt, tt * 128:(tt + 1) * 128],
                            rhs=W2[:, 0, kt, half * 512:(half + 1) * 512], start=(kt == 0), stop=(kt == KT - 1)),
                            reads=['mxb', 'WB'], writes=['B%d' % half])
                add_to_h(l, src, c * NTC + tt, [0, 1], False)

    kT_bf = PX[:, 0:1024].bitcast(BF16).rearrange("p (k t) -> p k t", k=KT)
    v_bf = PX[:, 1024:2048].bitcast(BF16).rearrange("p (k t) -> p k t", k=2)
    mnT = PX[:, 2048:3072].bitcast(BF16).rearrange("p (k t) -> p k t", k=KT)
    qT_bf = R32[:, 4096:6144].bitcast(BF16)[:, 0:KT * TC].rearrange("p (k t) -> p k t", k=KT)
    eT_bf = PX[:, 3072:3584].bitcast(BF16)[:, 0:2 * TC].rearrange("p (k t) -> p k t", k=2)
    oT_bf = R32[:, 6144:8192].bitcast(BF16)[:, 0:KT * TC].rearrange("p (k t) -> p k t", k=KT)
    rden = PX[:, 3584:3584 + TC]

    def phase_xattn(l):
        fence()
        load_gb(norm_mem_g[l:l + 1, :])
        for mt in range(2):
            rms_to_T(memx[mt * 128:(mt + 1) * 128, :], mnT, 'mnT', mt * 128)
        load_w_bf(W2[:, 0], 'WB', xa_wk[l], KT, D)
        load_w_bf(W2[:, 1], 'WB', xa_wv[l], KT, D)
        for f in range(KT):
            m = f % 2
            for kt in range(KT):
                P.pe(lambda e, f=f, kt=kt, m=m: e.matmul(out=B[m][:, 0:256], lhsT=W2[:, 0, kt, f * 128:(f + 1) * 128],
                                                        rhs=mnT[:, kt, :], start=(kt == 0), stop=(kt == KT - 1)),
                     reads=['WB', 'mnT'], writes=['B%d' % m])
            P.act(lambda e, f=f, m=m: e.copy(out=kT_bf[:, f, :], in_=B[m][:, 0:256]), reads=['B%d' % m],
                  writes=['kT_bf'])
        for mt in range(2):
            for half in range(2):
                m = 2 + half
                for kt in range(KT):
                    P.pe(lambda e, mt=mt, half=half, kt=kt, m=m: e.matmul(
                        out=B[m][:], lhsT=mnT[:, kt, mt * 128:(mt + 1) * 128],
                        rhs=W2[:, 1, kt, half * 512:(half + 1) * 512], start=(kt == 0), stop=(kt == KT - 1)),
                        reads=['WB', 'mnT'], writes=['B%d' % m])
                P.act(lambda e, mt=mt, half=half, m=m: e.copy(out=v_bf[:, mt, half * 512:(half + 1) * 512],
                                                             in_=B[m][:]), reads=['B%d' % m], writes=['v_bf'])
        load_w_bf(W2[:, 0], 'WB', xa_wq[l], KT, D)
        load_w_bf(W2[:, 1], 'WB', xa_wo[l], KT, D)
        load_gb(norm_xattn_g[l:l + 1, :])
        for c in range(T // TC):
            ci = cnt['chunk'] % 2
            cnt['chunk'] += 1
            for tt in range(NTC):
                r0 = c * TC + tt * 128
                rms_to_T(out[r0:r0 + 128, :], xnT[ci], 'xnT%d' % ci, tt * 128, src_key=('h', r0 // 128))
            for f in range(KT):
                m = f % 2
                for kt in range(KT):
                    P.pe(lambda e, f=f, kt=kt, m=m, ci=ci: e.matmul(
                        out=B[m][:, 0:TC], lhsT=W2[:, 0, kt, f * 128:(f + 1) * 128], rhs=xnT[ci][:, kt, :],
                        start=(kt == 0), stop=(kt == KT - 1)), reads=['WB', 'xnT%d' % ci], writes=['B%d' % m])
                P.act(lambda e, f=f, m=m: e.copy(out=qT_bf[:, f, :], in_=B[m][:, 0:TC]), reads=['B%d' % m],
                      writes=['qT_bf'])
            for hd in range(4):
                for mt in range(2):
                    m = 2 + mt
                    for ff in range(2):
                        f = hd * 2 + ff
                        P.pe(lambda e, f=f, ff=ff, mt=mt, m=m: e.matmul(
                            out=B[m][:, 0:TC], lhsT=kT_bf[:, f, mt * 128:(mt + 1) * 128], rhs=qT_bf[:, f, :],
                            start=(ff == 0), stop=(ff == 1)), reads=['kT_bf', 'qT_bf'], writes=['B%d' % m])
                    P.act(lambda e, mt=mt, m=m: e.activation(out=eT_bf[:, mt, :], in_=B[m][:, 0:TC], func=AF.Exp,
                                                            scale=1.0 / 16.0), reads=['B%d' % m], writes=['eT_bf'])
                for mt in range(2):
                    P.pe(lambda e, mt=mt: e.matmul(out=B[4][:, 0:TC], lhsT=ones_bf[:], rhs=eT_bf[:, mt, :],
                                                   start=(mt == 0), stop=(mt == 1)),
                         reads=['ones_bf', 'eT_bf'], writes=['B4'])
                P.dve(lambda e: e.reciprocal(out=rden[:], in_=B[4][:, 0:TC]), reads=['B4'], writes=['rden'])
                for ff in range(2):
                    f = hd * 2 + ff
                    for mt in range(2):
                        P.pe(lambda e, f=f, mt=mt: e.matmul(out=B[5][:, 0:TC], lhsT=v_bf[:, mt, f * 128:(f + 1) * 128],
                                                            rhs=eT_bf[:, mt, :], start=(mt == 0), stop=(mt == 1)),
                             reads=['v_bf', 'eT_bf'], writes=['B5'])
                    P.dve(lambda e, f=f: e.tensor_tensor(out=oT_bf[:, f, :], in0=B[5][:, 0:TC], in1=rden[:],
                                                         op=ALU.mult), reads=['B5', 'rden'], writes=['oT_bf'])
            for tt in range(NTC):
                for half in range(2):
                    for f in range(KT):
                        P.pe(lambda e, tt=tt, half=half, f=f: e.matmul(
                            out=B[half][:], lhsT=oT_bf[:, f, tt * 128:(tt + 1) * 128],
                            rhs=W2[:, 1, f, half * 512:(half + 1) * 512], start=(f == 0), stop=(f == KT - 1)),
                            reads=['oT_bf', 'WB'], writes=['B%d' % half])
                add_to_h(l, out, c * NTC + tt, [0, 1], False)

    TM = min(T, 2 * TC)
    NTM = TM // 128
    TBH = min(512, TM)
    xfT = XN[:, 0:KT * TM].rearrange("p (k t) -> p k t", k=KT)
    xfTf = PX[:, 3072:4096].rearrange("p (k t) -> p k t", k=KT)
    rw = sb("rw", [128, KT, 36])
    rbb = sb("rbb", [128, 36])
    lg = sb("lg", [128, 36])
    gates = sb("gates", [128, NTM, 32])
    rt = sb("rt", [128, 8, 32])
    r1 = sb("r1", [128, 16])
    macc = R32[:, 0:NTM * D].rearrange("p (t d) -> p t d", t=NTM)
    hidT = PX[:, 0:2048].bitcast(BF16)[:, 0:4 * TM].rearrange("p (k t) -> p k t", k=4)
    sg = [PX[:, 2048 + i * 512:2560 + i * 512] for i in range(2)]
    EW = WB[:, 0:3 * 4096].rearrange("p (w n) -> p w n", w=3)
    wg_bf = EW[:, 0].rearrange("p (k n) -> p k n", k=8)
    wu_bf = EW[:, 1].rearrange("p (k n) -> p k n", k=8)
    wd_bf = EW[:, 2].rearrange("p (k n) -> p k n", k=4)
    BIG = 1.0e4
    dbg_g = dscr("dbg_g", [128, 32]); dbg_r1 = dscr("dbg_r1", [128, 16]); dbg_lg = dscr("dbg_lg", [128, 36])

    def route(tt):
        g = lambda i: rt[:, i, :]
        dv = lambda fn, r, w: P.dve(fn, reads=r, writes=w)
        dv(lambda e: e.tensor_reduce(out=r1[:, 0:1], in_=lg[:, 0:4], axis=AX.X, op=ALU.max), ['lg'], ['r1'])
        dv(lambda e: e.tensor_scalar(out=rt[:, 0, 0:4], in0=lg[:, 0:4], scalar1=r1[:, 0:1], scalar2=None,
                                     op0=ALU.is_equal), ['lg', 'r1'], ['rt'])
        dv(lambda e: e.tensor_scalar(out=rt[:, 1, 0:4], in0=lg[:, 0:4], scalar1=r1[:, 0:1], scalar2=None,
                                     op0=ALU.subtract), ['lg', 'r1'], ['rt'])
        P.act(lambda e: e.activation(out=rt[:, 1, 0:4], in_=rt[:, 1, 0:4], func=AF.Exp, accum_out=r1[:, 1:2]),
              ['rt'], ['rt', 'r1'])
        dv(lambda e: e.reciprocal(out=r1[:, 2:3], in_=r1[:, 1:2]), ['r1'], ['r1'])
        dv(lambda e: e.tensor_scalar(out=rt[:, 0, 0:4], in0=rt[:, 0, 0:4], scalar1=-1.0, scalar2=BIG,
                                     op0=ALU.add, op1=ALU.mult), ['rt'], ['rt'])
        dv(lambda e: e.tensor_tensor(out=rt[:, 2, :].rearrange("p (g x) -> p g x", g=4),
                                     in0=lg[:, 4:36].rearrange("p (g x) -> p g x", g=4),
                                     in1=rt[:, 0, 0:4].unsqueeze(2).to_broadcast([128, 4, 8]), op=ALU.add),
           ['lg', 'rt'], ['rt'])
        dv(lambda e: e.tensor_reduce(out=r1[:, 3:4], in_=g(2), axis=AX.X, op=ALU.max), ['rt'], ['r1'])
        dv(lambda e: e.tensor_scalar(out=g(3), in0=g(2), scalar1=r1[:, 3:4], scalar2=None, op0=ALU.is_equal),
           ['rt', 'r1'], ['rt'])
        dv(lambda e: e.scalar_tensor_tensor(out=g(4), in0=g(3), scalar=-BIG, in1=g(2), op0=ALU.mult, op1=ALU.add),
           ['rt'], ['rt'])
        dv(lambda e: e.tensor_reduce(out=r1[:, 4:5], in_=g(4), axis=AX.X, op=ALU.max), ['rt'], ['r1'])
        dv(lambda e: e.tensor_scalar(out=g(5), in0=g(4), scalar1=r1[:, 4:5], scalar2=None, op0=ALU.is_equal),
           ['rt', 'r1'], ['rt'])
        dv(lambda e: e.tensor_tensor(out=r1[:, 5:6], in0=r1[:, 4:5], in1=r1[:, 3:4], op=ALU.subtract),
           ['r1'], ['r1'])
        P.act(lambda e: e.activation(out=r1[:, 6:7], in_=r1[:, 5:6], func=AF.Exp), ['r1'], ['r1'])
        dv(lambda e: e.tensor_scalar(out=r1[:, 7:8], in0=r1[:, 6:7], scalar1=1.0, scalar2=None, op0=ALU.add),
           ['r1'], ['r1'])
        dv(lambda e: e.reciprocal(out=r1[:, 7:8], in_=r1[:, 7:8]), ['r1'], ['r1'])
        dv(lambda e: e.tensor_tensor(out=r1[:, 8:9], in0=r1[:, 7:8], in1=r1[:, 2:3], op=ALU.mult), ['r1'], ['r1'])
        dv(lambda e: e.tensor_tensor(out=r1[:, 9:10], in0=r1[:, 2:3], in1=r1[:, 8:9], op=ALU.subtract),
           ['r1'], ['r1'])
        dv(lambda e: e.tensor_scalar(out=g(6), in0=g(3), scalar1=r1[:, 8:9], scalar2=None, op0=ALU.mult),
           ['rt', 'r1'], ['rt'])
        dv(lambda e: e.scalar_tensor_tensor(out=gates[:, tt, :], in0=g(5), scalar=r1[:, 9:10], in1=g(6),
                                            op0=ALU.mult, op1=ALU.add), ['rt', 'r1'], ['gates'])

    def phase_moe(l):
        fence()
        load_gb(norm_ffn_g[l:l + 1, :])
        P.dma(lambda e: e.dma_start(out=rw[:], in_=moe_rw[l].rearrange("(k p) n -> p k n", p=128)), writes=['rw'])
        P.dma(lambda e: e.dma_start(out=rbb[:], in_=moe_rb[l:l + 1, :].partition_broadcast(128)), writes=['rbb'])
        for c in range(T // TM):
            for tt in range(NTM):
                r0 = c * TM + tt * 128
                rms_to_T(out[r0:r0 + 128, :], xfT, 'xfT', tt * 128, src_key=('h', r0 // 128),
                         dstTf=xfTf, dstf_key='xfTf')
                for kt in range(KT):
                    P.pe(lambda e, kt=kt: e.matmul(out=B[3][:, 0:36], lhsT=xfTf[:, kt, :], rhs=rw[:, kt, :],
                                                   start=(kt == 0), stop=(kt == KT - 1)),
                         reads=['xfTf', 'rw'], writes=['B3'])
                P.dve(lambda e: e.tensor_tensor(out=lg[:], in0=B[3][:, 0:36], in1=rbb[:], op=ALU.add),
                      reads=['B3', 'rbb'], writes=['lg'])
                route(tt)
            if dbg:
                P.dma(lambda e: e.dma_start(out=dbg_g[:, :], in_=gates[:, 0, :]), reads=['gates'], writes=['dbg_g'])
                P.dma(lambda e: e.dma_start(out=dbg_r1[:, :], in_=r1[:, :]), reads=['r1'], writes=['dbg_r1'])
                P.dma(lambda e: e.dma_start(out=dbg_lg[:, :], in_=lg[:, :]), reads=['lg'], writes=['dbg_lg'])
            for ex in range(32):
                load_w_bf(wg_bf, 'WB', moe_wg[l, ex], 8, 512)
                load_w_bf(wu_bf, 'WB', moe_wu[l, ex], 8, 512)
                load_w_bf(wd_bf, 'WB', moe_wd[l, ex], 4, D)
                for tb in range(TM // TBH):
                    for mt in range(4):
                        for kt in range(KT):
                            P.pe(lambda e, mt=mt, kt=kt, tb=tb: e.matmul(
                                out=B[0][:, 0:TBH], lhsT=wg_bf[:, kt, mt * 128:(mt + 1) * 128],
                                rhs=xfT[:, kt, tb * TBH:(tb + 1) * TBH], start=(kt == 0), stop=(kt == KT - 1)),
                                reads=['WB', 'xfT'], writes=['B0'])
                        for kt in range(KT):
                            P.pe(lambda e, mt=mt, kt=kt, tb=tb: e.matmul(
                                out=B[1][:, 0:TBH], lhsT=wu_bf[:, kt, mt * 128:(mt + 1) * 128],
                                rhs=xfT[:, kt, tb * TBH:(tb + 1) * TBH], start=(kt == 0), stop=(kt == KT - 1)),
                                reads=['WB', 'xfT'], writes=['B1'])
                        j = cnt['mm'] % 2
                        cnt['mm'] += 1
                        P.act(lambda e, j=j: e.activation(out=sg[j][:, 0:TBH], in_=B[0][:, 0:TBH], func=AF.Silu),
                              reads=['B0'], writes=['sg%d' % j])
                        P.dve(lambda e, j=j, mt=mt, tb=tb: e.tensor_tensor(
                            out=hidT[:, mt, tb * TBH:(tb + 1) * TBH], in0=sg[j][:, 0:TBH], in1=B[1][:, 0:TBH], op=ALU.mult),
                            reads=['sg%d' % j, 'B1'], writes=['hidT'])
                for tt in range(NTM):
                    for half in range(2):
                        bk = 2 + half
                        for kt in range(4):
                            P.pe(lambda e, tt=tt, half=half, kt=kt, bk=bk: e.matmul(
                                out=B[bk][:], lhsT=hidT[:, kt, tt * 128:(tt + 1) * 128],
                                rhs=wd_bf[:, kt, half * 512:(half + 1) * 512], start=(kt == 0), stop=(kt == 3)),
                                reads=['hidT', 'WB'], writes=['B%d' % bk])
                        sl = macc[:, tt, half * 512:(half + 1) * 512]
                        if ex == 0:
                            P.dve(lambda e, sl=sl, tt=tt, bk=bk, ex=ex: e.tensor_scalar(
                                out=sl, in0=B[bk][:], scalar1=gates[:, tt, ex:ex + 1], scalar2=None, op0=ALU.mult),
                                reads=['B%d' % bk, 'gates'], writes=['macc'])
                        else:
                            P.dve(lambda e, sl=sl, tt=tt, bk=bk, ex=ex: e.scalar_tensor_tensor(
                                out=sl, in0=B[bk][:], scalar=gates[:, tt, ex:ex + 1], in1=sl, op0=ALU.mult,
                                op1=ALU.add), reads=['B%d' % bk, 'gates', 'macc'], writes=['macc'])
            for tt in range(NTM):
                i = cnt['tile'] % 2
                cnt['tile'] += 1
                r0 = c * TM + tt * 128
                tk = r0 // 128
                P.dma(lambda e, i=i, r0=r0: e.dma_start(out=hacc[i][:], in_=out[r0:r0 + 128, :]),
                      reads=[('h', tk)], writes=['hacc%d' % i])
                P.pool(lambda e, i=i, tt=tt: e.tensor_tensor(out=hacc[i][:], in0=hacc[i][:], in1=macc[:, tt, :],
                                                             op=ALU.add), reads=['hacc%d' % i, 'macc'],
                       writes=['hacc%d' % i])
                P.dma(lambda e, i=i, r0=r0: e.dma_start(out=out[r0:r0 + 128, :], in_=hacc[i][:]),
                      reads=['hacc%d' % i], writes=[('h', tk)])

    def phase_final():
        load_gb(norm_final_g[0:1, :])
        for tk in range(NT):
            i = cnt['tile'] % 2
            cnt['tile'] += 1
            r0 = tk * 128
            P.dma(lambda e, i=i, r0=r0: e.dma_start(out=hb[i][:], in_=out[r0:r0 + 128, :]), reads=[('h', tk)],
                  writes=['hb%d' % i])
            P.act(lambda e, i=i: e.activation(out=xn[i][:], in_=hb[i][:], func=AF.Square, accum_out=ss[i][:]),
                  reads=['hb%d' % i], writes=['xn%d' % i, 'ss%d' % i])
            P.dve(lambda e, i=i: e.tensor_scalar(out=rstd[i][:], in0=ss[i][:], scalar1=1.0 / D, scalar2=1e-6,
                                                 op0=ALU.mult, op1=ALU.add), reads=['ss%d' % i], writes=['rstd%d' % i])
            P.act(lambda e, i=i: e.sqrt(out=rstd[i][:], in_=rstd[i][:]), reads=['rstd%d' % i], writes=['rstd%d' % i])
            P.dve(lambda e, i=i: e.reciprocal(out=rstd[i][:], in_=rstd[i][:]), reads=['rstd%d' % i],
                  writes=['rstd%d' % i])
            P.dve(lambda e, i=i: e.scalar_tensor_tensor(out=xnf[i][:], in0=hb[i][:], scalar=rstd[i][:, 0:1], in1=gb[:],
                                                        op0=ALU.mult, op1=ALU.mult),
                  reads=['hb%d' % i, 'rstd%d' % i, 'gb'], writes=['xnf%d' % i])
            P.dma(lambda e, i=i, r0=r0: e.dma_start(out=out[r0:r0 + 128, :], in_=xnf[i][:]), reads=['xnf%d' % i],
                  writes=[('h', tk)])

    colsB = din("colsB", [L, 128, 80])
    gla_w_up = din("gla_w_up", [L, 16, 128])
    TBk = min(512, T)
    Wt = [R32[:, i * 512:(i + 1) * 512] for i in range(16)] + \
         [XN[:, i * 1024:(i + 1) * 1024].bitcast(F32) for i in range(8)]
    WK = ['Wt%d' % i for i in range(24)]
    ALIAS.extend(WK)
    cB = sb("cB", [128, 80])
    onesm = sb("onesm", [128, 128])
    ones64 = sb("ones64", [128, 64])
    P.dve(lambda e: e.memset(onesm[:], 1.0 / 256.0), writes=['onesm'])
    P.dve(lambda e: e.memset(ones64[:], 1.0), writes=['ones64'])
    ubuf = [sb("ubuf%d" % i, [128, 30 + TBk]) for i in range(2)]
    stG = sb("stG", [64, 128])
    wup = sb("wup", [16, 128])
    zT = sb("zT", [16, TBk])
    ktok = sb("ktok", [128, 4, 128])
    vtok = sb("vtok", [128, 4, 256])
    ark = sb("ark", [128, 256])
    mask_incl = c_f32[:, 128:384]
    blk64 = c_f32[:, 384:512]

    dbg_outs = {}

    def dbgdump(name, ap, key, shape):
        if not dbg:
            return
        t = nc.dram_tensor("dbg_" + name, list(shape), F32, kind="ExternalOutput").ap()
        P.dma(lambda e: e.dma_start(out=t, in_=ap), reads=[key], writes=['dbg_' + name])

    def ldrow(w, row0, c0, n=128, cols=None):
        cols = TBk if cols is None else cols
        P.dma(lambda e: e.dma_start(out=Wt[w][0:n, 0:cols], in_=pT[row0:row0 + n, c0:c0 + cols]),
              reads=['pT'], writes=[WK[w]])

    def strow(w, row0, c0):
        P.dma(lambda e: e.dma_start(out=mixT[row0:row0 + 128, c0:c0 + TBk], in_=Wt[w][:, 0:TBk]),
              reads=[WK[w]], writes=['mixT'])

    def conv_block(l, blk):
        c0 = blk * TBk
        for ct in range(2):
            ub = ubuf[ct]
            uk = 'ubuf%d' % ct
            if blk == 0:
                P.pool(lambda e, ub=ub: e.memset(ub[:, 0:30], 0.0), writes=[uk])
            else:
                P.act(lambda e, ub=ub: e.copy(out=ub[:, 0:30], in_=ub[:, TBk:TBk + 30]), reads=[uk], writes=[uk])
            ldrow(0, 1936 + ct * 128, c0)
            ldrow(1, 2192 + ct * 128, c0)
            P.act(lambda e: e.activation(out=Wt[1][:, 0:TBk], in_=Wt[1][:, 0:TBk], func=AF.Sigmoid),
                  reads=[WK[1]], writes=[WK[1]])
            P.pool(lambda e, ub=ub: e.tensor_tensor(out=ub[:, 30:30 + TBk], in0=Wt[0][:, 0:TBk], in1=Wt[1][:, 0:TBk],
                                                    op=ALU.mult), reads=[WK[0], WK[1], uk], writes=[uk])
            acc = 2 + ct
            P.dve(lambda e, ub=ub, ct=ct, acc=acc: e.tensor_scalar(
                out=Wt[acc][:, 0:TBk], in0=ub[:, 0:TBk], scalar1=cB[:, ct * 31:ct * 31 + 1], scalar2=None,
                op0=ALU.mult), reads=[uk, 'cB'], writes=[WK[acc]])
            for k in range(1, 31):
                P.dve(lambda e, ub=ub, ct=ct, acc=acc, k=k: e.scalar_tensor_tensor(
                    out=Wt[acc][:, 0:TBk], in0=ub[:, k:k + TBk], scalar=cB[:, ct * 31 + k:ct * 31 + k + 1],
                    in1=Wt[acc][:, 0:TBk], op0=ALU.mult, op1=ALU.add), reads=[uk, 'cB', WK[acc]], writes=[WK[acc]])
            P.act(lambda e, ct=ct, acc=acc: e.activation(out=Wt[acc][:, 0:TBk], in_=Wt[acc][:, 0:TBk],
                                                         func=AF.Identity, bias=cB[:, 62 + ct:63 + ct]),
                  reads=[WK[acc], 'cB'], writes=[WK[acc]])
            P.act(lambda e, ct=ct, acc=acc: e.activation(out=Wt[4 + ct][:, 0:TBk], in_=Wt[acc][:, 0:TBk],
                                                         func=AF.Square), reads=[WK[acc]], writes=[WK[4 + ct]])
        for ct in range(2):
            P.pe(lambda e, ct=ct: e.matmul(out=B[0][:, 0:TBk], lhsT=onesm[:], rhs=Wt[2 + ct][:, 0:TBk],
                                           start=(ct == 0), stop=(ct == 1)), reads=['onesm', WK[2 + ct]], writes=['B0'])
        for ct in range(2):
            P.pe(lambda e, ct=ct: e.matmul(out=B[1][:, 0:TBk], lhsT=onesm[:], rhs=Wt[4 + ct][:, 0:TBk],
                                           start=(ct == 0), stop=(ct == 1)), reads=['onesm', WK[4 + ct]], writes=['B1'])
        P.act(lambda e: e.copy(out=Wt[6][:, 0:TBk], in_=B[0][:, 0:TBk]), reads=['B0'], writes=[WK[6]])
        P.dve(lambda e: e.tensor_tensor(out=Wt[7][:, 0:TBk], in0=Wt[6][:, 0:TBk], in1=Wt[6][:, 0:TBk], op=ALU.mult),
              reads=[WK[6]], writes=[WK[7]])
        P.dve(lambda e: e.tensor_tensor(out=Wt[7][:, 0:TBk], in0=B[1][:, 0:TBk], in1=Wt[7][:, 0:TBk],
                                        op=ALU.subtract), reads=['B1', WK[7]], writes=[WK[7]])
        P.dve(lambda e: e.tensor_scalar(out=Wt[7][:, 0:TBk], in0=Wt[7][:, 0:TBk], scalar1=1e-5, scalar2=None,
                                        op0=ALU.add), reads=[WK[7]], writes=[WK[7]])
        P.act(lambda e: e.sqrt(out=Wt[7][:, 0:TBk], in_=Wt[7][:, 0:TBk]), reads=[WK[7]], writes=[WK[7]])
        P.dve(lambda e: e.reciprocal(out=Wt[7][:, 0:TBk], in_=Wt[7][:, 0:TBk]), reads=[WK[7]], writes=[WK[7]])
        for ct in range(2):
            acc = 2 + ct
            P.dve(lambda e, acc=acc: e.tensor_tensor(out=Wt[acc][:, 0:TBk], in0=Wt[acc][:, 0:TBk],
                                                     in1=Wt[6][:, 0:TBk], op=ALU.subtract),
                  reads=[WK[acc], WK[6]], writes=[WK[acc]])
            P.dve(lambda e, acc=acc: e.tensor_tensor(out=Wt[acc][:, 0:TBk], in0=Wt[acc][:, 0:TBk],
                                                     in1=Wt[7][:, 0:TBk], op=ALU.mult),
                  reads=[WK[acc], WK[7]], writes=[WK[acc]])
            P.act(lambda e, acc=acc, ct=ct: e.activation(out=Wt[acc][:, 0:TBk], in_=Wt[acc][:, 0:TBk], func=AF.Silu,
                                                         scale=cB[:, 64 + ct:65 + ct], bias=cB[:, 66 + ct:67 + ct]),
                  reads=[WK[acc], 'cB'], writes=[WK[acc]])
            strow(acc, 768 + ct * 128, c0)

    def gla_block(l, blk):
        c0 = blk * TBk
        NCH = TBk // 64
        QT, KTt, LA, BC, EB, ENB, RT, KTT = (0, 1), (2, 3), (4, 5), (6, 7), (8, 9), (10, 11), (12, 13), (14, 15)
        V0, V1, G0, G1, OB0, OB1, SQ, TMP = 16, 17, 18, 19, 20, 21, 22, 23
        for hf in range(2):
            ldrow(QT[hf], 256 + hf * 64, c0, n=64); ldrow(KTt[hf], 384 + hf * 64, c0, n=64)
        ldrow(V0, 512, c0); ldrow(V1, 640, c0)
        ldrow(G0, 768, c0); ldrow(G1, 896, c0)
        P.dma(lambda e: e.dma_start(out=zT[:, :], in_=pT[1024:1040, c0:c0 + TBk]), reads=['pT'], writes=['zT'])
        if blk == 0:
            P.pool(lambda e: e.memset(stG[:], 0.0), writes=['stG'])
        for hf in range(2):
            la, bc, eb, enb, rt, ktt = Wt[LA[hf]], Wt[BC[hf]], Wt[EB[hf]], Wt[ENB[hf]], Wt[RT[hf]], Wt[KTT[hf]]
            kla, kbc, keb, kenb, krt, kktt = (WK[LA[hf]], WK[BC[hf]], WK[EB[hf]], WK[ENB[hf]], WK[RT[hf]],
                                              WK[KTT[hf]])
            P.pe(lambda e, hf=hf: e.matmul(out=B[0][0:64, 0:TBk], lhsT=wup[:, hf * 64:(hf + 1) * 64], rhs=zT[:, :],
                                           start=True, stop=True), reads=['wup', 'zT'], writes=['B0'])
            P.act(lambda e, la=la, hf=hf: e.activation(out=la[0:64, 0:TBk], in_=B[0][0:64, 0:TBk], func=AF.Exp,
                                                       scale=-1.0, bias=cB[0:64, 71 + 2 * hf:72 + 2 * hf]),
                  reads=['B0', 'cB'], writes=[kla])
            P.act(lambda e, la=la: e.activation(out=la[0:64, 0:TBk], in_=la[0:64, 0:TBk], func=AF.Ln, bias=1.0),
                  reads=[kla], writes=[kla])
            P.dve(lambda e, la=la: e.tensor_scalar(out=la[0:64, 0:TBk], in0=la[0:64, 0:TBk], scalar1=-1.0 / 16.0,
                                                   scalar2=None, op0=ALU.mult), reads=[kla], writes=[kla])
            for c in range(NCH):
                P.dve(lambda e, c=c, la=la, bc=bc: e.tensor_tensor_scan(
                    out=bc[0:64, c * 64:(c + 1) * 64], data0=ones64[0:64, :], data1=la[0:64, c * 64:(c + 1) * 64],
                    initial=0.0, op0=ALU.mult, op1=ALU.add), reads=['ones64', kla], writes=[kbc])
            P.act(lambda e, bc=bc, eb=eb: e.activation(out=eb[0:64, 0:TBk], in_=bc[0:64, 0:TBk], func=AF.Exp),
                  reads=[kbc], writes=[keb])
            P.act(lambda e, bc=bc, enb=enb: e.activation(out=enb[0:64, 0:TBk], in_=bc[0:64, 0:TBk], func=AF.Exp,
                                                         scale=-1.0), reads=[kbc], writes=[kenb])
            P.dve(lambda e, hf=hf, rt=rt, eb=eb: e.scalar_tensor_tensor(
                out=rt[0:64, 0:TBk], in0=Wt[QT[hf]][0:64, 0:TBk], scalar=32.0 ** -0.5, in1=eb[0:64, 0:TBk],
                op0=ALU.mult, op1=ALU.mult), reads=[WK[QT[hf]], keb], writes=[krt])
            P.pool(lambda e, hf=hf, ktt=ktt, enb=enb: e.tensor_tensor(
                out=ktt[0:64, 0:TBk], in0=Wt[KTt[hf]][0:64, 0:TBk], in1=enb[0:64, 0:TBk], op=ALU.mult),
                reads=[WK[KTt[hf]], kenb], writes=[kktt])
        if DBG_STOP <= 1:
            return
        for b4 in range(TBk // 128):
            sl = slice(b4 * 128, (b4 + 1) * 128)
            for hf in range(2):
                P.pe(lambda e, sl=sl, hf=hf: e.transpose(out=B[1][:, hf * 64:(hf + 1) * 64],
                                                         in_=Wt[KTT[hf]][0:64, sl], identity=identf[0:64, 0:64]),
                     reads=[WK[KTT[hf]], 'c_f32'], writes=['B1'])
            P.pe(lambda e, sl=sl: e.transpose(out=B[1][:, 128:256], in_=Wt[V0][:, sl], identity=identf),
                 reads=[WK[V0], 'c_f32'], writes=['B1'])
            P.pe(lambda e, sl=sl: e.transpose(out=B[1][:, 256:384], in_=Wt[V1][:, sl], identity=identf),
                 reads=[WK[V1], 'c_f32'], writes=['B1'])
            P.act(lambda e, b4=b4: e.copy(out=ktok[:, b4, :], in_=B[1][:, 0:128]), reads=['B1'], writes=['ktok'])
            P.act(lambda e, b4=b4: e.copy(out=vtok[:, b4, :], in_=B[1][:, 128:384]), reads=['B1'], writes=['vtok'])
        if DBG_STOP <= 2:
            return
        for c in range(NCH):
            b4, pb = c // 2, (c % 2) * 64
            cs = slice(c * 64, (c + 1) * 64)
            for hd in range(4):
                hf = hd // 2
                hs = slice((hd % 2) * 32, (hd % 2) * 32 + 32)
                P.pe(lambda e, hd=hd, hf=hf, hs=hs, cs=cs, pb=pb: e.matmul(
                    out=B[2][pb:pb + 64, hd * 64:(hd + 1) * 64], lhsT=Wt[KTT[hf]][hs, cs], rhs=Wt[RT[hf]][hs, cs],
                    start=True, stop=True), reads=[WK[KTT[hf]], WK[RT[hf]]], writes=['B2'])
            if DBG_STOP == 3 and DBG_VAR == 1:
                continue
            P.dve(lambda e, pb=pb: e.tensor_tensor(out=ark[pb:pb + 64, :], in0=B[2][pb:pb + 64, 0:256],
                                                   in1=mask_incl[pb:pb + 64, :], op=ALU.mult),
                  reads=['B2', 'c_f32'], writes=['ark'])
            if DBG_STOP <= 3:
                continue
            for hd in range(4):
                hf = hd // 2
                hs = slice((hd % 2) * 32, (hd % 2) * 32 + 32)
                vt, vb = hd // 2, (hd % 2) * 64
                P.pe(lambda e, hf=hf, hs=hs, cs=cs, vt=vt, vb=vb: e.matmul(
                    out=B[3][vb:vb + 64, vt * 64:(vt + 1) * 64], lhsT=stG[hs, hf * 64:(hf + 1) * 64],
                    rhs=Wt[RT[hf]][hs, cs], start=True, stop=False), reads=['stG', WK[RT[hf]]], writes=['B3'])
                P.pe(lambda e, hd=hd, b4=b4, pb=pb, vt=vt, vb=vb: e.matmul(
                    out=B[3][vb:vb + 64, vt * 64:(vt + 1) * 64], lhsT=vtok[pb:pb + 64, b4, hd * 64:(hd + 1) * 64],
                    rhs=ark[pb:pb + 64, hd * 64:(hd + 1) * 64], start=False, stop=True),
                    reads=['vtok', 'ark'], writes=['B3'])
            P.act(lambda e, cs=cs: e.copy(out=Wt[OB0][:, cs], in_=B[3][:, 0:64]), reads=['B3'], writes=[WK[OB0]])
            P.act(lambda e, cs=cs: e.copy(out=Wt[OB1][:, cs], in_=B[3][:, 64:128]), reads=['B3'], writes=[WK[OB1]])
            if DBG_STOP <= 4:
                continue
            for hd in range(4):
                hf = hd // 2
                hs = slice((hd % 2) * 32, (hd % 2) * 32 + 32)
                P.pe(lambda e, hd=hd, hf=hf, hs=hs, b4=b4, pb=pb: e.matmul(
                    out=B[4][hs, hf * 64:(hf + 1) * 64], lhsT=ktok[pb:pb + 64, b4, hd * 32:(hd + 1) * 32],
                    rhs=vtok[pb:pb + 64, b4, hd * 64:(hd + 1) * 64], start=True, stop=True),
                    reads=['ktok', 'vtok'], writes=['B4'])
            P.dve(lambda e: e.tensor_tensor(out=stG[0:64, :], in0=B[4][0:64, 0:128], in1=stG[0:64, :], op=ALU.add),
                  reads=['B4', 'stG'], writes=['stG'])
            for hf in range(2):
                P.dve(lambda e, c=c, hf=hf: e.tensor_scalar(
                    out=stG[0:64, hf * 64:(hf + 1) * 64], in0=stG[0:64, hf * 64:(hf + 1) * 64],
                    scalar1=Wt[EB[hf]][0:64, c * 64 + 63:c * 64 + 64], scalar2=None, op0=ALU.mult),
                    reads=['stG', WK[EB[hf]]], writes=['stG'])
        if DBG_STOP <= 5:
            return
        for vt, (OB, G) in enumerate(((OB0, G0), (OB1, G1))):
            P.act(lambda e, OB=OB: e.activation(out=Wt[SQ][:, 0:TBk], in_=Wt[OB][:, 0:TBk], func=AF.Square),
                  reads=[WK[OB]], writes=[WK[SQ]])
            P.pe(lambda e: e.matmul(out=B[5][:, 0:TBk], lhsT=blk64, rhs=Wt[SQ][:, 0:TBk], start=True, stop=True),
                 reads=['c_f32', WK[SQ]], writes=['B5'])
            P.dve(lambda e: e.tensor_scalar(out=Wt[TMP][:, 0:TBk], in0=B[5][:, 0:TBk], scalar1=1e-6, scalar2=None,
                                            op0=ALU.add), reads=['B5'], writes=[WK[TMP]])
            P.act(lambda e: e.sqrt(out=Wt[TMP][:, 0:TBk], in_=Wt[TMP][:, 0:TBk]), reads=[WK[TMP]], writes=[WK[TMP]])
            P.dve(lambda e: e.reciprocal(out=Wt[TMP][:, 0:TBk], in_=Wt[TMP][:, 0:TBk]), reads=[WK[TMP]],
                  writes=[WK[TMP]])
            P.dve(lambda e, OB=OB, vt=vt: e.scalar_tensor_tensor(
                out=Wt[OB][:, 0:TBk], in0=Wt[OB][:, 0:TBk], scalar=cB[:, 69 + vt:70 + vt], in1=Wt[TMP][:, 0:TBk],
                op0=ALU.mult, op1=ALU.mult), reads=[WK[OB], WK[TMP], 'cB'], writes=[WK[OB]])
            P.act(lambda e, G=G: e.activation(out=Wt[G][:, 0:TBk], in_=Wt[G][:, 0:TBk], func=AF.Silu),
                  reads=[WK[G]], writes=[WK[G]])
            P.pool(lambda e, OB=OB, G=G: e.tensor_tensor(out=Wt[OB][:, 0:TBk], in0=Wt[OB][:, 0:TBk],
                                                         in1=Wt[G][:, 0:TBk], op=ALU.mult),
                   reads=[WK[OB], WK[G]], writes=[WK[OB]])
            strow(OB, 256 + vt * 128, c0)

    s5p_in = din("s5p", [L, 128, 24])
    s5B_in = din("s5B", [L, 128, 16, 128])
    s5C_in = din("s5C", [L, 128, 2, 8, 16])
    s5_glu_w = din("s5_glu_w", [L, 256, 256])
    s5p = sb("s5p_sb", [128, 24])
    s5t = sb("s5t", [128, 16, 8])
    s5C = sb("s5C_sb", [128, 2, 8, 16])
    s5Cp = sb("s5Cp", [128, 2, 8, 16])
    gluw = sb("gluw", [128, 2, 256])
    carry = sb("carry", [128, 2, 8])
    WBf = WB[:].bitcast(F32)
    sinT = [WBf[:, j * 512:(j + 1) * 512] for j in range(8)]
    cosT = [WBf[:, (8 + j) * 512:(9 + j) * 512] for j in range(8)]
    Bm = wst[0][:, 0:2048].rearrange("p (m c) -> p m c", m=16)
    CL = wst[1][:, 0:2048].rearrange("p (m c) -> p m c", m=16)
    iota_t = c_f32[:, 1024:1536]
    mcol = c_f32[:, 1536:1538]
    PI = float(np.pi)

    s5i = sb("s5i", [128, 512], I32)

    def sin_red(dst, x, n, rk, wk, add=0.0):
        q, m = Wt[22][:, 0:n], Wt[23][:, 0:n]
        qi = s5i[:, 0:n]
        kq, km = WK[22], WK[23]
        P.dve(lambda e: e.tensor_scalar(out=dst, in0=x, scalar1=add, scalar2=None, op0=ALU.add), reads=rk, writes=wk)
        P.dve(lambda e: e.tensor_scalar(out=q, in0=dst, scalar1=1.0 / (2 * PI), scalar2=None, op0=ALU.mult),
              reads=wk, writes=[kq])
        P.dve(lambda e: e.tensor_copy(out=qi, in_=q), reads=[kq], writes=['s5i'])
        P.dve(lambda e: e.tensor_copy(out=q, in_=qi), reads=['s5i'], writes=[kq])
        P.dve(lambda e: e.scalar_tensor_tensor(out=dst, in0=q, scalar=-2 * PI, in1=dst, op0=ALU.mult, op1=ALU.add),
              reads=[kq] + wk, writes=wk)
        P.dve(lambda e: e.tensor_scalar(out=m, in0=dst, scalar1=PI, scalar2=None, op0=ALU.is_gt), reads=wk, writes=[km])
        P.dve(lambda e: e.scalar_tensor_tensor(out=dst, in0=m, scalar=-2 * PI, in1=dst, op0=ALU.mult, op1=ALU.add),
              reads=[km] + wk, writes=wk)
        P.dve(lambda e: e.tensor_scalar(out=m, in0=dst, scalar1=-PI, scalar2=None, op0=ALU.is_lt), reads=wk, writes=[km])
        P.dve(lambda e: e.scalar_tensor_tensor(out=dst, in0=m, scalar=2 * PI, in1=dst, op0=ALU.mult, op1=ALU.add),
              reads=[km] + wk, writes=wk)
        P.act(lambda e: e.activation(out=dst, in_=dst, func=AF.Sin), reads=wk, writes=wk)

    def s5_setup(l):
        t = lambda i: s5t[:, i, :]
        dv = lambda fn: P.dve(fn, reads=['s5t', 's5p'], writes=['s5t'])
        P.dma(lambda e: e.dma_start(out=s5p[:], in_=s5p_in[l]), writes=['s5p'])
        P.dma(lambda e: e.dma_start(out=Bm, in_=s5B_in[l]), writes=['wst0'])
        P.dma(lambda e: e.dma_start(out=s5C[:], in_=s5C_in[l]), writes=['s5C'])
        P.dma(lambda e: e.dma_start(out=gluw[:], in_=s5_glu_w[l].rearrange("(k p) n -> p k n", p=128)),
              writes=['gluw'])
        P.pool(lambda e: e.memset(carry[:], 0.0), writes=['carry'])
        lr, li, ldt = s5p[:, 0:8], s5p[:, 8:16], s5p[:, 16:24]
        P.act(lambda e: e.activation(out=t(0), in_=ldt, func=AF.Exp), reads=['s5p'], writes=['s5t'])
        dv(lambda e: e.tensor_tensor(out=t(1), in0=lr, in1=t(0), op=ALU.mult))
        dv(lambda e: e.tensor_tensor(out=t(2), in0=li, in1=t(0), op=ALU.mult))
        P.act(lambda e: e.activation(out=t(3), in_=t(1), func=AF.Exp), reads=['s5t'], writes=['s5t'])
        sin_red(t(4), t(2), 8, ['s5t'], ['s5t'])
        sin_red(t(5), t(2), 8, ['s5t'], ['s5t'], add=0.5 * PI)
        dv(lambda e: e.tensor_tensor(out=t(6), in0=t(3), in1=t(5), op=ALU.mult))
        dv(lambda e: e.tensor_scalar(out=t(6), in0=t(6), scalar1=-1.0, scalar2=None, op0=ALU.add))
        dv(lambda e: e.tensor_tensor(out=t(7), in0=t(3), in1=t(4), op=ALU.mult))
        dv(lambda e: e.tensor_tensor(out=t(8), in0=lr, in1=lr, op=ALU.mult))
        dv(lambda e: e.tensor_tensor(out=t(9), in0=li, in1=li, op=ALU.mult))
        dv(lambda e: e.tensor_tensor(out=t(8), in0=t(8), in1=t(9), op=ALU.add))
        dv(lambda e: e.reciprocal(out=t(8), in_=t(8)))
        dv(lambda e: e.tensor_tensor(out=t(9), in0=t(6), in1=lr, op=ALU.mult))
        dv(lambda e: e.tensor_tensor(out=t(10), in0=t(7), in1=li, op=ALU.mult))
        dv(lambda e: e.tensor_tensor(out=t(9), in0=t(9), in1=t(10), op=ALU.add))
        dv(lambda e: e.tensor_tensor(out=t(9), in0=t(9), in1=t(8), op=ALU.mult))
        dv(lambda e: e.tensor_tensor(out=t(10), in0=t(7), in1=lr, op=ALU.mult))
        dv(lambda e: e.tensor_tensor(out=t(11), in0=t(6), in1=li, op=ALU.mult))
        dv(lambda e: e.tensor_tensor(out=t(10), in0=t(10), in1=t(11), op=ALU.subtract))
        dv(lambda e: e.tensor_tensor(out=t(10), in0=t(10), in1=t(8), op=ALU.mult))
        bc = lambda i: s5t[:, i, :].unsqueeze(2).to_broadcast([128, 8, 16])
        cr, ci = s5C[:, 0], s5C[:, 1]
        d2 = lambda fn: P.dve(fn, reads=['s5t', 's5C', 's5Cp'], writes=['s5Cp'])
        d2(lambda e: e.tensor_tensor(out=s5Cp[:, 0], in0=cr, in1=bc(9), op=ALU.mult))
        d2(lambda e: e.tensor_tensor(out=s5Cp[:, 1], in0=ci, in1=bc(10), op=ALU.mult))
        d2(lambda e: e.tensor_tensor(out=s5Cp[:, 0], in0=s5Cp[:, 0], in1=s5Cp[:, 1], op=ALU.subtract))
        d2(lambda e: e.tensor_tensor(out=s5Cp[:, 1], in0=cr, in1=bc(10), op=ALU.mult))
        P.dve(lambda e: e.tensor_tensor(out=s5C[:, 0], in0=ci, in1=bc(9), op=ALU.mult), reads=['s5t', 's5C'],
              writes=['s5C'])
        d2(lambda e: e.tensor_tensor(out=s5Cp[:, 1], in0=s5Cp[:, 1], in1=s5C[:, 0], op=ALU.add))
        P.pool(lambda e: e.memset(wst[1][:, 0:2048], 0.0), writes=['wst1'])
        P.dve(lambda e: e.tensor_scalar(out=s5t[:, 12, 0:2], in0=mcol, scalar1=-1.0, scalar2=None, op0=ALU.mult),
              reads=['c_f32'], writes=['s5t'])
        for j in range(8):
            for two in range(2):
                c0 = (j % 4) * 32 + two * 16
                P.dve(lambda e, j=j, two=two, c0=c0: e.tensor_scalar(
                    out=CL[:, j, c0:c0 + 16], in0=s5Cp[:, 0, j, :], scalar1=mcol[:, two:two + 1], scalar2=None,
                    op0=ALU.mult), reads=['s5Cp', 'c_f32'], writes=['wst1'])
                P.dve(lambda e, j=j, two=two, c0=c0: e.tensor_scalar(
                    out=CL[:, 8 + j, c0:c0 + 16], in0=s5Cp[:, 1, j, :], scalar1=s5t[:, 12, two:two + 1],
                    scalar2=None, op0=ALU.mult), reads=['s5Cp', 's5t'], writes=['wst1'])
            for tab, off in ((sinT[j], 0.0), (cosT[j], 0.5 * PI)):
                P.dve(lambda e, j=j, tab=tab: e.tensor_scalar(
                    out=tab, in0=iota_t, scalar1=s5t[:, 2, j:j + 1], scalar2=None, op0=ALU.mult),
                    reads=['s5t', 'c_f32'], writes=['WB'])
                sin_red(tab, tab, 512, ['WB'], ['WB'], add=off)

    def s5_block(l, blk):
        c0 = blk * TBk
        U = (0, 1)
        BR, BI, T1, T2, T3, T4, ZR, ZI, XR, XI, GT0, GT1 = range(2, 14)
        ldrow(U[0], 0, c0); ldrow(U[1], 128, c0)
        W = lambda i: Wt[i][:, 0:TBk]
        for j in range(8):
            ct = j // 4
            P.pe(lambda e, j=j, ct=ct: e.matmul(out=B[0][:, 0:TBk], lhsT=Bm[:, j, :], rhs=W(U[ct]), start=True,
                                                stop=True), reads=['wst0', WK[U[ct]]], writes=['B0'])
            P.pe(lambda e, j=j, ct=ct: e.matmul(out=B[1][:, 0:TBk], lhsT=Bm[:, 8 + j, :], rhs=W(U[ct]), start=True,
                                                stop=True), reads=['wst0', WK[U[ct]]], writes=['B1'])
            P.act(lambda e: e.copy(out=W(BR), in_=B[0][:, 0:TBk]), reads=['B0'], writes=[WK[BR]])
            P.act(lambda e: e.copy(out=W(BI), in_=B[1][:, 0:TBk]), reads=['B1'], writes=[WK[BI]])
            sn, cs_ = sinT[j][:, 0:TBk], cosT[j][:, 0:TBk]
            P.dve(lambda e, cs_=cs_: e.tensor_tensor(out=W(T1), in0=W(BR), in1=cs_, op=ALU.mult),
                  reads=[WK[BR], 'WB'], writes=[WK[T1]])
            P.pool(lambda e, sn=sn: e.tensor_tensor(out=W(T2), in0=W(BI), in1=sn, op=ALU.mult),
                   reads=[WK[BI], 'WB'], writes=[WK[T2]])
            P.dve(lambda e, cs_=cs_: e.tensor_tensor(out=W(T3), in0=W(BI), in1=cs_, op=ALU.mult),
                  reads=[WK[BI], 'WB'], writes=[WK[T3]])
            P.pool(lambda e, sn=sn: e.tensor_tensor(out=W(T4), in0=W(BR), in1=sn, op=ALU.mult),
                   reads=[WK[BR], 'WB'], writes=[WK[T4]])
            P.dve(lambda e: e.tensor_tensor(out=W(T1), in0=W(T1), in1=W(T2), op=ALU.add),
                  reads=[WK[T1], WK[T2]], writes=[WK[T1]])
            P.dve(lambda e: e.tensor_tensor(out=W(T3), in0=W(T3), in1=W(T4), op=ALU.subtract),
                  reads=[WK[T3], WK[T4]], writes=[WK[T3]])
            rb = s5t[:, 3, j:j + 1].to_broadcast([128, TBk])
            P.dve(lambda e, j=j, rb=rb: e.tensor_tensor_scan(out=W(ZR), data0=rb, data1=W(T1),
                                                             initial=carry[:, 0, j:j + 1], op0=ALU.mult, op1=ALU.add),
                  reads=['s5t', WK[T1], 'carry'], writes=[WK[ZR]])
            P.dve(lambda e, j=j, rb=rb: e.tensor_tensor_scan(out=W(ZI), data0=rb, data1=W(T3),
                                                             initial=carry[:, 1, j:j + 1], op0=ALU.mult, op1=ALU.add),
                  reads=['s5t', WK[T3], 'carry'], writes=[WK[ZI]])
            P.pool(lambda e, sn=sn: e.tensor_tensor(out=W(T2), in0=W(ZI), in1=sn, op=ALU.mult),
                   reads=[WK[ZI], 'WB'], writes=[WK[T2]])
            P.pool(lambda e, sn=sn: e.tensor_tensor(out=W(T4), in0=W(ZR), in1=sn, op=ALU.mult),
                   reads=[WK[ZR], 'WB'], writes=[WK[T4]])
            P.dve(lambda e, cs_=cs_: e.tensor_tensor(out=W(XR), in0=W(ZR), in1=cs_, op=ALU.mult),
                  reads=[WK[ZR], 'WB'], writes=[WK[XR]])
            P.dve(lambda e: e.tensor_tensor(out=W(XR), in0=W(XR), in1=W(T2), op=ALU.subtract),
                  reads=[WK[XR], WK[T2]], writes=[WK[XR]])
            P.dve(lambda e, cs_=cs_: e.tensor_tensor(out=W(XI), in0=W(ZI), in1=cs_, op=ALU.mult),
                  reads=[WK[ZI], 'WB'], writes=[WK[XI]])
            P.dve(lambda e: e.tensor_tensor(out=W(XI), in0=W(XI), in1=W(T4), op=ALU.add),
                  reads=[WK[XI], WK[T4]], writes=[WK[XI]])
            P.act(lambda e, j=j: e.copy(out=carry[:, 0, j:j + 1], in_=Wt[XR][:, TBk - 1:TBk]), reads=[WK[XR]],
                  writes=['carry'])
            P.act(lambda e, j=j: e.copy(out=carry[:, 1, j:j + 1], in_=Wt[XI][:, TBk - 1:TBk]), reads=[WK[XI]],
                  writes=['carry'])
            P.pe(lambda e, j=j, ct=ct: e.matmul(out=B[2 + ct][:, 0:TBk], lhsT=CL[:, j, :], rhs=W(XR),
                                                start=(j % 4 == 0), stop=False), reads=['wst1', WK[XR]],
                 writes=['B%d' % (2 + ct)])
            P.pe(lambda e, j=j, ct=ct: e.matmul(out=B[2 + ct][:, 0:TBk], lhsT=CL[:, 8 + j, :], rhs=W(XI),
                                                start=False, stop=(j % 4 == 3)), reads=['wst1', WK[XI]],
                 writes=['B%d' % (2 + ct)])
        for ct, GT in enumerate((GT0, GT1)):
            P.dve(lambda e, ct=ct, GT=GT: e.scalar_tensor_tensor(
                out=W(GT), in0=W(U[ct]), scalar=cB[:, 74 + ct:75 + ct], in1=B[2 + ct][:, 0:TBk], op0=ALU.mult,
                op1=ALU.add), reads=[WK[U[ct]], 'cB', 'B%d' % (2 + ct)], writes=[WK[GT]])
            P.act(lambda e, GT=GT: e.activation(out=W(GT), in_=W(GT), func=AF.Gelu), reads=[WK[GT]], writes=[WK[GT]])
        for ot, GT in enumerate((GT0, GT1)):
            for kt, GK in enumerate((GT0, GT1)):
                P.pe(lambda e, ot=ot, kt=kt, GK=GK: e.matmul(out=B[4][:, 0:TBk], lhsT=gluw[:, kt, ot * 128:(ot + 1) * 128],
                                                            rhs=W(GK), start=(kt == 0), stop=(kt == 1)),
                     reads=['gluw', WK[GK]], writes=['B4'])
            P.act(lambda e, ot=ot: e.activation(out=W(T1), in_=B[4][:, 0:TBk], func=AF.Sigmoid,
                                                bias=cB[:, 76 + ot:77 + ot]), reads=['B4', 'cB'], writes=[WK[T1]])
            P.pool(lambda e, GT=GT: e.tensor_tensor(out=W(T2), in0=W(GT), in1=W(T1), op=ALU.mult),
                   reads=[WK[GT], WK[T1]], writes=[WK[T2]])
            strow(T2, ot * 128, c0)

    colsR_in = din("colsR", [L, 128, 24])
    rw_w2 = din("rw_w2", [L, 32, 256]); rw_a2 = din("rw_a2", [L, 32, 256]); rw_g2 = din("rw_g2", [L, 64, 256])
    TBr = min(256, T)
    NCr = TBr // 64
    Ht = [Wt[i // 2][:, (i % 2) * 256:(i % 2) * 256 + 256] for i in range(48)]
    HK = ['Ht%d' % i for i in range(48)]
    ALIAS.extend(HK)
    cR = sb("cR", [128, 24])
    lora = sb("lora", [128, 256])
    stR = sb("stR", [128, 2, 64])
    btok, ktok2, vtok2 = [PX[:, 4096 + i * 512:4608 + i * 512].rearrange("p (b c) -> p b c", b=2) for i in range(3)]
    GMS = [PX[:, i * 512:(i + 1) * 512] for i in range(8)]
    GZ, GN, GAK, GRK, GRB, GP, GX, GU = GMS
    GKEY = ['gm%d' % i for i in range(8)]

    def gkey(ap):
        for i, g_ in enumerate(GMS):
            if g_ is ap:
                return GKEY[i]
        raise KeyError
    mask_su = c_f32[:, 512:768]
    mask_sl = c_f32[:, 1600:1856]
    I4 = c_f32[:, 1856:2112]
    EM05 = float(np.exp(-0.5))

    def rw_block(l, blk):
        fence()
        c0 = blk * TBr
        H = lambda i: Ht[i][:, 0:TBr]
        X = list(range(0, 7))
        XP = 7
        LW, AA, GG, KK = (8, 9), (10, 11), (12, 13), (14, 15)
        LC, WI, WN, WE = (16, 17), (18, 19), (20, 21), (22, 23)
        RT_, AT_, BT_, KT_ = (24, 25), (26, 27), (28, 29), (30, 31)
        YT, BON, T1, T2 = (32, 33), (34, 35), 36, 37
        R_, K_, V_ = (0, 1), (2, 3), (4, 5)
        Zt = 6
        if blk == 0:
            P.pool(lambda e: e.memset(stR[:], 0.0), writes=['stR'])
        for i in range(7):
            r0 = 1040 + i * 128
            P.dma(lambda e, i=i, r0=r0: e.dma_start(out=H(X[i]), in_=pT[r0:r0 + 128, c0:c0 + TBr]), reads=['pT'],
                  writes=[HK[X[i]]])
            if blk == 0:
                P.pool(lambda e: e.memset(Ht[XP][:, 0:1], 0.0), writes=[HK[XP]])
                P.dma(lambda e, r0=r0: e.dma_start(out=Ht[XP][:, 1:TBr], in_=pT[r0:r0 + 128, 0:TBr - 1]),
                      reads=['pT'], writes=[HK[XP]])
            else:
                P.dma(lambda e, r0=r0: e.dma_start(out=H(XP), in_=pT[r0:r0 + 128, c0 - 1:c0 - 1 + TBr]),
                      reads=['pT'], writes=[HK[XP]])
            P.dve(lambda e, i=i: e.tensor_tensor(out=H(XP), in0=H(XP), in1=H(X[i]), op=ALU.subtract),
                  reads=[HK[XP], HK[X[i]]], writes=[HK[XP]])
            P.dve(lambda e, i=i: e.scalar_tensor_tensor(out=H(X[i]), in0=H(XP), scalar=cR[:, i:i + 1], in1=H(X[i]),
                                                        op0=ALU.mult, op1=ALU.add),
                  reads=[HK[XP], HK[X[i]], 'cR'], writes=[HK[X[i]]])
        P.act(lambda e: e.activation(out=Ht[Zt][0:32, 0:TBr], in_=Ht[Zt][0:32, 0:TBr], func=AF.Tanh),
              reads=[HK[Zt]], writes=[HK[Zt]])
        P.act(lambda e: e.activation(out=Ht[Zt][64:128, 0:TBr], in_=Ht[Zt][64:128, 0:TBr], func=AF.Sigmoid),
              reads=[HK[Zt]], writes=[HK[Zt]])
        for ct in range(2):
            cs2 = slice(ct * 128, (ct + 1) * 128)
            P.pe(lambda e, cs2=cs2: e.matmul(out=B[0][:, 0:TBr], lhsT=lora[0:32, cs2], rhs=Ht[Zt][0:32, 0:TBr],
                                             start=True, stop=True), reads=['lora', HK[Zt]], writes=['B0'])
            P.act(lambda e, ct=ct: e.activation(out=H(LW[ct]), in_=B[0][:, 0:TBr], func=AF.Sigmoid,
                                                bias=cR[:, 7 + ct:8 + ct]), reads=['B0', 'cR'], writes=[HK[LW[ct]]])
            P.dve(lambda e, ct=ct: e.tensor_scalar(out=H(LW[ct]), in0=H(LW[ct]), scalar1=-EM05, scalar2=None,
                                                   op0=ALU.mult), reads=[HK[LW[ct]]], writes=[HK[LW[ct]]])
            P.pe(lambda e, cs2=cs2: e.matmul(out=B[1][:, 0:TBr], lhsT=lora[32:64, cs2], rhs=Ht[Zt][32:64, 0:TBr],
                                             start=True, stop=True), reads=['lora', HK[Zt]], writes=['B1'])
            P.act(lambda e, ct=ct: e.activation(out=H(AA[ct]), in_=B[1][:, 0:TBr], func=AF.Sigmoid,
                                                bias=cR[:, 9 + ct:10 + ct]), reads=['B1', 'cR'], writes=[HK[AA[ct]]])
            P.pe(lambda e, cs2=cs2: e.matmul(out=B[2][:, 0:TBr], lhsT=lora[64:128, cs2], rhs=Ht[Zt][64:128, 0:TBr],
                                             start=True, stop=True), reads=['lora', HK[Zt]], writes=['B2'])
            P.act(lambda e, ct=ct: e.copy(out=H(GG[ct]), in_=B[2][:, 0:TBr]), reads=['B2'], writes=[HK[GG[ct]]])
            P.dve(lambda e, ct=ct: e.tensor_scalar(out=H(KK[ct]), in0=H(K_[ct]), scalar1=cR[:, 11 + ct:12 + ct],
                                                   scalar2=None, op0=ALU.mult), reads=[HK[K_[ct]], 'cR'],
                  writes=[HK[KK[ct]]])
            P.act(lambda e, ct=ct: e.activation(out=H(T1), in_=H(KK[ct]), func=AF.Square), reads=[HK[KK[ct]]],
                  writes=[HK[T1]])
            P.pe(lambda e: e.matmul(out=B[3][:, 0:TBr], lhsT=blk64, rhs=H(T1), start=True, stop=True),
                 reads=['c_f32', HK[T1]], writes=['B3'])
            P.act(lambda e: e.activation(out=H(T1), in_=B[3][:, 0:TBr], func=AF.Sqrt, scale=64.0), reads=['B3'],
                  writes=[HK[T1]])
            P.dve(lambda e: e.tensor_scalar(out=H(T1), in0=H(T1), scalar1=1e-12, scalar2=None, op0=ALU.max),
                  reads=[HK[T1]], writes=[HK[T1]])
            P.dve(lambda e: e.reciprocal(out=H(T1), in_=H(T1)), reads=[HK[T1]], writes=[HK[T1]])
            P.dve(lambda e, ct=ct: e.tensor_tensor(out=H(KK[ct]), in0=H(KK[ct]), in1=H(T1), op=ALU.mult),
                  reads=[HK[KK[ct]], HK[T1]], writes=[HK[KK[ct]]])
            P.dve(lambda e, ct=ct: e.tensor_scalar(out=H(T1), in0=H(AA[ct]), scalar1=cR[:, 13 + ct:14 + ct],
                                                   scalar2=cR[:, 21 + ct:22 + ct], op0=ALU.mult, op1=ALU.add),
                  reads=[HK[AA[ct]], 'cR'], writes=[HK[T1]])
            P.dve(lambda e, ct=ct: e.tensor_tensor(out=H(K_[ct]), in0=H(K_[ct]), in1=H(T1), op=ALU.mult),
                  reads=[HK[K_[ct]], HK[T1]], writes=[HK[K_[ct]]])
            P.dve(lambda e, ct=ct: e.scalar_tensor_tensor(out=H(T1), in0=H(R_[ct]), scalar=cR[:, 15 + ct:16 + ct],
                                                          in1=H(K_[ct]), op0=ALU.mult, op1=ALU.mult),
                  reads=[HK[R_[ct]], HK[K_[ct]], 'cR'], writes=[HK[T1]])
            P.pe(lambda e: e.matmul(out=B[3][:, 0:TBr], lhsT=blk64, rhs=H(T1), start=True, stop=True),
                 reads=['c_f32', HK[T1]], writes=['B3'])
            P.dve(lambda e, ct=ct: e.scalar_tensor_tensor(out=H(BON[ct]), in0=B[3][:, 0:TBr], scalar=64.0,
                                                          in1=H(V_[ct]), op0=ALU.mult, op1=ALU.mult),
                  reads=['B3', HK[V_[ct]]], writes=[HK[BON[ct]]])
            for c in range(NCr):
                cs = slice(c * 64, (c + 1) * 64)
                P.dve(lambda e, ct=ct, cs=cs: e.tensor_tensor_scan(out=Ht[LC[ct]][:, cs], data0=ones64[:],
                                                                   data1=Ht[LW[ct]][:, cs], initial=0.0,
                                                                   op0=ALU.mult, op1=ALU.add),
                      reads=['ones64', HK[LW[ct]]], writes=[HK[LC[ct]]])
            P.act(lambda e, ct=ct: e.activation(out=H(WI[ct]), in_=H(LC[ct]), func=AF.Exp), reads=[HK[LC[ct]]],
                  writes=[HK[WI[ct]]])
            P.act(lambda e, ct=ct: e.activation(out=H(WN[ct]), in_=H(LC[ct]), func=AF.Exp, scale=-1.0),
                  reads=[HK[LC[ct]]], writes=[HK[WN[ct]]])
            P.dve(lambda e, ct=ct: e.tensor_tensor(out=H(WE[ct]), in0=H(LC[ct]), in1=H(LW[ct]), op=ALU.subtract),
                  reads=[HK[LC[ct]], HK[LW[ct]]], writes=[HK[WE[ct]]])
            P.act(lambda e, ct=ct: e.activation(out=H(WE[ct]), in_=H(WE[ct]), func=AF.Exp), reads=[HK[WE[ct]]],
                  writes=[HK[WE[ct]]])
            P.dve(lambda e, ct=ct: e.tensor_tensor(out=H(RT_[ct]), in0=H(R_[ct]), in1=H(WI[ct]), op=ALU.mult),
                  reads=[HK[R_[ct]], HK[WI[ct]]], writes=[HK[RT_[ct]]])
            P.dve(lambda e, ct=ct: e.scalar_tensor_tensor(out=H(AT_[ct]), in0=H(KK[ct]), scalar=-1.0, in1=H(WE[ct]),
                                                          op0=ALU.mult, op1=ALU.mult),
                  reads=[HK[KK[ct]], HK[WE[ct]]], writes=[HK[AT_[ct]]])
            P.pool(lambda e, ct=ct: e.tensor_tensor(out=H(BT_[ct]), in0=H(KK[ct]), in1=H(AA[ct]), op=ALU.mult),
                   reads=[HK[KK[ct]], HK[AA[ct]]], writes=[HK[BT_[ct]]])
            P.pool(lambda e, ct=ct: e.tensor_tensor(out=H(BT_[ct]), in0=H(BT_[ct]), in1=H(WN[ct]), op=ALU.mult),
                   reads=[HK[BT_[ct]], HK[WN[ct]]], writes=[HK[BT_[ct]]])
            P.pool(lambda e, ct=ct: e.tensor_tensor(out=H(KT_[ct]), in0=H(K_[ct]), in1=H(WN[ct]), op=ALU.mult),
                   reads=[HK[K_[ct]], HK[WN[ct]]], writes=[HK[KT_[ct]]])
        for b2 in range(TBr // 128):
            sl = slice(b2 * 128, (b2 + 1) * 128)
            for (srcs, dst, dk, bk) in ((BT_, btok, 'btok', 0), (KT_, ktok2, 'ktok2', 1), (V_, vtok2, 'vtok2', 2)):
                for ct in range(2):
                    P.pe(lambda e, srcs=srcs, ct=ct, sl=sl, bk=bk: e.transpose(
                        out=B[bk][:, ct * 128:(ct + 1) * 128], in_=Ht[srcs[ct]][:, sl], identity=identf),
                        reads=[HK[srcs[ct]], 'c_f32'], writes=['B%d' % bk])
                P.act(lambda e, dst=dst, b2=b2, bk=bk: e.copy(out=dst[:, b2, :], in_=B[bk][:, 0:256]),
                      reads=['B%d' % bk], writes=[dk])
        def gram(bank, lt, rt, dst, dkey, mask):
            for c in range(NCr):
                b2, pb = c // 2, (c % 2) * 64
                cs = slice(c * 64, (c + 1) * 64)
                for hd in range(4):
                    ct, hs = hd // 2, slice((hd % 2) * 64, (hd % 2) * 64 + 64)
                    col = b2 * 256 + hd * 64
                    P.pe(lambda e, ct=ct, hs=hs, cs=cs, pb=pb, col=col: e.matmul(
                        out=B[bank][pb:pb + 64, col:col + 64], lhsT=Ht[lt[ct]][hs, cs], rhs=Ht[rt[ct]][hs, cs],
                        start=True, stop=True), reads=[HK[lt[ct]], HK[rt[ct]]], writes=['B%d' % bank])
            nb = TBr // 128
            P.dve(lambda e: e.tensor_tensor(
                out=dst[:, 0:nb * 256].rearrange("p (b c) -> p b c", b=nb),
                in0=B[bank][:, 0:nb * 256].rearrange("p (b c) -> p b c", b=nb),
                in1=mask.unsqueeze(1).to_broadcast([128, nb, 256]), op=ALU.mult),
                reads=['B%d' % bank, 'c_f32'], writes=[dkey])
        gram(0, BT_, AT_, GZ, 'gm0', mask_su)
        gram(1, AT_, BT_, GN, 'gm1', mask_sl)
        gram(2, KT_, AT_, GAK, 'gm2', mask_su)
        gram(3, KT_, RT_, GRK, 'gm3', mask_incl)
        gram(4, BT_, RT_, GRB, 'gm4', mask_incl)
        nb = TBr // 128
        NW = nb * 256
        v3 = lambda ap: ap[:, 0:NW].rearrange("p (b c) -> p b c", b=nb)
        P.dve(lambda e: e.tensor_tensor(out=v3(GP), in0=v3(GZ), in1=I4.unsqueeze(1).to_broadcast([128, nb, 256]),
                                        op=ALU.add), reads=['gm0', 'c_f32'], writes=['gm5'])
        Zc, Zt_ = GZ, GN
        Za, Zb = GX, GU
        for j in range(1, 6):
            def allblk(fn):
                for c in range(NCr):
                    b2, pb = c // 2, (c % 2) * 64
                    for hd in range(4):
                        col = b2 * 256 + hd * 64
                        fn(pb, col)
            allblk(lambda pb, col, Zc=Zc, Zt_=Zt_: P.pe(lambda e: e.matmul(
                out=B[0][pb:pb + 64, col:col + 64], lhsT=Zt_[pb:pb + 64, col:col + 64], rhs=Zc[pb:pb + 64, col:col + 64],
                start=True, stop=True), reads=[gkey(Zc),
                                               gkey(Zt_)], writes=['B0']))
            allblk(lambda pb, col, Zc=Zc, Zt_=Zt_: P.pe(lambda e: e.matmul(
                out=B[1][pb:pb + 64, col:col + 64], lhsT=Zc[pb:pb + 64, col:col + 64], rhs=Zt_[pb:pb + 64, col:col + 64],
                start=True, stop=True), reads=[gkey(Zc),
                                               gkey(Zt_)], writes=['B1']))
            nZ, nZt = (Za, Zb) if j % 2 == 1 else (GZ, GN)
            kZ = gkey(nZ)
            kZt = gkey(nZt)
            P.act(lambda e, nZ=nZ: e.copy(out=nZ[:, 0:NW], in_=B[0][:, 0:NW]), reads=['B0'], writes=[kZ])
            P.act(lambda e, nZt=nZt: e.copy(out=nZt[:, 0:NW], in_=B[1][:, 0:NW]), reads=['B1'], writes=[kZt])
            Zc, Zt_ = nZ, nZt
            allblk(lambda pb, col, Zt_=Zt_, kZt=kZt: P.pe(lambda e: e.matmul(
                out=B[2][pb:pb + 64, col:col + 64], lhsT=Zt_[pb:pb + 64, col:col + 64], rhs=GP[pb:pb + 64, col:col + 64],
                start=True, stop=True), reads=[kZt, 'gm5'], writes=['B2']))
            P.dve(lambda e: e.tensor_tensor(out=GP[:, 0:NW], in0=GP[:, 0:NW], in1=B[2][:, 0:NW], op=ALU.add),
                  reads=['gm5', 'B2'], writes=['gm5'])
        for c in range(NCr):
            b2, pb = c // 2, (c % 2) * 64
            cs = slice(c * 64, (c + 1) * 64)
            ps_ = slice(pb, pb + 64)
            for hd in range(4):
                ct, hs = hd // 2, slice((hd % 2) * 64, (hd % 2) * 64 + 64)
                col = b2 * 256 + hd * 64
                P.pe(lambda e, ps_=ps_, ct=ct, hs=hs, cs=cs, hd=hd: e.matmul(
                    out=B[3][ps_, hd * 64:(hd + 1) * 64], lhsT=Ht[AT_[ct]][hs, cs], rhs=stR[hs, ct, :],
                    start=True, stop=False), reads=[HK[AT_[ct]], 'stR'], writes=['B3'])
                P.pe(lambda e, ps_=ps_, hd=hd, col=col, b2=b2: e.matmul(
                    out=B[3][ps_, hd * 64:(hd + 1) * 64], lhsT=GAK[ps_, col:col + 64],
                    rhs=vtok2[ps_, b2, hd * 64:(hd + 1) * 64], start=False, stop=True),
                    reads=['gm2', 'vtok2'], writes=['B3'])
            P.act(lambda e, ps_=ps_: e.copy(out=GX[ps_, 0:256], in_=B[3][ps_, 0:256]), reads=['B3'], writes=['gm6'])
            for hd in range(4):
                col = b2 * 256 + hd * 64
                P.pe(lambda e, ps_=ps_, hd=hd, col=col: e.matmul(
                    out=B[4][ps_, hd * 64:(hd + 1) * 64], lhsT=GP[ps_, col:col + 64],
                    rhs=GX[ps_, hd * 64:(hd + 1) * 64], start=True, stop=True), reads=['gm5', 'gm6'], writes=['B4'])
            P.act(lambda e, ps_=ps_: e.copy(out=GU[ps_, 0:256], in_=B[4][ps_, 0:256]), reads=['B4'], writes=['gm7'])
            for hd in range(4):
                ct, hs = hd // 2, slice((hd % 2) * 64, (hd % 2) * 64 + 64)
                col = b2 * 256 + hd * 64
                hcol = slice(hd * 64, (hd + 1) * 64)
                P.pe(lambda e, ps_=ps_, ct=ct, hs=hs, cs=cs: e.matmul(
                    out=B[5][hs, ct * 64:(ct + 1) * 64], lhsT=stR[hs, ct, :], rhs=Ht[RT_[ct]][hs, cs],
                    start=True, stop=False), reads=['stR', HK[RT_[ct]]], writes=['B5'])
                P.pe(lambda e, ps_=ps_, ct=ct, hs=hs, col=col, hcol=hcol: e.matmul(
                    out=B[5][hs, ct * 64:(ct + 1) * 64], lhsT=GU[ps_, hcol], rhs=GRB[ps_, col:col + 64],
                    start=False, stop=False), reads=['gm7', 'gm4'], writes=['B5'])
                P.pe(lambda e, ps_=ps_, ct=ct, hs=hs, col=col, hcol=hcol, b2=b2: e.matmul(
                    out=B[5][hs, ct * 64:(ct + 1) * 64], lhsT=vtok2[ps_, b2, hcol], rhs=GRK[ps_, col:col + 64],
                    start=False, stop=True), reads=['vtok2', 'gm3'], writes=['B5'])
                P.pe(lambda e, ps_=ps_, ct=ct, hs=hs, hcol=hcol, b2=b2: e.matmul(
                    out=B[2][hs, ct * 64:(ct + 1) * 64], lhsT=btok[ps_, b2, hcol], rhs=GU[ps_, hcol],
                    start=True, stop=False), reads=['btok', 'gm7'], writes=['B2'])
                P.pe(lambda e, ps_=ps_, ct=ct, hs=hs, hcol=hcol, b2=b2: e.matmul(
                    out=B[2][hs, ct * 64:(ct + 1) * 64], lhsT=ktok2[ps_, b2, hcol], rhs=vtok2[ps_, b2, hcol],
                    start=False, stop=True), reads=['ktok2', 'vtok2'], writes=['B2'])
            for ct in range(2):
                P.act(lambda e, ps_=ps_, ct=ct, cs=cs: e.copy(out=Ht[YT[ct]][:, cs], in_=B[5][:, ct * 64:(ct + 1) * 64]),
                      reads=['B5'], writes=[HK[YT[ct]]])
                P.dve(lambda e, ps_=ps_, ct=ct: e.tensor_tensor(out=stR[:, ct, :], in0=stR[:, ct, :],
                                                       in1=B[2][:, ct * 64:(ct + 1) * 64], op=ALU.add),
                      reads=['stR', 'B2'], writes=['stR'])
                P.dve(lambda e, ps_=ps_, ct=ct, c=c: e.tensor_scalar(out=stR[:, ct, :], in0=stR[:, ct, :],
                                                            scalar1=Ht[WI[ct]][:, c * 64 + 63:c * 64 + 64],
                                                            scalar2=None, op0=ALU.mult),
                      reads=['stR', HK[WI[ct]]], writes=['stR'])
        if blk == 0 and DBG_VAR == 7:
            for nm, idx in (('lw', LW[0]), ('aa', AA[0]), ('kk', KK[0]), ('kmod', K_[0]), ('gg', GG[0]), ('bon', BON[0]),
                            ('y', YT[0]), ('rt', RT_[0]), ('at', AT_[0]), ('bt', BT_[0]), ('kt', KT_[0]), ('r', R_[0]),
                            ('v', V_[0])):
                dbgdump(nm, H(idx), HK[idx], [128, TBr])
            for nm, gi in (('gz', 0), ('gn', 1), ('gak', 2), ('grk', 3), ('grb', 4), ('gp', 5), ('gx', 6), ('gu', 7)):
                dbgdump(nm, GMS[gi], GKEY[gi], [128, 512])
            dbgdump('vtok', vtok2[:, 0, :], 'vtok2', [128, 256])
            dbgdump('btok', btok[:, 0, :], 'btok', [128, 256])
        for ct in range(2):
            P.pe(lambda e, ct=ct: e.matmul(out=B[0][:, 0:TBr], lhsT=blk64, rhs=H(YT[ct]), start=True, stop=True),
                 reads=['c_f32', HK[YT[ct]]], writes=['B0'])
            P.act(lambda e, ct=ct: e.activation(out=H(T1), in_=H(YT[ct]), func=AF.Square), reads=[HK[YT[ct]]],
                  writes=[HK[T1]])
            P.pe(lambda e: e.matmul(out=B[1][:, 0:TBr], lhsT=blk64, rhs=H(T1), start=True, stop=True),
                 reads=['c_f32', HK[T1]], writes=['B1'])
            P.act(lambda e: e.copy(out=H(T2), in_=B[0][:, 0:TBr]), reads=['B0'], writes=[HK[T2]])
            P.dve(lambda e: e.tensor_tensor(out=H(T1), in0=H(T2), in1=H(T2), op=ALU.mult), reads=[HK[T2]],
                  writes=[HK[T1]])
            P.dve(lambda e: e.tensor_tensor(out=H(T1), in0=B[1][:, 0:TBr], in1=H(T1), op=ALU.subtract),
                  reads=['B1', HK[T1]], writes=[HK[T1]])
            P.dve(lambda e: e.tensor_scalar(out=H(T1), in0=H(T1), scalar1=64e-5, scalar2=None, op0=ALU.add),
                  reads=[HK[T1]], writes=[HK[T1]])
            P.act(lambda e: e.sqrt(out=H(T1), in_=H(T1)), reads=[HK[T1]], writes=[HK[T1]])
            P.dve(lambda e: e.reciprocal(out=H(T1), in_=H(T1)), reads=[HK[T1]], writes=[HK[T1]])
            P.dve(lambda e, ct=ct: e.tensor_tensor(out=H(YT[ct]), in0=H(YT[ct]), in1=H(T2), op=ALU.subtract),
                  reads=[HK[YT[ct]], HK[T2]], writes=[HK[YT[ct]]])
            P.dve(lambda e, ct=ct: e.tensor_tensor(out=H(YT[ct]), in0=H(YT[ct]), in1=H(T1), op=ALU.mult),
                  reads=[HK[YT[ct]], HK[T1]], writes=[HK[YT[ct]]])
            P.act(lambda e, ct=ct: e.activation(out=H(YT[ct]), in_=H(YT[ct]), func=AF.Identity,
                                                scale=cR[:, 17 + ct:18 + ct], bias=cR[:, 19 + ct:20 + ct]),
                  reads=[HK[YT[ct]], 'cR'], writes=[HK[YT[ct]]])
            P.dve(lambda e, ct=ct: e.tensor_tensor(out=H(YT[ct]), in0=H(YT[ct]), in1=H(BON[ct]), op=ALU.add),
                  reads=[HK[YT[ct]], HK[BON[ct]]], writes=[HK[YT[ct]]])
            P.dve(lambda e, ct=ct: e.tensor_tensor(out=H(YT[ct]), in0=H(YT[ct]), in1=H(GG[ct]), op=ALU.mult),
                  reads=[HK[YT[ct]], HK[GG[ct]]], writes=[HK[YT[ct]]])
            P.dma(lambda e, ct=ct: e.dma_start(out=mixT[512 + ct * 128:512 + (ct + 1) * 128, c0:c0 + TBr],
                                               in_=H(YT[ct])), reads=[HK[YT[ct]]], writes=['mixT'])

    def rw_setup(l):
        P.dma(lambda e: e.dma_start(out=cR[:], in_=colsR_in[l]), writes=['cR'])
        P.dma(lambda e: e.dma_start(out=lora[0:32, :], in_=rw_w2[l]), writes=['lora'])
        P.dma(lambda e: e.dma_start(out=lora[32:64, :], in_=rw_a2[l]), writes=['lora'])
        P.dma(lambda e: e.dma_start(out=lora[64:128, :], in_=rw_g2[l]), writes=['lora'])
        for ct in range(2):
            P.dve(lambda e, ct=ct: e.tensor_scalar(out=cR[:, 21 + ct:22 + ct], in0=cR[:, 13 + ct:14 + ct],
                                                   scalar1=-1.0, scalar2=1.0, op0=ALU.mult, op1=ALU.add),
                  reads=['cR'], writes=['cR'])

    def phase_B(l, which=B_WHICH):
        fence()
        P.dma(lambda e: e.dma_start(out=cB[:], in_=colsB[l]), writes=['cB'])
        P.dma(lambda e: e.dma_start(out=wup[:], in_=gla_w_up[l]), writes=['wup'])
        P.dve(lambda e: e.tensor_scalar(out=cB[:, 71:72], in0=cB[:, 68:69], scalar1=-1.0, scalar2=None, op0=ALU.mult),
              reads=['cB'], writes=['cB'])
        P.dve(lambda e: e.tensor_scalar(out=cB[:, 73:74], in0=cB[:, 72:73], scalar1=-1.0, scalar2=None, op0=ALU.mult),
              reads=['cB'], writes=['cB'])
        if 's5' in which:
            s5_setup(l)
        if 'rw' in which:
            rw_setup(l)
            for blk in range(T // TBr):
                rw_block(l, blk)
        for blk in range(T // TBk):
            fence()
            if 's5' in which:
                s5_block(l, blk)
            if 'conv' in which:
                conv_block(l, blk)
            if 'gla' in which:
                gla_block(l, blk)

    for l in range(L):
        src = x if l == 0 else out
        if 'A' in phases:
            phase_A(l, src)
        if 'B' in phases:
            phase_B(l)
        if 'O' in phases:
            phase_outproj(l, src)
        if 'X' in phases:
            phase_xattn(l)
        if 'M' in phases:
            phase_moe(l)
    if 'F' in phases:
        phase_final()

    P.emit()
    return nc, es


def make_consts():
    c = np.zeros((128, 2112), np.float32)
    c[:, 1024:1536] = np.arange(1, 513, dtype=np.float32)[None, :]
    c[0:64, 1536] = 1.0
    c[64:128, 1537] = 1.0
    c[:, 0:128] = np.eye(128, dtype=np.float32)
    p = np.arange(128)[:, None] % 64
    t = np.arange(64)[None, :]
    incl = (p <= t).astype(np.float32)
    strict = (p < t).astype(np.float32)
    for h in range(4):
        c[:, 128 + h * 64:128 + (h + 1) * 64] = incl
        c[:, 512 + h * 64:512 + (h + 1) * 64] = strict
    for h in range(4):
        c[:, 1600 + h * 64:1600 + (h + 1) * 64] = (p > t).astype(np.float32)
        c[:, 1856 + h * 64:1856 + (h + 1) * 64] = (p == t).astype(np.float32)
    q = np.arange(128)
    c[:, 384:512] = (q[:, None] // 64 == q[None, :] // 64).astype(np.float32) / 64.0
    return c


ALL_PHASES = ('A', 'B', 'O', 'X', 'M', 'F')
B_WHICH = ('s5', 'conv', 'gla', 'rw')
DBG_STOP = 99
DBG_VAR = 0


def make_colsB(inputs, L):
    g = lambda k: np.asarray(inputs[k], dtype=np.float32)
    c = np.zeros((L, 128, 80), np.float32)
    cw = g("conv_w")
    for ct in range(2):
        c[:, :, ct * 31:(ct + 1) * 31] = cw[:, :, ct * 128:(ct + 1) * 128].transpose(0, 2, 1)
        c[:, :, 62 + ct] = g("conv_b")[:, ct * 128:(ct + 1) * 128]
        c[:, :, 64 + ct] = g("conv_ln_g")[:, ct * 128:(ct + 1) * 128]
        c[:, :, 66 + ct] = g("conv_ln_b")[:, ct * 128:(ct + 1) * 128]
        c[:, :, 69 + ct] = g("gla_norm_g")[:, ct * 128:(ct + 1) * 128]
    for ct in range(2):
        c[:, :, 74 + ct] = g("s5_d")[:, ct * 128:(ct + 1) * 128]
        c[:, :, 76 + ct] = g("s5_glu_b")[:, ct * 128:(ct + 1) * 128]
    c[:, 0:64, 68] = g("gla_b_up")[:, 0:64]
    c[:, 0:64, 72] = g("gla_b_up")[:, 64:128]
    return c


def make_colsR(inputs, L):
    g = lambda k: np.asarray(inputs[k], dtype=np.float32)
    c = np.zeros((L, 128, 24), np.float32)
    c[:, :, 0:7] = g("rw_mu").reshape(L, 7, 128).transpose(0, 2, 1)
    for i, k in enumerate(("rw_w0", "rw_a0", "rw_k_k", "rw_k_a", "rw_r_k", "rw_ln_g", "rw_ln_b")):
        c[:, :, 7 + 2 * i:9 + 2 * i] = g(k).reshape(L, 2, 128).transpose(0, 2, 1)
    return c


def make_s5(inputs, L):
    g = lambda k: np.asarray(inputs[k], dtype=np.float32)
    pl = lambda a: a.reshape(L, 8, 2, 64).transpose(0, 2, 3, 1).reshape(L, 128, 8)
    ldt = np.broadcast_to(g("s5_log_dt")[:, :, None], (L, 16, 64))
    s5p = np.concatenate([pl(g("s5_lam_re")), pl(g("s5_lam_im")), pl(ldt)], axis=-1)
    Bm = np.zeros((L, 128, 16, 128), np.float32)
    for ri, key in enumerate(("s5_b_re", "s5_b_im")):
        b = g(key)
        for j in range(8):
            for two in range(2):
                r0 = (j % 4) * 32 + two * 16
                Bm[:, r0:r0 + 16, ri * 8 + j, two * 64:(two + 1) * 64] = b[:, 2 * j + two].transpose(0, 2, 1)
    C = np.zeros((L, 128, 2, 8, 16), np.float32)
    for ri, key in enumerate(("s5_c_re", "s5_c_im")):
        c = g(key)
        C[:, :, ri] = c.reshape(L, 8, 2, 16, 64).transpose(0, 2, 4, 1, 3).reshape(L, 128, 8, 16)
    return {"s5p": np.ascontiguousarray(s5p), "s5B": Bm, "s5C": C}


def prep_inputs(inputs, b, L):
    f = lambda k: np.ascontiguousarray(np.asarray(inputs[k], dtype=np.float32))
    m = {
        "x": np.ascontiguousarray(np.asarray(inputs["x"], np.float32)[b]),
        "mem": np.ascontiguousarray(np.asarray(inputs["mem"], np.float32)[b]),
        "consts": make_consts(),
        "norm_mix_g": f("norm_mix_g"), "w_in": f("w_in"), "w_out": f("w_out"),
        "beta_c": np.ascontiguousarray(f("mix_beta").reshape(L, KT, 128).transpose(0, 2, 1)),
        "norm_xattn_g": f("norm_xattn_g"), "norm_mem_g": f("norm_mem_g"),
        "xa_wq": f("xa_wq"), "xa_wk": f("xa_wk"), "xa_wv": f("xa_wv"), "xa_wo": f("xa_wo"),
        "norm_ffn_g": f("norm_ffn_g"),
        "moe_rw": np.ascontiguousarray(np.concatenate([f("moe_group_w"), f("moe_expert_w")], axis=-1)),
        "moe_rb": np.ascontiguousarray(np.concatenate([f("moe_group_b"), f("moe_expert_b")], axis=-1)),
        "moe_w_gate": f("moe_w_gate"), "moe_w_up": f("moe_w_up"), "moe_w_down": f("moe_w_down"),
        "norm_final_g": f("norm_final_g").reshape(1, D),
        "colsB": make_colsB(inputs, L), "gla_w_up": f("gla_w_up"),
        **make_s5(inputs, L), "s5_glu_w": f("s5_glu_w"),
        "colsR": make_colsR(inputs, L), "rw_w2": f("rw_w2"), "rw_a2": f("rw_a2"), "rw_g2": f("rw_g2"),
    }
    return m


def kernel(**inputs):
    x = np.asarray(inputs["x"])
    Bsz, T, _ = x.shape
    L = np.asarray(inputs["w_in"]).shape[0]
    nc, es = build(T, L, dbg=False, phases=ALL_PHASES)
    n_cores = 8
    maps = [prep_inputs(inputs, c % Bsz, L) for c in range(Bsz)]
    in_maps = [maps[c % Bsz] for c in range(n_cores)]
    res = run_bass_kernel_spmd(nc, in_maps, core_ids=list(range(n_cores)))
    outs = [np.asarray(res.results[c]["out"], dtype=np.float32) for c in range(Bsz)]
    return np.stack(outs, axis=0)
```

```python
import numpy as np
from contextlib import ExitStack
import concourse.bass as bass
import concourse.mybir as mybir
from concourse.bass_utils import run_bass_kernel_spmd

F32 = mybir.dt.float32
BF16 = mybir.dt.bfloat16
I32 = mybir.dt.int32
ALU = mybir.AluOpType
AF = mybir.ActivationFunctionType
AX = mybir.AxisListType

D = 1024
KT = D // 128
IN_COLS = 2448
NDMA = 16


class Prog:
    ENG = ['pe', 'act', 'dve', 'pool', 'sp']

    def __init__(self, nc, es):
        self.nc = nc
        self.es = es
        self.ops = []
        self.last_w = {}
        self.readers = {}
        self.eng_cnt = {e: 0 for e in self.ENG}
        self.dma_cnt = [0] * NDMA
        self.dma_rr = 0

    def op(self, eng, fn, reads=(), writes=(), dma=False):
        deps = set()
        for k in reads:
            if k in self.last_w:
                deps.add(self.last_w[k])
        for k in writes:
            if k in self.last_w:
                deps.add(self.last_w[k])
            deps.update(self.readers.get(k, ()))
        if dma:
            s = self.dma_rr
            self.dma_rr = (s + 1) % NDMA
            prev = self.dma_cnt[s]
            self.dma_cnt[s] += 16
            if prev > 0:
                deps.add(('d%d' % s, prev))
            tok = ('d%d' % s, prev + 16)
        else:
            self.eng_cnt[eng] += 1
            tok = (eng, self.eng_cnt[eng])
        if eng == 'pe':
            deps = {d for d in deps if d[0] != 'pe'}
        self.ops.append((eng, fn, deps, tok, dma))
        for k in writes:
            self.last_w[k] = tok
            self.readers[k] = []
        for k in reads:
            if k not in writes:
                self.readers.setdefault(k, []).append(tok)
        return tok

    def pe(self, fn, reads=(), writes=()):
        return self.op('pe', fn, reads, writes)

    def act(self, fn, reads=(), writes=()):
        return self.op('act', fn, reads, writes)

    def dve(self, fn, reads=(), writes=()):
        return self.op('dve', fn, reads, writes)

    def pool(self, fn, reads=(), writes=()):
        return self.op('pool', fn, reads, writes)

    def dma(self, fn, reads=(), writes=(), q='sp'):
        return self.op(q, fn, reads, writes, dma=True)

    def emit(self):
        nc = self.nc
        sems = {}
        for e in self.ENG:
            sems[e] = self.es.enter_context(nc.semaphore('s_' + e))
        for i in range(NDMA):
            sems['d%d' % i] = self.es.enter_context(nc.semaphore('s_d%d' % i))
        final_waits = [('d%d' % i, self.dma_cnt[i]) for i in range(NDMA) if self.dma_cnt[i] > 0]
        final_waits += [(e, self.eng_cnt[e]) for e in self.ENG if self.eng_cnt[e] > 0]
        by_eng = {e: [o for o in self.ops if o[0] == e] for e in self.ENG}

        class PEProxy:
            def __init__(self, eng, waited):
                self.eng, self.waited, self.last, self.before = eng, waited, None, 0

            def _sync(self, ap):
                rows = ap.partition_size()
                rg = (ap.base_partition(), 32 if rows <= 32 else (64 if rows <= 64 else 128))
                if self.last is not None and rg != self.last and self.before > 0 \
                        and self.waited.get('pe', 0) < self.before:
                    self.eng.wait_ge(sems['pe'], self.before)
                    self.waited['pe'] = self.before
                self.last = rg

            def matmul(self, **kw):
                self._sync(kw['lhsT'])
                return self.eng.matmul(**kw)

            def transpose(self, **kw):
                self._sync(kw['in_'])
                return self.eng.transpose(**kw)

        def run(eng_name, eng):
            waited = {}
            proxy = PEProxy(eng, waited) if eng_name == 'pe' else None
            for (_, fn, deps, tok, dma) in by_eng[eng_name]:
                for (s, v) in sorted(deps):
                    if waited.get(s, 0) < v:
                        eng.wait_ge(sems[s], v)
                        waited[s] = v
                if eng_name == 'pe':
                    proxy.before = tok[1] - 1
                    ins = fn(proxy)
                else:
                    ins = fn(eng)
                ins.then_inc(sems[tok[0]], 16 if dma else 1)
            if eng_name == 'sp':
                for (s, v) in final_waits:
                    if waited.get(s, 0) < v:
                        eng.wait_ge(sems[s], v)

        with nc.Block() as block:
            @block.tensor
            def _(e):
                run('pe', e)

            @block.scalar
            def _(e):
                run('act', e)

            @block.vector
            def _(e):
                run('dve', e)

            @block.gpsimd
            def _(e):
                run('pool', e)

            @block.sync
            def _(e):
                run('sp', e)


class Ctx:
    pass


def build(T, L, dbg=False, phases=('A',)):
    nc = bass.Bass("TRN2", target_bir_lowering=False)
    es = ExitStack()
    P = Prog(nc, es)
    NT = T // 128

    def din(name, shape, dt=F32):
        return nc.dram_tensor(name, list(shape), dt, kind="ExternalInput").ap()

    def dscr(name, shape, dt=F32):
        kind = "ExternalOutput" if dbg else "Internal"
        return nc.dram_tensor(name, list(shape), dt, kind=kind).ap()

    def sb(name, shape, dt=F32):
        return es.enter_context(nc.sbuf_tensor(name, list(shape), dt))

    def ps(name, shape, dt=F32):
        return es.enter_context(nc.psum_tensor(name, list(shape), dt))

    x = din("x", [T, D])
    consts = din("consts", [128, 2112])
    norm_mix_g = din("norm_mix_g", [L, D])
    w_in = din("w_in", [L, D, IN_COLS])
    out = nc.dram_tensor("out", [T, D], F32, kind="ExternalOutput").ap()
    pT = dscr("pT", [IN_COLS, T])
    mixT = (din if 'Ctest' in phases else dscr)("mixT", [D, T])
    memx = din("mem", [256, D])
    w_out = din("w_out", [L, D, D])
    beta_c = din("beta_c", [L, 128, KT])
    norm_xattn_g = din("norm_xattn_g", [L, D])
    norm_mem_g = din("norm_mem_g", [L, D])
    xa_wq = din("xa_wq", [L, D, D]); xa_wk = din("xa_wk", [L, D, D])
    xa_wv = din("xa_wv", [L, D, D]); xa_wo = din("xa_wo", [L, D, D])
    norm_ffn_g = din("norm_ffn_g", [L, D])
    moe_rw = din("moe_rw", [L, D, 36])
    moe_rb = din("moe_rb", [L, 36])
    moe_wg = din("moe_w_gate", [L, 32, D, 512]); moe_wu = din("moe_w_up", [L, 32, D, 512])
    moe_wd = din("moe_w_down", [L, 32, 512, D])
    norm_final_g = din("norm_final_g", [1, D])

    c_f32 = sb("c_f32", [128, 2112])
    ident = sb("ident", [128, 128], BF16)
    ones_bf = sb("ones_bf", [128, 128], BF16)
    P.dma(lambda e: e.dma_start(out=c_f32[:], in_=consts[:, :]), writes=['c_f32'])
    P.act(lambda e: e.copy(out=ident[:], in_=c_f32[:, 0:128]), reads=['c_f32'], writes=['ident'])
    P.dve(lambda e: e.memset(ones_bf[:], 1.0), writes=['ones_bf'])
    identf = c_f32[:, 0:128]

    hb = [sb("hb%d" % i, [128, D]) for i in range(2)]
    ss = [sb("ss%d" % i, [128, 1]) for i in range(2)]
    rstd = [sb("rstd%d" % i, [128, 1]) for i in range(2)]
    xn = [sb("xn%d" % i, [128, D], BF16) for i in range(2)]
    xnf = [sb("xnf%d" % i, [128, D]) for i in range(2)]
    gb = sb("gb", [128, D])
    NTC = min(4, NT)
    TC = NTC * 128
    XN = sb("XN", [128, max(2 * KT * TC, 8192)], BF16)
    xnT = [XN[:, i * KT * TC:(i + 1) * KT * TC].rearrange("p (k t) -> p k t", k=KT) for i in range(2)]
    psT = [ps("psT%d" % i, [128, KT, 128], BF16) for i in range(2)]
    B = [ps("B%d" % i, [128, 512]) for i in range(6)]
    wst = [sb("wst%d" % i, [128, 2560]) for i in range(2)]
    WB = sb("WB", [128, 24576], BF16)
    po = [sb("po%d" % i, [128, 512]) for i in range(2)]
    win_bf = WB[:, 0:KT * IN_COLS].rearrange("p (k n) -> p k n", k=KT)

    cnt = {'tile': 0, 'chunk': 0, 'mm': 0, 'w': 0}

    def load_gb(vec_row):
        P.dma(lambda e: e.dma_start(out=gb[:], in_=vec_row.partition_broadcast(128)), writes=['gb'])

    def rms_to_T(src_rows, dstT, dst_key, tcol, src_key=None, dstTf=None, dstf_key=None):
        i = cnt['tile'] % 2
        cnt['tile'] += 1
        P.dma(lambda e: e.dma_start(out=hb[i][:], in_=src_rows), reads=[src_key] if src_key else [],
              writes=['hb%d' % i])
        P.act(lambda e: e.activation(out=xn[i][:], in_=hb[i][:], func=AF.Square, accum_out=ss[i][:]),
              reads=['hb%d' % i], writes=['xn%d' % i, 'ss%d' % i])
        P.dve(lambda e: e.tensor_scalar(out=rstd[i][:], in0=ss[i][:], scalar1=1.0 / D, scalar2=1e-6,
                                        op0=ALU.mult, op1=ALU.add), reads=['ss%d' % i], writes=['rstd%d' % i])
        P.act(lambda e: e.sqrt(out=rstd[i][:], in_=rstd[i][:]), reads=['rstd%d' % i], writes=['rstd%d' % i])
        P.dve(lambda e: e.reciprocal(out=rstd[i][:], in_=rstd[i][:]), reads=['rstd%d' % i], writes=['rstd%d' % i])
        P.dve(lambda e: e.scalar_tensor_tensor(out=xn[i][:], in0=hb[i][:], scalar=rstd[i][:, 0:1], in1=gb[:],
                                               op0=ALU.mult, op1=ALU.mult),
              reads=['hb%d' % i, 'rstd%d' % i, 'gb'], writes=['xn%d' % i])
        for kt in range(KT):
            P.pe(lambda e, kt=kt: e.transpose(out=psT[i][:, kt, :], in_=xn[i][:, kt * 128:(kt + 1) * 128],
                                              identity=ident[:]),
                 reads=['xn%d' % i, 'ident'], writes=['psT%d' % i])
        P.act(lambda e: e.copy(out=dstT[:, :, tcol:tcol + 128], in_=psT[i][:]),
              reads=['psT%d' % i], writes=[dst_key])
        if dstTf is not None:
            P.dve(lambda e: e.scalar_tensor_tensor(out=xnf[i][:], in0=hb[i][:], scalar=rstd[i][:, 0:1], in1=gb[:],
                                                    op0=ALU.mult, op1=ALU.mult),
                   reads=['hb%d' % i, 'rstd%d' % i, 'gb'], writes=['xnf%d' % i])
            for half in range(2):
                bk = 4 + half
                for q in range(4):
                    kt = half * 4 + q
                    P.pe(lambda e, kt=kt, q=q, bk=bk: e.transpose(out=B[bk][:, q * 128:(q + 1) * 128],
                                                                 in_=xnf[i][:, kt * 128:(kt + 1) * 128],
                                                                 identity=identf),
                         reads=['xnf%d' % i, 'c_f32'], writes=['B%d' % bk])
                P.act(lambda e, half=half, bk=bk: e.copy(
                    out=dstTf[:, half * 4:half * 4 + 4, 0:128],
                    in_=B[bk][:].rearrange("p (q t) -> p q t", q=4)),
                    reads=['B%d' % bk], writes=[dstf_key])

    def load_w_bf(dst3, dst_key, src2d, nk, ncols, scale_col=None, scale_key=None):
        if scale_col is None:
            step = max(1, nk // 2)
            for k in range(0, nk, step):
                n = min(step, nk - k)
                P.dma(lambda e, k=k, n=n: e.dma_start(
                    out=dst3[:, k:k + n, 0:ncols],
                    in_=src2d[k * 128:(k + n) * 128, :].rearrange("(k p) n -> p k n", p=128)),
                    writes=[dst_key], q='pool')
            return
        rows_per = max(1, 2560 // ncols)
        k = 0
        while k < nk:
            n = min(rows_per, nk - k)
            j = cnt['w'] % 2
            cnt['w'] += 1
            st = wst[j][:, 0:n * ncols].rearrange("p (k n) -> p k n", k=n)
            P.dma(lambda e, st=st, k=k, n=n: e.dma_start(
                out=st, in_=src2d[k * 128:(k + n) * 128, :].rearrange("(k p) n -> p k n", p=128)),
                writes=['wst%d' % j], q='sp')
            for kk in range(n):
                P.pool(lambda e, st=st, k=k, kk=kk: e.tensor_scalar(
                    out=dst3[:, k + kk, 0:ncols], in0=st[:, kk, :], scalar1=scale_col[:, k + kk:k + kk + 1],
                    scalar2=None, op0=ALU.mult), reads=['wst%d' % j, scale_key], writes=[dst_key])
            k += n

    fz = sb("fz", [128, 1])
    ALIAS = ['ktok', 'vtok', 'ark', 'zT', 's5i', 'WB', 'EW0', 'EW1', 'xnT0', 'xnT1', 'xfT', 'mst', 'qT_bf', 'oT_bf',
             'macc', 'mxb', 'kT_bf', 'v_bf', 'mnT', 'eT_bf', 'rden', 'xfTf', 'hidT', 'sg0', 'sg1', 'btok', 'ktok2',
             'vtok2'] + ['gm%d' % i for i in range(8)]

    def fence():
        P.dve(lambda e: e.memset(fz[:], 0.0), reads=[], writes=ALIAS + ['fz'])

    def phase_A(l, src):
        fence()
        load_w_bf(win_bf, 'WB', w_in[l], KT, IN_COLS)
        load_gb(norm_mix_g[l:l + 1, :])
        NF = (IN_COLS + 127) // 128
        for c in range(T // TC):
            ci = cnt['chunk'] % 2
            cnt['chunk'] += 1
            for tt in range(NTC):
                r0 = c * TC + tt * 128
                rms_to_T(src[r0:r0 + 128, :], xnT[ci], 'xnT%d' % ci, tt * 128, src_key=('h', r0 // 128))
            for f in range(NF):
                fw = min(128, IN_COLS - f * 128)
                m = cnt['mm'] % 2
                cnt['mm'] += 1
                for kt in range(KT):
                    P.pe(lambda e, f=f, fw=fw, kt=kt, m=m, ci=ci: e.matmul(
                        out=B[m][0:fw, 0:TC], lhsT=win_bf[:, kt, f * 128:f * 128 + fw],
                        rhs=xnT[ci][:, kt, :], start=(kt == 0), stop=(kt == KT - 1)),
                        reads=['WB', 'xnT%d' % ci], writes=['B%d' % m])
                P.act(lambda e, fw=fw, m=m: e.copy(out=po[m][0:fw, 0:TC], in_=B[m][0:fw, 0:TC]),
                      reads=['B%d' % m], writes=['po%d' % m])
                P.dma(lambda e, f=f, fw=fw, m=m, c=c: e.dma_start(
                    out=pT[f * 128:f * 128 + fw, c * TC:(c + 1) * TC], in_=po[m][0:fw, 0:TC]),
                    reads=['po%d' % m], writes=['pT'])

    colv = sb("colv", [128, 64])
    R32 = sb("R32", [128, 8192])
    mst = R32[:, 0:KT * TC].rearrange("p (k t) -> p k t", k=KT)
    PX = sb("PX", [128, 6144])
    mxb = PX[:, 0:2048].bitcast(BF16)[:, 0:KT * TC].rearrange("p (k t) -> p k t", k=KT)
    hacc = [sb("hacc%d" % i, [128, D]) for i in range(2)]
    W2 = WB[:, 0:2 * KT * D].rearrange("p (w k n) -> p w k n", w=2, k=KT)

    def add_to_h(l, src, tok_tile, banks, first_src_x):
        i = cnt['tile'] % 2
        cnt['tile'] += 1
        r0 = tok_tile * 128
        P.dma(lambda e: e.dma_start(out=hacc[i][:], in_=src[r0:r0 + 128, :]), reads=[('h', tok_tile)],
              writes=['hacc%d' % i])
        for half, bk in enumerate(banks):
            P.dve(lambda e, half=half, bk=bk: e.tensor_tensor(
                out=hacc[i][:, half * 512:(half + 1) * 512], in0=hacc[i][:, half * 512:(half + 1) * 512],
                in1=B[bk][:], op=ALU.add), reads=['B%d' % bk, 'hacc%d' % i], writes=['hacc%d' % i])
        P.dma(lambda e: e.dma_start(out=out[r0:r0 + 128, :], in_=hacc[i][:]), reads=['hacc%d' % i],
              writes=[('h', tok_tile)])

    def phase_outproj(l, src):
        fence()
        P.dma(lambda e: e.dma_start(out=colv[:, 0:KT], in_=beta_c[l]), writes=['colv'])
        load_w_bf(W2[:, 0], 'WB', w_out[l], KT, D, scale_col=colv, scale_key='colv')
        for c in range(T // TC):
            P.dma(lambda e, c=c: e.dma_start(
                out=mst[:], in_=mixT[:, c * TC:(c + 1) * TC].rearrange("(k p) t -> p k t", p=128)),
                reads=['mixT'], writes=['mst'])
            P.act(lambda e: e.copy(out=mxb[:], in_=mst[:]), reads=['mst'], writes=['mxb'])
            for tt in range(NTC):
                for half in range(2):
                    for kt in range(KT):
                        P.pe(lambda e, tt=tt, half=half, kt=kt: e.matmul(
                            out=B[half][:], lhsT=mxb[:, kt, tt * 128:(tt + 1) * 128],
                            rhs=W2[:, 0, kt, half * 512:(half + 1) * 512], start=(kt == 0), stop=(kt == KT - 1)),
                            reads=['mxb', 'WB'], writes=['B%d' % half])
                add_to_h(l, src, c * NTC + tt, [0, 1], False)

    kT_bf = PX[:, 0:1024].bitcast(BF16).rearrange("p (k t) -> p k t", k=KT)
    v_bf = PX[:, 1024:2048].bitcast(BF16).rearrange("p (k t) -> p k t", k=2)
    mnT = PX[:, 2048:3072].bitcast(BF16).rearrange("p (k t) -> p k t", k=KT)
    qT_bf = R32[:, 4096:6144].bitcast(BF16)[:, 0:KT * TC].rearrange("p (k t) -> p k t", k=KT)
    eT_bf = PX[:, 3072:3584].bitcast(BF16)[:, 0:2 * TC].rearrange("p (k t) -> p k t", k=2)
    oT_bf = R32[:, 6144:8192].bitcast(BF16)[:, 0:KT * TC].rearrange("p (k t) -> p k t", k=KT)
    rden = PX[:, 3584:3584 + TC]

    def phase_xattn(l):
        fence()
        load_gb(norm_mem_g[l:l + 1, :])
        for mt in range(2):
            rms_to_T(memx[mt * 128:(mt + 1) * 128, :], mnT, 'mnT', mt * 128)
        load_w_bf(W2[:, 0], 'WB', xa_wk[l], KT, D)
        load_w_bf(W2[:, 1], 'WB', xa_wv[l], KT, D)
        for f in range(KT):
            m = f % 2
            for kt in range(KT):
                P.pe(lambda e, f=f, kt=kt, m=m: e.matmul(out=B[m][:, 0:256], lhsT=W2[:, 0, kt, f * 128:(f + 1) * 128],
                                                        rhs=mnT[:, kt, :], start=(kt == 0), stop=(kt == KT - 1)),
                     reads=['WB', 'mnT'], writes=['B%d' % m])
            P.act(lambda e, f=f, m=m: e.copy(out=kT_bf[:, f, :], in_=B[m][:, 0:256]), reads=['B%d' % m],
                  writes=['kT_bf'])
        for mt in range(2):
            for half in range(2):
                m = 2 + half
                for kt in range(KT):
                    P.pe(lambda e, mt=mt, half=half, kt=kt, m=m: e.matmul(
                        out=B[m][:], lhsT=mnT[:, kt, mt * 128:(mt + 1) * 128],
                        rhs=W2[:, 1, kt, half * 512:(half + 1) * 512], start=(kt == 0), stop=(kt == KT - 1)),
                        reads=['WB', 'mnT'], writes=['B%d' % m])
                P.act(lambda e, mt=mt, half=half, m=m: e.copy(out=v_bf[:, mt, half * 512:(half + 1) * 512],
                                                             in_=B[m][:]), reads=['B%d' % m], writes=['v_bf'])
        load_w_bf(W2[:, 0], 'WB', xa_wq[l], KT, D)
        load_w_bf(W2[:, 1], 'WB', xa_wo[l], KT, D)
        load_gb(norm_xattn_g[l:l + 1, :])
        for c in range(T // TC):
            ci = cnt['chunk'] % 2
            cnt['chunk'] += 1
            for tt in range(NTC):
                r0 = c * TC + tt * 128
                rms_to_T(out[r0:r0 + 128, :], xnT[ci], 'xnT%d' % ci, tt * 128, src_key=('h', r0 // 128))
            for f in range(KT):
                m = f % 2
                for kt in range(KT):
                    P.pe(lambda e, f=f, kt=kt, m=m, ci=ci: e.matmul(
                        out=B[m][:, 0:TC], lhsT=W2[:, 0, kt, f * 128:(f + 1) * 128], rhs=xnT[ci][:, kt, :],
                        start=(kt == 0), stop=(kt == KT - 1)), reads=['WB', 'xnT%d' % ci], writes=['B%d' % m])
                P.act(lambda e, f=f, m=m: e.copy(out=qT_bf[:, f, :], in_=B[m][:, 0:TC]), reads=['B%d' % m],
                      writes=['qT_bf'])
            for hd in range(4):
                for mt in range(2):
                    m = 2 + mt
                    for ff in range(2):
                        f = hd * 2 + ff
                        P.pe(lambda e, f=f, ff=ff, mt=mt, m=m: e.matmul(
                            out=B[m][:, 0:TC], lhsT=kT_bf[:, f, mt * 128:(mt + 1) * 128], rhs=qT_bf[:, f, :],
                            start=(ff == 0), stop=(ff == 1)), reads=['kT_bf', 'qT_bf'], writes=['B%d' % m])
                    P.act(lambda e, mt=mt, m=m: e.activation(out=eT_bf[:, mt, :], in_=B[m][:, 0:TC], func=AF.Exp,
                                                            scale=1.0 / 16.0), reads=['B%d' % m], writes=['eT_bf'])
                for mt in range(2):
                    P.pe(lambda e, mt=mt: e.matmul(out=B[4][:, 0:TC], lhsT=ones_bf[:], rhs=eT_bf[:, mt, :],
                                                   start=(mt == 0), stop=(mt == 1)),
                         reads=['ones_bf', 'eT_bf'], writes=['B4'])
                P.dve(lambda e: e.reciprocal(out=rden[:], in_=B[4][:, 0:TC]), reads=['B4'], writes=['rden'])
                for ff in range(2):
                    f = hd * 2 + ff
                    for mt in range(2):
                        P.pe(lambda e, f=f, mt=mt: e.matmul(out=B[5][:, 0:TC], lhsT=v_bf[:, mt, f * 128:(f + 1) * 128],
                                                            rhs=eT_bf[:, mt, :], start=(mt == 0), stop=(mt == 1)),
                             reads=['v_bf', 'eT_bf'], writes=['B5'])
                    P.dve(lambda e, f=f: e.tensor_tensor(out=oT_bf[:, f, :], in0=B[5][:, 0:TC], in1=rden[:],
                                                         op=ALU.mult), reads=['B5', 'rden'], writes=['oT_bf'])
            for tt in range(NTC):
                for half in range(2):
                    for f in range(KT):
                        P.pe(lambda e, tt=tt, half=half, f=f: e.matmul(
                            out=B[half][:], lhsT=oT_bf[:, f, tt * 128:(tt + 1) * 128],
                            rhs=W2[:, 1, f, half * 512:(half + 1) * 512], start=(f == 0), stop=(f == KT - 1)),
                            reads=['oT_bf', 'WB'], writes=['B%d' % half])
                add_to_h(l, out, c * NTC + tt, [0, 1], False)

    TM = min(T, 2 * TC)
    NTM = TM // 128
    TBH = min(512, TM)
    xfT = XN[:, 0:KT * TM].rearrange("p (k t) -> p k t", k=KT)
    xfTf = PX[:, 3072:4096].rearrange("p (k t) -> p k t", k=KT)
    rw = sb("rw", [128, KT, 36])
    rbb = sb("rbb", [128, 36])
    lg = sb("lg", [128, 36])
    gates = sb("gates", [128, NTM, 32])
    rt = sb("rt", [128, 8, 32])
    r1 = sb("r1", [128, 16])
    macc = R32[:, 0:NTM * D].rearrange("p (t d) -> p t d", t=NTM)
    hidT = PX[:, 0:2048].bitcast(BF16)[:, 0:4 * TM].rearrange("p (k t) -> p k t", k=4)
    sg = [PX[:, 2048 + i * 512:2560 + i * 512] for i in range(2)]
    EWS = []
    for i_ in range(2):
        EW = WB[:, i_ * 12288:(i_ + 1) * 12288].rearrange("p (w n) -> p w n", w=3)
        EWS.append((EW[:, 0].rearrange("p (k n) -> p k n", k=8), EW[:, 1].rearrange("p (k n) -> p k n", k=8),
                    EW[:, 2].rearrange("p (k n) -> p k n", k=4)))
    BIG = 1.0e4
    dbg_g = dscr("dbg_g", [128, 32]); dbg_r1 = dscr("dbg_r1", [128, 16]); dbg_lg = dscr("dbg_lg", [128, 36])

    def route(tt):
        g = lambda i: rt[:, i, :]
        dv = lambda fn, r, w: P.dve(fn, reads=r, writes=w)
        dv(lambda e: e.tensor_reduce(out=r1[:, 0:1], in_=lg[:, 0:4], axis=AX.X, op=ALU.max), ['lg'], ['r1'])
        dv(lambda e: e.tensor_scalar(out=rt[:, 0, 0:4], in0=lg[:, 0:4], scalar1=r1[:, 0:1], scalar2=None,
                                     op0=ALU.is_equal), ['lg', 'r1'], ['rt'])
        dv(lambda e: e.tensor_scalar(out=rt[:, 1, 0:4], in0=lg[:, 0:4], scalar1=r1[:, 0:1], scalar2=None,
                                     op0=ALU.subtract), ['lg', 'r1'], ['rt'])
        P.act(lambda e: e.activation(out=rt[:, 1, 0:4], in_=rt[:, 1, 0:4], func=AF.Exp, accum_out=r1[:, 1:2]),
              ['rt'], ['rt', 'r1'])
        dv(lambda e: e.reciprocal(out=r1[:, 2:3], in_=r1[:, 1:2]), ['r1'], ['r1'])
        dv(lambda e: e.tensor_scalar(out=rt[:, 0, 0:4], in0=rt[:, 0, 0:4], scalar1=-1.0, scalar2=BIG,
                                     op0=ALU.add, op1=ALU.mult), ['rt'], ['rt'])
        dv(lambda e: e.tensor_tensor(out=rt[:, 2, :].rearrange("p (g x) -> p g x", g=4),
                                     in0=lg[:, 4:36].rearrange("p (g x) -> p g x", g=4),
                                     in1=rt[:, 0, 0:4].unsqueeze(2).to_broadcast([128, 4, 8]), op=ALU.add),
           ['lg', 'rt'], ['rt'])
        dv(lambda e: e.tensor_reduce(out=r1[:, 3:4], in_=g(2), axis=AX.X, op=ALU.max), ['rt'], ['r1'])
        dv(lambda e: e.tensor_scalar(out=g(3), in0=g(2), scalar1=r1[:, 3:4], scalar2=None, op0=ALU.is_equal),
           ['rt', 'r1'], ['rt'])
        dv(lambda e: e.scalar_tensor_tensor(out=g(4), in0=g(3), scalar=-BIG, in1=g(2), op0=ALU.mult, op1=ALU.add),
           ['rt'], ['rt'])
        dv(lambda e: e.tensor_reduce(out=r1[:, 4:5], in_=g(4), axis=AX.X, op=ALU.max), ['rt'], ['r1'])
        dv(lambda e: e.tensor_scalar(out=g(5), in0=g(4), scalar1=r1[:, 4:5], scalar2=None, op0=ALU.is_equal),
           ['rt', 'r1'], ['rt'])
        dv(lambda e: e.tensor_tensor(out=r1[:, 5:6], in0=r1[:, 4:5], in1=r1[:, 3:4], op=ALU.subtract),
           ['r1'], ['r1'])
        P.act(lambda e: e.activation(out=r1[:, 6:7], in_=r1[:, 5:6], func=AF.Exp), ['r1'], ['r1'])
        dv(lambda e: e.tensor_scalar(out=r1[:, 7:8], in0=r1[:, 6:7], scalar1=1.0, scalar2=None, op0=ALU.add),
           ['r1'], ['r1'])
        dv(lambda e: e.reciprocal(out=r1[:, 7:8], in_=r1[:, 7:8]), ['r1'], ['r1'])
        dv(lambda e: e.tensor_tensor(out=r1[:, 8:9], in0=r1[:, 7:8], in1=r1[:, 2:3], op=ALU.mult), ['r1'], ['r1'])
        dv(lambda e: e.tensor_tensor(out=r1[:, 9:10], in0=r1[:, 2:3], in1=r1[:, 8:9], op=ALU.subtract),
           ['r1'], ['r1'])
        dv(lambda e: e.tensor_scalar(out=g(6), in0=g(3), scalar1=r1[:, 8:9], scalar2=None, op0=ALU.mult),
           ['rt', 'r1'], ['rt'])
        dv(lambda e: e.scalar_tensor_tensor(out=gates[:, tt, :], in0=g(5), scalar=r1[:, 9:10], in1=g(6),
                                            op0=ALU.mult, op1=ALU.add), ['rt', 'r1'], ['gates'])

    def phase_moe(l):
        fence()
        load_gb(norm_ffn_g[l:l + 1, :])
        P.dma(lambda e: e.dma_start(out=rw[:], in_=moe_rw[l].rearrange("(k p) n -> p k n", p=128)), writes=['rw'])
        P.dma(lambda e: e.dma_start(out=rbb[:], in_=moe_rb[l:l + 1, :].partition_broadcast(128)), writes=['rbb'])
        for c in range(T // TM):
            for tt in range(NTM):
                r0 = c * TM + tt * 128
                rms_to_T(out[r0:r0 + 128, :], xfT, 'xfT', tt * 128, src_key=('h', r0 // 128),
                         dstTf=xfTf, dstf_key='xfTf')
                for kt in range(KT):
                    P.pe(lambda e, kt=kt: e.matmul(out=B[3][:, 0:36], lhsT=xfTf[:, kt, :], rhs=rw[:, kt, :],
                                                   start=(kt == 0), stop=(kt == KT - 1)),
                         reads=['xfTf', 'rw'], writes=['B3'])
                P.dve(lambda e: e.tensor_tensor(out=lg[:], in0=B[3][:, 0:36], in1=rbb[:], op=ALU.add),
                      reads=['B3', 'rbb'], writes=['lg'])
                route(tt)
            if dbg:
                P.dma(lambda e: e.dma_start(out=dbg_g[:, :], in_=gates[:, 0, :]), reads=['gates'], writes=['dbg_g'])
                P.dma(lambda e: e.dma_start(out=dbg_r1[:, :], in_=r1[:, :]), reads=['r1'], writes=['dbg_r1'])
                P.dma(lambda e: e.dma_start(out=dbg_lg[:, :], in_=lg[:, :]), reads=['lg'], writes=['dbg_lg'])
            for ex in range(32):
                wg_bf, wu_bf, wd_bf = EWS[ex % 2]
                ewk = 'EW%d' % (ex % 2)
                load_w_bf(wg_bf, ewk, moe_wg[l, ex], 8, 512)
                load_w_bf(wu_bf, ewk, moe_wu[l, ex], 8, 512)
                load_w_bf(wd_bf, ewk, moe_wd[l, ex], 4, D)
                for tb in range(TM // TBH):
                    for mt in range(4):
                        for kt in range(KT):
                            P.pe(lambda e, mt=mt, kt=kt, tb=tb, wg_bf=wg_bf: e.matmul(
                                out=B[0][:, 0:TBH], lhsT=wg_bf[:, kt, mt * 128:(mt + 1) * 128],
                                rhs=xfT[:, kt, tb * TBH:(tb + 1) * TBH], start=(kt == 0), stop=(kt == KT - 1)),
                                reads=[ewk, 'xfT'], writes=['B0'])
                        for kt in range(KT):
                            P.pe(lambda e, mt=mt, kt=kt, tb=tb, wu_bf=wu_bf: e.matmul(
                                out=B[1][:, 0:TBH], lhsT=wu_bf[:, kt, mt * 128:(mt + 1) * 128],
                                rhs=xfT[:, kt, tb * TBH:(tb + 1) * TBH], start=(kt == 0), stop=(kt == KT - 1)),
                                reads=[ewk, 'xfT'], writes=['B1'])
                        j = cnt['mm'] % 2
                        cnt['mm'] += 1
                        P.act(lambda e, j=j: e.activation(out=sg[j][:, 0:TBH], in_=B[0][:, 0:TBH], func=AF.Silu),
                              reads=['B0'], writes=['sg%d' % j])
                        P.dve(lambda e, j=j, mt=mt, tb=tb: e.tensor_tensor(
                            out=hidT[:, mt, tb * TBH:(tb + 1) * TBH], in0=sg[j][:, 0:TBH], in1=B[1][:, 0:TBH], op=ALU.mult),
                            reads=['sg%d' % j, 'B1'], writes=['hidT'])
                for tt in range(NTM):
                    for half in range(2):
                        bk = 2 + half
                        for kt in range(4):
                            P.pe(lambda e, tt=tt, half=half, kt=kt, bk=bk, wd_bf=wd_bf: e.matmul(
                                out=B[bk][:], lhsT=hidT[:, kt, tt * 128:(tt + 1) * 128],
                                rhs=wd_bf[:, kt, half * 512:(half + 1) * 512], start=(kt == 0), stop=(kt == 3)),
                                reads=['hidT', ewk], writes=['B%d' % bk])
                        sl = macc[:, tt, half * 512:(half + 1) * 512]
                        if ex == 0:
                            P.dve(lambda e, sl=sl, tt=tt, bk=bk, ex=ex: e.tensor_scalar(
                                out=sl, in0=B[bk][:], scalar1=gates[:, tt, ex:ex + 1], scalar2=None, op0=ALU.mult),
                                reads=['B%d' % bk, 'gates'], writes=['macc'])
                        else:
                            P.dve(lambda e, sl=sl, tt=tt, bk=bk, ex=ex: e.scalar_tensor_tensor(
                                out=sl, in0=B[bk][:], scalar=gates[:, tt, ex:ex + 1], in1=sl, op0=ALU.mult,
                                op1=ALU.add), reads=['B%d' % bk, 'gates', 'macc'], writes=['macc'])
            for tt in range(NTM):
                i = cnt['tile'] % 2
                cnt['tile'] += 1
                r0 = c * TM + tt * 128
                tk = r0 // 128
                P.dma(lambda e, i=i, r0=r0: e.dma_start(out=hacc[i][:], in_=out[r0:r0 + 128, :]),
                      reads=[('h', tk)], writes=['hacc%d' % i])
                P.pool(lambda e, i=i, tt=tt: e.tensor_tensor(out=hacc[i][:], in0=hacc[i][:], in1=macc[:, tt, :],
                                                             op=ALU.add), reads=['hacc%d' % i, 'macc'],
                       writes=['hacc%d' % i])
                P.dma(lambda e, i=i, r0=r0: e.dma_start(out=out[r0:r0 + 128, :], in_=hacc[i][:]),
                      reads=['hacc%d' % i], writes=[('h', tk)])

    def phase_final():
        load_gb(norm_final_g[0:1, :])
        for tk in range(NT):
            i = cnt['tile'] % 2
            cnt['tile'] += 1
            r0 = tk * 128
            P.dma(lambda e, i=i, r0=r0: e.dma_start(out=hb[i][:], in_=out[r0:r0 + 128, :]), reads=[('h', tk)],
                  writes=['hb%d' % i])
            P.act(lambda e, i=i: e.activation(out=xn[i][:], in_=hb[i][:], func=AF.Square, accum_out=ss[i][:]),
                  reads=['hb%d' % i], writes=['xn%d' % i, 'ss%d' % i])
            P.dve(lambda e, i=i: e.tensor_scalar(out=rstd[i][:], in0=ss[i][:], scalar1=1.0 / D, scalar2=1e-6,
                                                 op0=ALU.mult, op1=ALU.add), reads=['ss%d' % i], writes=['rstd%d' % i])
            P.act(lambda e, i=i: e.sqrt(out=rstd[i][:], in_=rstd[i][:]), reads=['rstd%d' % i], writes=['rstd%d' % i])
            P.dve(lambda e, i=i: e.reciprocal(out=rstd[i][:], in_=rstd[i][:]), reads=['rstd%d' % i],
                  writes=['rstd%d' % i])
            P.dve(lambda e, i=i: e.scalar_tensor_tensor(out=xnf[i][:], in0=hb[i][:], scalar=rstd[i][:, 0:1], in1=gb[:],
                                                        op0=ALU.mult, op1=ALU.mult),
                  reads=['hb%d' % i, 'rstd%d' % i, 'gb'], writes=['xnf%d' % i])
            P.dma(lambda e, i=i, r0=r0: e.dma_start(out=out[r0:r0 + 128, :], in_=xnf[i][:]), reads=['xnf%d' % i],
                  writes=[('h', tk)])

    colsB = din("colsB", [L, 128, 80])
    gla_w_up = din("gla_w_up", [L, 16, 128])
    TBk = min(512, T)
    Wt = [R32[:, i * 512:(i + 1) * 512] for i in range(16)] + \
         [XN[:, i * 1024:(i + 1) * 1024].bitcast(F32) for i in range(8)]
    WK = ['Wt%d' % i for i in range(24)]
    ALIAS.extend(WK)
    cB = sb("cB", [128, 80])
    onesm = sb("onesm", [128, 128])
    ones64 = sb("ones64", [128, 64])
    P.dve(lambda e: e.memset(onesm[:], 1.0 / 256.0), writes=['onesm'])
    P.dve(lambda e: e.memset(ones64[:], 1.0), writes=['ones64'])
    ubuf = [sb("ubuf%d" % i, [128, 30 + TBk]) for i in range(2)]
    stG = sb("stG", [64, 128])
    wup = sb("wup", [16, 128])
    zT = PX[0:16, 1792:1792 + TBk]
    ktok = PX[:, 0:512].rearrange("p (b c) -> p b c", b=4)
    vtok = PX[:, 512:1536].rearrange("p (b c) -> p b c", b=4)
    ark = PX[:, 1536:1792]
    mask_incl = c_f32[:, 128:384]
    blk64 = c_f32[:, 384:512]

    dbg_outs = {}

    def dbgdump(name, ap, key, shape):
        if not dbg:
            return
        t = nc.dram_tensor("dbg_" + name, list(shape), F32, kind="ExternalOutput").ap()
        P.dma(lambda e: e.dma_start(out=t, in_=ap), reads=[key], writes=['dbg_' + name])

    def ldrow(w, row0, c0, n=128, cols=None):
        cols = TBk if cols is None else cols
        P.dma(lambda e: e.dma_start(out=Wt[w][0:n, 0:cols], in_=pT[row0:row0 + n, c0:c0 + cols]),
              reads=['pT'], writes=[WK[w]])

    def strow(w, row0, c0):
        P.dma(lambda e: e.dma_start(out=mixT[row0:row0 + 128, c0:c0 + TBk], in_=Wt[w][:, 0:TBk]),
              reads=[WK[w]], writes=['mixT'])

    def conv_block(l, blk):
        c0 = blk * TBk
        for ct in range(2):
            ub = ubuf[ct]
            uk = 'ubuf%d' % ct
            if blk == 0:
                P.pool(lambda e, ub=ub: e.memset(ub[:, 0:30], 0.0), writes=[uk])
            else:
                P.act(lambda e, ub=ub: e.copy(out=ub[:, 0:30], in_=ub[:, TBk:TBk + 30]), reads=[uk], writes=[uk])
            ldrow(0, 1936 + ct * 128, c0)
            ldrow(1, 2192 + ct * 128, c0)
            P.act(lambda e: e.activation(out=Wt[1][:, 0:TBk], in_=Wt[1][:, 0:TBk], func=AF.Sigmoid),
                  reads=[WK[1]], writes=[WK[1]])
            P.pool(lambda e, ub=ub: e.tensor_tensor(out=ub[:, 30:30 + TBk], in0=Wt[0][:, 0:TBk], in1=Wt[1][:, 0:TBk],
                                                    op=ALU.mult), reads=[WK[0], WK[1], uk], writes=[uk])
            acc = 2 + ct
            P.dve(lambda e, ub=ub, ct=ct, acc=acc: e.tensor_scalar(
                out=Wt[acc][:, 0:TBk], in0=ub[:, 0:TBk], scalar1=cB[:, ct * 31:ct * 31 + 1], scalar2=None,
                op0=ALU.mult), reads=[uk, 'cB'], writes=[WK[acc]])
            for k in range(1, 31):
                P.dve(lambda e, ub=ub, ct=ct, acc=acc, k=k: e.scalar_tensor_tensor(
                    out=Wt[acc][:, 0:TBk], in0=ub[:, k:k + TBk], scalar=cB[:, ct * 31 + k:ct * 31 + k + 1],
                    in1=Wt[acc][:, 0:TBk], op0=ALU.mult, op1=ALU.add), reads=[uk, 'cB', WK[acc]], writes=[WK[acc]])
            P.act(lambda e, ct=ct, acc=acc: e.activation(out=Wt[acc][:, 0:TBk], in_=Wt[acc][:, 0:TBk],
                                                         func=AF.Identity, bias=cB[:, 62 + ct:63 + ct]),
                  reads=[WK[acc], 'cB'], writes=[WK[acc]])
            P.act(lambda e, ct=ct, acc=acc: e.activation(out=Wt[4 + ct][:, 0:TBk], in_=Wt[acc][:, 0:TBk],
                                                         func=AF.Square), reads=[WK[acc]], writes=[WK[4 + ct]])
        for ct in range(2):
            P.pe(lambda e, ct=ct: e.matmul(out=B[0][:, 0:TBk], lhsT=onesm[:], rhs=Wt[2 + ct][:, 0:TBk],
                                           start=(ct == 0), stop=(ct == 1)), reads=['onesm', WK[2 + ct]], writes=['B0'])
        for ct in range(2):
            P.pe(lambda e, ct=ct: e.matmul(out=B[1][:, 0:TBk], lhsT=onesm[:], rhs=Wt[4 + ct][:, 0:TBk],
                                           start=(ct == 0), stop=(ct == 1)), reads=['onesm', WK[4 + ct]], writes=['B1'])
        P.act(lambda e: e.copy(out=Wt[6][:, 0:TBk], in_=B[0][:, 0:TBk]), reads=['B0'], writes=[WK[6]])
        P.dve(lambda e: e.tensor_tensor(out=Wt[7][:, 0:TBk], in0=Wt[6][:, 0:TBk], in1=Wt[6][:, 0:TBk], op=ALU.mult),
              reads=[WK[6]], writes=[WK[7]])
        P.dve(lambda e: e.tensor_tensor(out=Wt[7][:, 0:TBk], in0=B[1][:, 0:TBk], in1=Wt[7][:, 0:TBk],
                                        op=ALU.subtract), reads=['B1', WK[7]], writes=[WK[7]])
        P.dve(lambda e: e.tensor_scalar(out=Wt[7][:, 0:TBk], in0=Wt[7][:, 0:TBk], scalar1=1e-5, scalar2=None,
                                        op0=ALU.add), reads=[WK[7]], writes=[WK[7]])
        P.act(lambda e: e.sqrt(out=Wt[7][:, 0:TBk], in_=Wt[7][:, 0:TBk]), reads=[WK[7]], writes=[WK[7]])
        P.dve(lambda e: e.reciprocal(out=Wt[7][:, 0:TBk], in_=Wt[7][:, 0:TBk]), reads=[WK[7]], writes=[WK[7]])
        for ct in range(2):
            acc = 2 + ct
            P.dve(lambda e, acc=acc: e.tensor_tensor(out=Wt[acc][:, 0:TBk], in0=Wt[acc][:, 0:TBk],
                                                     in1=Wt[6][:, 0:TBk], op=ALU.subtract),
                  reads=[WK[acc], WK[6]], writes=[WK[acc]])
            P.dve(lambda e, acc=acc: e.tensor_tensor(out=Wt[acc][:, 0:TBk], in0=Wt[acc][:, 0:TBk],
                                                     in1=Wt[7][:, 0:TBk], op=ALU.mult),
                  reads=[WK[acc], WK[7]], writes=[WK[acc]])
            P.act(lambda e, acc=acc, ct=ct: e.activation(out=Wt[acc][:, 0:TBk], in_=Wt[acc][:, 0:TBk], func=AF.Silu,
                                                         scale=cB[:, 64 + ct:65 + ct], bias=cB[:, 66 + ct:67 + ct]),
                  reads=[WK[acc], 'cB'], writes=[WK[acc]])
            strow(acc, 768 + ct * 128, c0)

    def gla_block(l, blk):
        c0 = blk * TBk
        NCH = TBk // 64
        QT, KTt, LA, BC, EB, ENB, RT, KTT = (0, 1), (2, 3), (4, 5), (6, 7), (8, 9), (10, 11), (12, 13), (14, 15)
        V0, V1, G0, G1, OB0, OB1, SQ, TMP = 16, 17, 18, 19, 20, 21, 22, 23
        for hf in range(2):
            ldrow(QT[hf], 256 + hf * 64, c0, n=64); ldrow(KTt[hf], 384 + hf * 64, c0, n=64)
        ldrow(V0, 512, c0); ldrow(V1, 640, c0)
        ldrow(G0, 768, c0); ldrow(G1, 896, c0)
        P.dma(lambda e: e.dma_start(out=zT[:, :], in_=pT[1024:1040, c0:c0 + TBk]), reads=['pT'], writes=['zT'])
        if blk == 0:
            P.pool(lambda e: e.memset(stG[:], 0.0), writes=['stG'])
        for hf in range(2):
            la, bc, eb, enb, rt, ktt = Wt[LA[hf]], Wt[BC[hf]], Wt[EB[hf]], Wt[ENB[hf]], Wt[RT[hf]], Wt[KTT[hf]]
            kla, kbc, keb, kenb, krt, kktt = (WK[LA[hf]], WK[BC[hf]], WK[EB[hf]], WK[ENB[hf]], WK[RT[hf]],
                                              WK[KTT[hf]])
            P.pe(lambda e, hf=hf: e.matmul(out=B[0][0:64, 0:TBk], lhsT=wup[:, hf * 64:(hf + 1) * 64], rhs=zT[:, :],
                                           start=True, stop=True), reads=['wup', 'zT'], writes=['B0'])
            P.act(lambda e, la=la, hf=hf: e.activation(out=la[0:64, 0:TBk], in_=B[0][0:64, 0:TBk], func=AF.Exp,
                                                       scale=-1.0, bias=cB[0:64, 71 + 2 * hf:72 + 2 * hf]),
                  reads=['B0', 'cB'], writes=[kla])
            P.act(lambda e, la=la: e.activation(out=la[0:64, 0:TBk], in_=la[0:64, 0:TBk], func=AF.Ln, bias=1.0),
                  reads=[kla], writes=[kla])
            P.dve(lambda e, la=la: e.tensor_scalar(out=la[0:64, 0:TBk], in0=la[0:64, 0:TBk], scalar1=-1.0 / 16.0,
                                                   scalar2=None, op0=ALU.mult), reads=[kla], writes=[kla])
            for c in range(NCH):
                P.dve(lambda e, c=c, la=la, bc=bc: e.tensor_tensor_scan(
                    out=bc[0:64, c * 64:(c + 1) * 64], data0=ones64[0:64, :], data1=la[0:64, c * 64:(c + 1) * 64],
                    initial=0.0, op0=ALU.mult, op1=ALU.add), reads=['ones64', kla], writes=[kbc])
            P.act(lambda e, bc=bc, eb=eb: e.activation(out=eb[0:64, 0:TBk], in_=bc[0:64, 0:TBk], func=AF.Exp),
                  reads=[kbc], writes=[keb])
            P.act(lambda e, bc=bc, enb=enb: e.activation(out=enb[0:64, 0:TBk], in_=bc[0:64, 0:TBk], func=AF.Exp,
                                                         scale=-1.0), reads=[kbc], writes=[kenb])
            P.dve(lambda e, hf=hf, rt=rt, eb=eb: e.scalar_tensor_tensor(
                out=rt[0:64, 0:TBk], in0=Wt[QT[hf]][0:64, 0:TBk], scalar=32.0 ** -0.5, in1=eb[0:64, 0:TBk],
                op0=ALU.mult, op1=ALU.mult), reads=[WK[QT[hf]], keb], writes=[krt])
            P.pool(lambda e, hf=hf, ktt=ktt, enb=enb: e.tensor_tensor(
                out=ktt[0:64, 0:TBk], in0=Wt[KTt[hf]][0:64, 0:TBk], in1=enb[0:64, 0:TBk], op=ALU.mult),
                reads=[WK[KTt[hf]], kenb], writes=[kktt])
        if DBG_STOP <= 1:
            return
        for b4 in range(TBk // 128):
            sl = slice(b4 * 128, (b4 + 1) * 128)
            for hf in range(2):
                P.pe(lambda e, sl=sl, hf=hf: e.transpose(out=B[1][:, hf * 64:(hf + 1) * 64],
                                                         in_=Wt[KTT[hf]][0:64, sl], identity=identf[0:64, 0:64]),
                     reads=[WK[KTT[hf]], 'c_f32'], writes=['B1'])
            P.pe(lambda e, sl=sl: e.transpose(out=B[1][:, 128:256], in_=Wt[V0][:, sl], identity=identf),
                 reads=[WK[V0], 'c_f32'], writes=['B1'])
            P.pe(lambda e, sl=sl: e.transpose(out=B[1][:, 256:384], in_=Wt[V1][:, sl], identity=identf),
                 reads=[WK[V1], 'c_f32'], writes=['B1'])
            P.act(lambda e, b4=b4: e.copy(out=ktok[:, b4, :], in_=B[1][:, 0:128]), reads=['B1'], writes=['ktok'])
            P.act(lambda e, b4=b4: e.copy(out=vtok[:, b4, :], in_=B[1][:, 128:384]), reads=['B1'], writes=['vtok'])
        if DBG_STOP <= 2:
            return
        for c in range(NCH):
            b4, pb = c // 2, (c % 2) * 64
            cs = slice(c * 64, (c + 1) * 64)
            for hd in range(4):
                hf = hd // 2
                hs = slice((hd % 2) * 32, (hd % 2) * 32 + 32)
                P.pe(lambda e, hd=hd, hf=hf, hs=hs, cs=cs, pb=pb: e.matmul(
                    out=B[2][pb:pb + 64, hd * 64:(hd + 1) * 64], lhsT=Wt[KTT[hf]][hs, cs], rhs=Wt[RT[hf]][hs, cs],
                    start=True, stop=True), reads=[WK[KTT[hf]], WK[RT[hf]]], writes=['B2'])
            if DBG_STOP == 3 and DBG_VAR == 1:
                continue
            P.dve(lambda e, pb=pb: e.tensor_tensor(out=ark[pb:pb + 64, :], in0=B[2][pb:pb + 64, 0:256],
                                                   in1=mask_incl[pb:pb + 64, :], op=ALU.mult),
                  reads=['B2', 'c_f32'], writes=['ark'])
            if DBG_STOP <= 3:
                continue
            for hd in range(4):
                hf = hd // 2
                hs = slice((hd % 2) * 32, (hd % 2) * 32 + 32)
                vt, vb = hd // 2, (hd % 2) * 64
                P.pe(lambda e, hf=hf, hs=hs, cs=cs, vt=vt, vb=vb: e.matmul(
                    out=B[3][vb:vb + 64, vt * 64:(vt + 1) * 64], lhsT=stG[hs, hf * 64:(hf + 1) * 64],
                    rhs=Wt[RT[hf]][hs, cs], start=True, stop=False), reads=['stG', WK[RT[hf]]], writes=['B3'])
                P.pe(lambda e, hd=hd, b4=b4, pb=pb, vt=vt, vb=vb: e.matmul(
                    out=B[3][vb:vb + 64, vt * 64:(vt + 1) * 64], lhsT=vtok[pb:pb + 64, b4, hd * 64:(hd + 1) * 64],
                    rhs=ark[pb:pb + 64, hd * 64:(hd + 1) * 64], start=False, stop=True),
                    reads=['vtok', 'ark'], writes=['B3'])
            P.act(lambda e, cs=cs: e.copy(out=Wt[OB0][:, cs], in_=B[3][:, 0:64]), reads=['B3'], writes=[WK[OB0]])
            P.act(lambda e, cs=cs: e.copy(out=Wt[OB1][:, cs], in_=B[3][:, 64:128]), reads=['B3'], writes=[WK[OB1]])
            if DBG_STOP <= 4:
                continue
            for hd in range(4):
                hf = hd // 2
                hs = slice((hd % 2) * 32, (hd % 2) * 32 + 32)
                P.pe(lambda e, hd=hd, hf=hf, hs=hs, b4=b4, pb=pb: e.matmul(
                    out=B[4][hs, hf * 64:(hf + 1) * 64], lhsT=ktok[pb:pb + 64, b4, hd * 32:(hd + 1) * 32],
                    rhs=vtok[pb:pb + 64, b4, hd * 64:(hd + 1) * 64], start=True, stop=True),
                    reads=['ktok', 'vtok'], writes=['B4'])
            P.dve(lambda e: e.tensor_tensor(out=stG[0:64, :], in0=B[4][0:64, 0:128], in1=stG[0:64, :], op=ALU.add),
                  reads=['B4', 'stG'], writes=['stG'])
            for hf in range(2):
                P.dve(lambda e, c=c, hf=hf: e.tensor_scalar(
                    out=stG[0:64, hf * 64:(hf + 1) * 64], in0=stG[0:64, hf * 64:(hf + 1) * 64],
                    scalar1=Wt[EB[hf]][0:64, c * 64 + 63:c * 64 + 64], scalar2=None, op0=ALU.mult),
                    reads=['stG', WK[EB[hf]]], writes=['stG'])
        if DBG_STOP <= 5:
            return
        for vt, (OB, G) in enumerate(((OB0, G0), (OB1, G1))):
            P.act(lambda e, OB=OB: e.activation(out=Wt[SQ][:, 0:TBk], in_=Wt[OB][:, 0:TBk], func=AF.Square),
                  reads=[WK[OB]], writes=[WK[SQ]])
            P.pe(lambda e: e.matmul(out=B[5][:, 0:TBk], lhsT=blk64, rhs=Wt[SQ][:, 0:TBk], start=True, stop=True),
                 reads=['c_f32', WK[SQ]], writes=['B5'])
            P.dve(lambda e: e.tensor_scalar(out=Wt[TMP][:, 0:TBk], in0=B[5][:, 0:TBk], scalar1=1e-6, scalar2=None,
                                            op0=ALU.add), reads=['B5'], writes=[WK[TMP]])
            P.act(lambda e: e.sqrt(out=Wt[TMP][:, 0:TBk], in_=Wt[TMP][:, 0:TBk]), reads=[WK[TMP]], writes=[WK[TMP]])
            P.dve(lambda e: e.reciprocal(out=Wt[TMP][:, 0:TBk], in_=Wt[TMP][:, 0:TBk]), reads=[WK[TMP]],
                  writes=[WK[TMP]])
            P.dve(lambda e, OB=OB, vt=vt: e.scalar_tensor_tensor(
                out=Wt[OB][:, 0:TBk], in0=Wt[OB][:, 0:TBk], scalar=cB[:, 69 + vt:70 + vt], in1=Wt[TMP][:, 0:TBk],
                op0=ALU.mult, op1=ALU.mult), reads=[WK[OB], WK[TMP], 'cB'], writes=[WK[OB]])
            P.act(lambda e, G=G: e.activation(out=Wt[G][:, 0:TBk], in_=Wt[G][:, 0:TBk], func=AF.Silu),
                  reads=[WK[G]], writes=[WK[G]])
            P.pool(lambda e, OB=OB, G=G: e.tensor_tensor(out=Wt[OB][:, 0:TBk], in0=Wt[OB][:, 0:TBk],
                                                         in1=Wt[G][:, 0:TBk], op=ALU.mult),
                   reads=[WK[OB], WK[G]], writes=[WK[OB]])
            strow(OB, 256 + vt * 128, c0)

    s5p_in = din("s5p", [L, 128, 24])
    s5B_in = din("s5B", [L, 128, 16, 128])
    s5C_in = din("s5C", [L, 128, 2, 8, 16])
    s5_glu_w = din("s5_glu_w", [L, 256, 256])
    s5p = sb("s5p_sb", [128, 24])
    s5t = sb("s5t", [128, 16, 8])
    s5C = sb("s5C_sb", [128, 2, 8, 16])
    s5Cp = sb("s5Cp", [128, 2, 8, 16])
    gluw = sb("gluw", [128, 2, 256])
    carry = sb("carry", [128, 2, 8])
    WBf = WB[:].bitcast(F32)
    sinT = [WBf[:, j * 512:(j + 1) * 512] for j in range(8)]
    cosT = [WBf[:, (8 + j) * 512:(9 + j) * 512] for j in range(8)]
    Bm = wst[0][:, 0:2048].rearrange("p (m c) -> p m c", m=16)
    CL = wst[1][:, 0:2048].rearrange("p (m c) -> p m c", m=16)
    iota_t = c_f32[:, 1024:1536]
    mcol = c_f32[:, 1536:1538]
    PI = float(np.pi)

    s5i = PX[:, 2304:2816].bitcast(I32)

    def sin_red(dst, x, n, rk, wk, add=0.0):
        q, m = Wt[22][:, 0:n], Wt[23][:, 0:n]
        qi = s5i[:, 0:n]
        kq, km = WK[22], WK[23]
        P.dve(lambda e: e.tensor_scalar(out=dst, in0=x, scalar1=add, scalar2=None, op0=ALU.add), reads=rk, writes=wk)
        P.dve(lambda e: e.tensor_scalar(out=q, in0=dst, scalar1=1.0 / (2 * PI), scalar2=None, op0=ALU.mult),
              reads=wk, writes=[kq])
        P.dve(lambda e: e.tensor_copy(out=qi, in_=q), reads=[kq], writes=['s5i'])
        P.dve(lambda e: e.tensor_copy(out=q, in_=qi), reads=['s5i'], writes=[kq])
        P.dve(lambda e: e.scalar_tensor_tensor(out=dst, in0=q, scalar=-2 * PI, in1=dst, op0=ALU.mult, op1=ALU.add),
              reads=[kq] + wk, writes=wk)
        P.dve(lambda e: e.tensor_scalar(out=m, in0=dst, scalar1=PI, scalar2=None, op0=ALU.is_gt), reads=wk, writes=[km])
        P.dve(lambda e: e.scalar_tensor_tensor(out=dst, in0=m, scalar=-2 * PI, in1=dst, op0=ALU.mult, op1=ALU.add),
              reads=[km] + wk, writes=wk)
        P.dve(lambda e: e.tensor_scalar(out=m, in0=dst, scalar1=-PI, scalar2=None, op0=ALU.is_lt), reads=wk, writes=[km])
        P.dve(lambda e: e.scalar_tensor_tensor(out=dst, in0=m, scalar=2 * PI, in1=dst, op0=ALU.mult, op1=ALU.add),
              reads=[km] + wk, writes=wk)
        P.act(lambda e: e.activation(out=dst, in_=dst, func=AF.Sin), reads=wk, writes=wk)

    def s5_setup(l):
        t = lambda i: s5t[:, i, :]
        dv = lambda fn: P.dve(fn, reads=['s5t', 's5p'], writes=['s5t'])
        P.dma(lambda e: e.dma_start(out=s5p[:], in_=s5p_in[l]), writes=['s5p'])
        P.dma(lambda e: e.dma_start(out=Bm, in_=s5B_in[l]), writes=['wst0'])
        P.dma(lambda e: e.dma_start(out=s5C[:], in_=s5C_in[l]), writes=['s5C'])
        P.dma(lambda e: e.dma_start(out=gluw[:], in_=s5_glu_w[l].rearrange("(k p) n -> p k n", p=128)),
              writes=['gluw'])
        P.pool(lambda e: e.memset(carry[:], 0.0), writes=['carry'])
        lr, li, ldt = s5p[:, 0:8], s5p[:, 8:16], s5p[:, 16:24]
        P.act(lambda e: e.activation(out=t(0), in_=ldt, func=AF.Exp), reads=['s5p'], writes=['s5t'])
        dv(lambda e: e.tensor_tensor(out=t(1), in0=lr, in1=t(0), op=ALU.mult))
        dv(lambda e: e.tensor_tensor(out=t(2), in0=li, in1=t(0), op=ALU.mult))
        P.act(lambda e: e.activation(out=t(3), in_=t(1), func=AF.Exp), reads=['s5t'], writes=['s5t'])
        sin_red(t(4), t(2), 8, ['s5t'], ['s5t'])
        sin_red(t(5), t(2), 8, ['s5t'], ['s5t'], add=0.5 * PI)
        dv(lambda e: e.tensor_tensor(out=t(6), in0=t(3), in1=t(5), op=ALU.mult))
        dv(lambda e: e.tensor_scalar(out=t(6), in0=t(6), scalar1=-1.0, scalar2=None, op0=ALU.add))
        dv(lambda e: e.tensor_tensor(out=t(7), in0=t(3), in1=t(4), op=ALU.mult))
        dv(lambda e: e.tensor_tensor(out=t(8), in0=lr, in1=lr, op=ALU.mult))
        dv(lambda e: e.tensor_tensor(out=t(9), in0=li, in1=li, op=ALU.mult))
        dv(lambda e: e.tensor_tensor(out=t(8), in0=t(8), in1=t(9), op=ALU.add))
        dv(lambda e: e.reciprocal(out=t(8), in_=t(8)))
        dv(lambda e: e.tensor_tensor(out=t(9), in0=t(6), in1=lr, op=ALU.mult))
        dv(lambda e: e.tensor_tensor(out=t(10), in0=t(7), in1=li, op=ALU.mult))
        dv(lambda e: e.tensor_tensor(out=t(9), in0=t(9), in1=t(10), op=ALU.add))
        dv(lambda e: e.tensor_tensor(out=t(9), in0=t(9), in1=t(8), op=ALU.mult))
        dv(lambda e: e.tensor_tensor(out=t(10), in0=t(7), in1=lr, op=ALU.mult))
        dv(lambda e: e.tensor_tensor(out=t(11), in0=t(6), in1=li, op=ALU.mult))
        dv(lambda e: e.tensor_tensor(out=t(10), in0=t(10), in1=t(11), op=ALU.subtract))
        dv(lambda e: e.tensor_tensor(out=t(10), in0=t(10), in1=t(8), op=ALU.mult))
        bc = lambda i: s5t[:, i, :].unsqueeze(2).to_broadcast([128, 8, 16])
        cr, ci = s5C[:, 0], s5C[:, 1]
        d2 = lambda fn: P.dve(fn, reads=['s5t', 's5C', 's5Cp'], writes=['s5Cp'])
        d2(lambda e: e.tensor_tensor(out=s5Cp[:, 0], in0=cr, in1=bc(9), op=ALU.mult))
        d2(lambda e: e.tensor_tensor(out=s5Cp[:, 1], in0=ci, in1=bc(10), op=ALU.mult))
        d2(lambda e: e.tensor_tensor(out=s5Cp[:, 0], in0=s5Cp[:, 0], in1=s5Cp[:, 1], op=ALU.subtract))
        d2(lambda e: e.tensor_tensor(out=s5Cp[:, 1], in0=cr, in1=bc(10), op=ALU.mult))
        P.dve(lambda e: e.tensor_tensor(out=s5C[:, 0], in0=ci, in1=bc(9), op=ALU.mult), reads=['s5t', 's5C'],
              writes=['s5C'])
        d2(lambda e: e.tensor_tensor(out=s5Cp[:, 1], in0=s5Cp[:, 1], in1=s5C[:, 0], op=ALU.add))
        P.pool(lambda e: e.memset(wst[1][:, 0:2048], 0.0), writes=['wst1'])
        P.dve(lambda e: e.tensor_scalar(out=s5t[:, 12, 0:2], in0=mcol, scalar1=-1.0, scalar2=None, op0=ALU.mult),
              reads=['c_f32'], writes=['s5t'])
        for j in range(8):
            for two in range(2):
                c0 = (j % 4) * 32 + two * 16
                P.dve(lambda e, j=j, two=two, c0=c0: e.tensor_scalar(
                    out=CL[:, j, c0:c0 + 16], in0=s5Cp[:, 0, j, :], scalar1=mcol[:, two:two + 1], scalar2=None,
                    op0=ALU.mult), reads=['s5Cp', 'c_f32'], writes=['wst1'])
                P.dve(lambda e, j=j, two=two, c0=c0: e.tensor_scalar(
                    out=CL[:, 8 + j, c0:c0 + 16], in0=s5Cp[:, 1, j, :], scalar1=s5t[:, 12, two:two + 1],
                    scalar2=None, op0=ALU.mult), reads=['s5Cp', 's5t'], writes=['wst1'])
            for tab, off in ((sinT[j], 0.0), (cosT[j], 0.5 * PI)):
                P.dve(lambda e, j=j, tab=tab: e.tensor_scalar(
                    out=tab, in0=iota_t, scalar1=s5t[:, 2, j:j + 1], scalar2=None, op0=ALU.mult),
                    reads=['s5t', 'c_f32'], writes=['WB'])
                sin_red(tab, tab, 512, ['WB'], ['WB'], add=off)

    def s5_block(l, blk):
        c0 = blk * TBk
        U = (0, 1)
        BR, BI, T1, T2, T3, T4, ZR, ZI, XR, XI, GT0, GT1 = range(2, 14)
        ldrow(U[0], 0, c0); ldrow(U[1], 128, c0)
        W = lambda i: Wt[i][:, 0:TBk]
        for j in range(8):
            ct = j // 4
            P.pe(lambda e, j=j, ct=ct: e.matmul(out=B[0][:, 0:TBk], lhsT=Bm[:, j, :], rhs=W(U[ct]), start=True,
                                                stop=True), reads=['wst0', WK[U[ct]]], writes=['B0'])
            P.pe(lambda e, j=j, ct=ct: e.matmul(out=B[1][:, 0:TBk], lhsT=Bm[:, 8 + j, :], rhs=W(U[ct]), start=True,
                                                stop=True), reads=['wst0', WK[U[ct]]], writes=['B1'])
            P.act(lambda e: e.copy(out=W(BR), in_=B[0][:, 0:TBk]), reads=['B0'], writes=[WK[BR]])
            P.act(lambda e: e.copy(out=W(BI), in_=B[1][:, 0:TBk]), reads=['B1'], writes=[WK[BI]])
            sn, cs_ = sinT[j][:, 0:TBk], cosT[j][:, 0:TBk]
            P.dve(lambda e, cs_=cs_: e.tensor_tensor(out=W(T1), in0=W(BR), in1=cs_, op=ALU.mult),
                  reads=[WK[BR], 'WB'], writes=[WK[T1]])
            P.pool(lambda e, sn=sn: e.tensor_tensor(out=W(T2), in0=W(BI), in1=sn, op=ALU.mult),
                   reads=[WK[BI], 'WB'], writes=[WK[T2]])
            P.dve(lambda e, cs_=cs_: e.tensor_tensor(out=W(T3), in0=W(BI), in1=cs_, op=ALU.mult),
                  reads=[WK[BI], 'WB'], writes=[WK[T3]])
            P.pool(lambda e, sn=sn: e.tensor_tensor(out=W(T4), in0=W(BR), in1=sn, op=ALU.mult),
                   reads=[WK[BR], 'WB'], writes=[WK[T4]])
            P.dve(lambda e: e.tensor_tensor(out=W(T1), in0=W(T1), in1=W(T2), op=ALU.add),
                  reads=[WK[T1], WK[T2]], writes=[WK[T1]])
            P.dve(lambda e: e.tensor_tensor(out=W(T3), in0=W(T3), in1=W(T4), op=ALU.subtract),
                  reads=[WK[T3], WK[T4]], writes=[WK[T3]])
            rb = s5t[:, 3, j:j + 1].to_broadcast([128, TBk])
            P.dve(lambda e, j=j, rb=rb: e.tensor_tensor_scan(out=W(ZR), data0=rb, data1=W(T1),
                                                             initial=carry[:, 0, j:j + 1], op0=ALU.mult, op1=ALU.add),
                  reads=['s5t', WK[T1], 'carry'], writes=[WK[ZR]])
            P.dve(lambda e, j=j, rb=rb: e.tensor_tensor_scan(out=W(ZI), data0=rb, data1=W(T3),
                                                             initial=carry[:, 1, j:j + 1], op0=ALU.mult, op1=ALU.add),
                  reads=['s5t', WK[T3], 'carry'], writes=[WK[ZI]])
            P.pool(lambda e, sn=sn: e.tensor_tensor(out=W(T2), in0=W(ZI), in1=sn, op=ALU.mult),
                   reads=[WK[ZI], 'WB'], writes=[WK[T2]])
            P.pool(lambda e, sn=sn: e.tensor_tensor(out=W(T4), in0=W(ZR), in1=sn, op=ALU.mult),
                   reads=[WK[ZR], 'WB'], writes=[WK[T4]])
            P.dve(lambda e, cs_=cs_: e.tensor_tensor(out=W(XR), in0=W(ZR), in1=cs_, op=ALU.mult),
                  reads=[WK[ZR], 'WB'], writes=[WK[XR]])
            P.dve(lambda e: e.tensor_tensor(out=W(XR), in0=W(XR), in1=W(T2), op=ALU.subtract),
                  reads=[WK[XR], WK[T2]], writes=[WK[XR]])
            P.dve(lambda e, cs_=cs_: e.tensor_tensor(out=W(XI), in0=W(ZI), in1=cs_, op=ALU.mult),
                  reads=[WK[ZI], 'WB'], writes=[WK[XI]])
            P.dve(lambda e: e.tensor_tensor(out=W(XI), in0=W(XI), in1=W(T4), op=ALU.add),
                  reads=[WK[XI], WK[T4]], writes=[WK[XI]])
            P.act(lambda e, j=j: e.copy(out=carry[:, 0, j:j + 1], in_=Wt[XR][:, TBk - 1:TBk]), reads=[WK[XR]],
                  writes=['carry'])
            P.act(lambda e, j=j: e.copy(out=carry[:, 1, j:j + 1], in_=Wt[XI][:, TBk - 1:TBk]), reads=[WK[XI]],
                  writes=['carry'])
            P.pe(lambda e, j=j, ct=ct: e.matmul(out=B[2 + ct][:, 0:TBk], lhsT=CL[:, j, :], rhs=W(XR),
                                                start=(j % 4 == 0), stop=False), reads=['wst1', WK[XR]],
                 writes=['B%d' % (2 + ct)])
            P.pe(lambda e, j=j, ct=ct: e.matmul(out=B[2 + ct][:, 0:TBk], lhsT=CL[:, 8 + j, :], rhs=W(XI),
                                                start=False, stop=(j % 4 == 3)), reads=['wst1', WK[XI]],
                 writes=['B%d' % (2 + ct)])
        for ct, GT in enumerate((GT0, GT1)):
            P.dve(lambda e, ct=ct, GT=GT: e.scalar_tensor_tensor(
                out=W(GT), in0=W(U[ct]), scalar=cB[:, 74 + ct:75 + ct], in1=B[2 + ct][:, 0:TBk], op0=ALU.mult,
                op1=ALU.add), reads=[WK[U[ct]], 'cB', 'B%d' % (2 + ct)], writes=[WK[GT]])
            P.act(lambda e, GT=GT: e.activation(out=W(GT), in_=W(GT), func=AF.Gelu), reads=[WK[GT]], writes=[WK[GT]])
        for ot, GT in enumerate((GT0, GT1)):
            for kt, GK in enumerate((GT0, GT1)):
                P.pe(lambda e, ot=ot, kt=kt, GK=GK: e.matmul(out=B[4][:, 0:TBk], lhsT=gluw[:, kt, ot * 128:(ot + 1) * 128],
                                                            rhs=W(GK), start=(kt == 0), stop=(kt == 1)),
                     reads=['gluw', WK[GK]], writes=['B4'])
            P.act(lambda e, ot=ot: e.activation(out=W(T1), in_=B[4][:, 0:TBk], func=AF.Sigmoid,
                                                bias=cB[:, 76 + ot:77 + ot]), reads=['B4', 'cB'], writes=[WK[T1]])
            P.pool(lambda e, GT=GT: e.tensor_tensor(out=W(T2), in0=W(GT), in1=W(T1), op=ALU.mult),
                   reads=[WK[GT], WK[T1]], writes=[WK[T2]])
            strow(T2, ot * 128, c0)

    colsR_in = din("colsR", [L, 128, 24])
    rw_w2 = din("rw_w2", [L, 32, 256]); rw_a2 = din("rw_a2", [L, 32, 256]); rw_g2 = din("rw_g2", [L, 64, 256])
    TBr = min(256, T)
    NCr = TBr // 64
    Ht = [Wt[i // 2][:, (i % 2) * 256:(i % 2) * 256 + 256] for i in range(48)]
    HK = ['Ht%d' % i for i in range(48)]
    ALIAS.extend(HK)
    cR = sb("cR", [128, 24])
    lora = sb("lora", [128, 256])
    stR = sb("stR", [128, 2, 64])
    btok, ktok2, vtok2 = [PX[:, 4096 + i * 512:4608 + i * 512].rearrange("p (b c) -> p b c", b=2) for i in range(3)]
    GMS = [PX[:, i * 512:(i + 1) * 512] for i in range(8)]
    GZ, GN, GAK, GRK, GRB, GP, GX, GU = GMS
    GKEY = ['gm%d' % i for i in range(8)]

    def gkey(ap):
        for i, g_ in enumerate(GMS):
            if g_ is ap:
                return GKEY[i]
        raise KeyError
    mask_su = c_f32[:, 512:768]
    mask_sl = c_f32[:, 1600:1856]
    I4 = c_f32[:, 1856:2112]
    EM05 = float(np.exp(-0.5))

    def rw_block(l, blk):
        fence()
        c0 = blk * TBr
        H = lambda i: Ht[i][:, 0:TBr]
        X = list(range(0, 7))
        XP = 7
        LW, AA, GG, KK = (8, 9), (10, 11), (12, 13), (14, 15)
        LC, WI, WN, WE = (16, 17), (18, 19), (20, 21), (22, 23)
        RT_, AT_, BT_, KT_ = (24, 25), (26, 27), (28, 29), (30, 31)
        YT, BON, T1, T2 = (32, 33), (34, 35), 36, 37
        R_, K_, V_ = (0, 1), (2, 3), (4, 5)
        Zt = 6
        if blk == 0:
            P.pool(lambda e: e.memset(stR[:], 0.0), writes=['stR'])
        for i in range(7):
            r0 = 1040 + i * 128
            P.dma(lambda e, i=i, r0=r0: e.dma_start(out=H(X[i]), in_=pT[r0:r0 + 128, c0:c0 + TBr]), reads=['pT'],
                  writes=[HK[X[i]]])
            if blk == 0:
                P.pool(lambda e: e.memset(Ht[XP][:, 0:1], 0.0), writes=[HK[XP]])
                P.dma(lambda e, r0=r0: e.dma_start(out=Ht[XP][:, 1:TBr], in_=pT[r0:r0 + 128, 0:TBr - 1]),
                      reads=['pT'], writes=[HK[XP]])
            else:
                P.dma(lambda e, r0=r0: e.dma_start(out=H(XP), in_=pT[r0:r0 + 128, c0 - 1:c0 - 1 + TBr]),
                      reads=['pT'], writes=[HK[XP]])
            P.dve(lambda e, i=i: e.tensor_tensor(out=H(XP), in0=H(XP), in1=H(X[i]), op=ALU.subtract),
                  reads=[HK[XP], HK[X[i]]], writes=[HK[XP]])
            P.dve(lambda e, i=i: e.scalar_tensor_tensor(out=H(X[i]), in0=H(XP), scalar=cR[:, i:i + 1], in1=H(X[i]),
                                                        op0=ALU.mult, op1=ALU.add),
                  reads=[HK[XP], HK[X[i]], 'cR'], writes=[HK[X[i]]])
        P.act(lambda e: e.activation(out=Ht[Zt][0:32, 0:TBr], in_=Ht[Zt][0:32, 0:TBr], func=AF.Tanh),
              reads=[HK[Zt]], writes=[HK[Zt]])
        P.act(lambda e: e.activation(out=Ht[Zt][64:128, 0:TBr], in_=Ht[Zt][64:128, 0:TBr], func=AF.Sigmoid),
              reads=[HK[Zt]], writes=[HK[Zt]])
        for ct in range(2):
            cs2 = slice(ct * 128, (ct + 1) * 128)
            P.pe(lambda e, cs2=cs2: e.matmul(out=B[0][:, 0:TBr], lhsT=lora[0:32, cs2], rhs=Ht[Zt][0:32, 0:TBr],
                                             start=True, stop=True), reads=['lora', HK[Zt]], writes=['B0'])
            P.act(lambda e, ct=ct: e.activation(out=H(LW[ct]), in_=B[0][:, 0:TBr], func=AF.Sigmoid,
                                                bias=cR[:, 7 + ct:8 + ct]), reads=['B0', 'cR'], writes=[HK[LW[ct]]])
            P.dve(lambda e, ct=ct: e.tensor_scalar(out=H(LW[ct]), in0=H(LW[ct]), scalar1=-EM05, scalar2=None,
                                                   op0=ALU.mult), reads=[HK[LW[ct]]], writes=[HK[LW[ct]]])
            P.pe(lambda e, cs2=cs2: e.matmul(out=B[1][:, 0:TBr], lhsT=lora[32:64, cs2], rhs=Ht[Zt][32:64, 0:TBr],
                                             start=True, stop=True), reads=['lora', HK[Zt]], writes=['B1'])
            P.act(lambda e, ct=ct: e.activation(out=H(AA[ct]), in_=B[1][:, 0:TBr], func=AF.Sigmoid,
                                                bias=cR[:, 9 + ct:10 + ct]), reads=['B1', 'cR'], writes=[HK[AA[ct]]])
            P.pe(lambda e, cs2=cs2: e.matmul(out=B[2][:, 0:TBr], lhsT=lora[64:128, cs2], rhs=Ht[Zt][64:128, 0:TBr],
                                             start=True, stop=True), reads=['lora', HK[Zt]], writes=['B2'])
            P.act(lambda e, ct=ct: e.copy(out=H(GG[ct]), in_=B[2][:, 0:TBr]), reads=['B2'], writes=[HK[GG[ct]]])
            P.dve(lambda e, ct=ct: e.tensor_scalar(out=H(KK[ct]), in0=H(K_[ct]), scalar1=cR[:, 11 + ct:12 + ct],
                                                   scalar2=None, op0=ALU.mult), reads=[HK[K_[ct]], 'cR'],
                  writes=[HK[KK[ct]]])
            P.act(lambda e, ct=ct: e.activation(out=H(T1), in_=H(KK[ct]), func=AF.Square), reads=[HK[KK[ct]]],
                  writes=[HK[T1]])
            P.pe(lambda e: e.matmul(out=B[3][:, 0:TBr], lhsT=blk64, rhs=H(T1), start=True, stop=True),
                 reads=['c_f32', HK[T1]], writes=['B3'])
            P.act(lambda e: e.activation(out=H(T1), in_=B[3][:, 0:TBr], func=AF.Sqrt, scale=64.0), reads=['B3'],
                  writes=[HK[T1]])
            P.dve(lambda e: e.tensor_scalar(out=H(T1), in0=H(T1), scalar1=1e-12, scalar2=None, op0=ALU.max),
                  reads=[HK[T1]], writes=[HK[T1]])
            P.dve(lambda e: e.reciprocal(out=H(T1), in_=H(T1)), reads=[HK[T1]], writes=[HK[T1]])
            P.dve(lambda e, ct=ct: e.tensor_tensor(out=H(KK[ct]), in0=H(KK[ct]), in1=H(T1), op=ALU.mult),
                  reads=[HK[KK[ct]], HK[T1]], writes=[HK[KK[ct]]])
            P.dve(lambda e, ct=ct: e.tensor_scalar(out=H(T1), in0=H(AA[ct]), scalar1=cR[:, 13 + ct:14 + ct],
                                                   scalar2=cR[:, 21 + ct:22 + ct], op0=ALU.mult, op1=ALU.add),
                  reads=[HK[AA[ct]], 'cR'], writes=[HK[T1]])
            P.dve(lambda e, ct=ct: e.tensor_tensor(out=H(K_[ct]), in0=H(K_[ct]), in1=H(T1), op=ALU.mult),
                  reads=[HK[K_[ct]], HK[T1]], writes=[HK[K_[ct]]])
            P.dve(lambda e, ct=ct: e.scalar_tensor_tensor(out=H(T1), in0=H(R_[ct]), scalar=cR[:, 15 + ct:16 + ct],
                                                          in1=H(K_[ct]), op0=ALU.mult, op1=ALU.mult),
                  reads=[HK[R_[ct]], HK[K_[ct]], 'cR'], writes=[HK[T1]])
            P.pe(lambda e: e.matmul(out=B[3][:, 0:TBr], lhsT=blk64, rhs=H(T1), start=True, stop=True),
                 reads=['c_f32', HK[T1]], writes=['B3'])
            P.dve(lambda e, ct=ct: e.scalar_tensor_tensor(out=H(BON[ct]), in0=B[3][:, 0:TBr], scalar=64.0,
                                                          in1=H(V_[ct]), op0=ALU.mult, op1=ALU.mult),
                  reads=['B3', HK[V_[ct]]], writes=[HK[BON[ct]]])
            for c in range(NCr):
                cs = slice(c * 64, (c + 1) * 64)
                P.dve(lambda e, ct=ct, cs=cs: e.tensor_tensor_scan(out=Ht[LC[ct]][:, cs], data0=ones64[:],
                                                                   data1=Ht[LW[ct]][:, cs], initial=0.0,
                                                                   op0=ALU.mult, op1=ALU.add),
                      reads=['ones64', HK[LW[ct]]], writes=[HK[LC[ct]]])
            P.act(lambda e, ct=ct: e.activation(out=H(WI[ct]), in_=H(LC[ct]), func=AF.Exp), reads=[HK[LC[ct]]],
                  writes=[HK[WI[ct]]])
            P.act(lambda e, ct=ct: e.activation(out=H(WN[ct]), in_=H(LC[ct]), func=AF.Exp, scale=-1.0),
                  reads=[HK[LC[ct]]], writes=[HK[WN[ct]]])
            P.dve(lambda e, ct=ct: e.tensor_tensor(out=H(WE[ct]), in0=H(LC[ct]), in1=H(LW[ct]), op=ALU.subtract),
                  reads=[HK[LC[ct]], HK[LW[ct]]], writes=[HK[WE[ct]]])
            P.act(lambda e, ct=ct: e.activation(out=H(WE[ct]), in_=H(WE[ct]), func=AF.Exp), reads=[HK[WE[ct]]],
                  writes=[HK[WE[ct]]])
            P.dve(lambda e, ct=ct: e.tensor_tensor(out=H(RT_[ct]), in0=H(R_[ct]), in1=H(WI[ct]), op=ALU.mult),
                  reads=[HK[R_[ct]], HK[WI[ct]]], writes=[HK[RT_[ct]]])
            P.dve(lambda e, ct=ct: e.scalar_tensor_tensor(out=H(AT_[ct]), in0=H(KK[ct]), scalar=-1.0, in1=H(WE[ct]),
                                                          op0=ALU.mult, op1=ALU.mult),
                  reads=[HK[KK[ct]], HK[WE[ct]]], writes=[HK[AT_[ct]]])
            P.pool(lambda e, ct=ct: e.tensor_tensor(out=H(BT_[ct]), in0=H(KK[ct]), in1=H(AA[ct]), op=ALU.mult),
                   reads=[HK[KK[ct]], HK[AA[ct]]], writes=[HK[BT_[ct]]])
            P.pool(lambda e, ct=ct: e.tensor_tensor(out=H(BT_[ct]), in0=H(BT_[ct]), in1=H(WN[ct]), op=ALU.mult),
                   reads=[HK[BT_[ct]], HK[WN[ct]]], writes=[HK[BT_[ct]]])
            P.pool(lambda e, ct=ct: e.tensor_tensor(out=H(KT_[ct]), in0=H(K_[ct]), in1=H(WN[ct]), op=ALU.mult),
                   reads=[HK[K_[ct]], HK[WN[ct]]], writes=[HK[KT_[ct]]])
        for b2 in range(TBr // 128):
            sl = slice(b2 * 128, (b2 + 1) * 128)
            for (srcs, dst, dk, bk) in ((BT_, btok, 'btok', 0), (KT_, ktok2, 'ktok2', 1), (V_, vtok2, 'vtok2', 2)):
                for ct in range(2):
                    P.pe(lambda e, srcs=srcs, ct=ct, sl=sl, bk=bk: e.transpose(
                        out=B[bk][:, ct * 128:(ct + 1) * 128], in_=Ht[srcs[ct]][:, sl], identity=identf),
                        reads=[HK[srcs[ct]], 'c_f32'], writes=['B%d' % bk])
                P.act(lambda e, dst=dst, b2=b2, bk=bk: e.copy(out=dst[:, b2, :], in_=B[bk][:, 0:256]),
                      reads=['B%d' % bk], writes=[dk])
        def gram(bank, lt, rt, dst, dkey, mask):
            for c in range(NCr):
                b2, pb = c // 2, (c % 2) * 64
                cs = slice(c * 64, (c + 1) * 64)
                for hd in range(4):
                    ct, hs = hd // 2, slice((hd % 2) * 64, (hd % 2) * 64 + 64)
                    col = b2 * 256 + hd * 64
                    P.pe(lambda e, ct=ct, hs=hs, cs=cs, pb=pb, col=col: e.matmul(
                        out=B[bank][pb:pb + 64, col:col + 64], lhsT=Ht[lt[ct]][hs, cs], rhs=Ht[rt[ct]][hs, cs],
                        start=True, stop=True), reads=[HK[lt[ct]], HK[rt[ct]]], writes=['B%d' % bank])
            nb = TBr // 128
            P.dve(lambda e: e.tensor_tensor(
                out=dst[:, 0:nb * 256].rearrange("p (b c) -> p b c", b=nb),
                in0=B[bank][:, 0:nb * 256].rearrange("p (b c) -> p b c", b=nb),
                in1=mask.unsqueeze(1).to_broadcast([128, nb, 256]), op=ALU.mult),
                reads=['B%d' % bank, 'c_f32'], writes=[dkey])
        gram(0, BT_, AT_, GZ, 'gm0', mask_su)
        gram(1, AT_, BT_, GN, 'gm1', mask_sl)
        gram(2, KT_, AT_, GAK, 'gm2', mask_su)
        gram(3, KT_, RT_, GRK, 'gm3', mask_incl)
        gram(4, BT_, RT_, GRB, 'gm4', mask_incl)
        nb = TBr // 128
        NW = nb * 256
        v3 = lambda ap: ap[:, 0:NW].rearrange("p (b c) -> p b c", b=nb)
        P.dve(lambda e: e.tensor_tensor(out=v3(GP), in0=v3(GZ), in1=I4.unsqueeze(1).to_broadcast([128, nb, 256]),
                                        op=ALU.add), reads=['gm0', 'c_f32'], writes=['gm5'])
        Zc, Zt_ = GZ, GN
        Za, Zb = GX, GU
        for j in range(1, 6):
            def allblk(fn):
                for c in range(NCr):
                    b2, pb = c // 2, (c % 2) * 64
                    for hd in range(4):
                        col = b2 * 256 + hd * 64
                        fn(pb, col)
            allblk(lambda pb, col, Zc=Zc, Zt_=Zt_: P.pe(lambda e: e.matmul(
                out=B[0][pb:pb + 64, col:col + 64], lhsT=Zt_[pb:pb + 64, col:col + 64], rhs=Zc[pb:pb + 64, col:col + 64],
                start=True, stop=True), reads=[gkey(Zc),
                                               gkey(Zt_)], writes=['B0']))
            allblk(lambda pb, col, Zc=Zc, Zt_=Zt_: P.pe(lambda e: e.matmul(
                out=B[1][pb:pb + 64, col:col + 64], lhsT=Zc[pb:pb + 64, col:col + 64], rhs=Zt_[pb:pb + 64, col:col + 64],
                start=True, stop=True), reads=[gkey(Zc),
                                               gkey(Zt_)], writes=['B1']))
            nZ, nZt = (Za, Zb) if j % 2 == 1 else (GZ, GN)
            kZ = gkey(nZ)
            kZt = gkey(nZt)
            P.act(lambda e, nZ=nZ: e.copy(out=nZ[:, 0:NW], in_=B[0][:, 0:NW]), reads=['B0'], writes=[kZ])
            P.act(lambda e, nZt=nZt: e.copy(out=nZt[:, 0:NW], in_=B[1][:, 0:NW]), reads=['B1'], writes=[kZt])
            Zc, Zt_ = nZ, nZt
            allblk(lambda pb, col, Zt_=Zt_, kZt=kZt: P.pe(lambda e: e.matmul(
                out=B[2][pb:pb + 64, col:col + 64], lhsT=Zt_[pb:pb + 64, col:col + 64], rhs=GP[pb:pb + 64, col:col + 64],
                start=True, stop=True), reads=[kZt, 'gm5'], writes=['B2']))
            P.dve(lambda e: e.tensor_tensor(out=GP[:, 0:NW], in0=GP[:, 0:NW], in1=B[2][:, 0:NW], op=ALU.add),
                  reads=['gm5', 'B2'], writes=['gm5'])
        for c in range(NCr):
            b2, pb = c // 2, (c % 2) * 64
            cs = slice(c * 64, (c + 1) * 64)
            ps_ = slice(pb, pb + 64)
            for hd in range(4):
                ct, hs = hd // 2, slice((hd % 2) * 64, (hd % 2) * 64 + 64)
                col = b2 * 256 + hd * 64
                P.pe(lambda e, ps_=ps_, ct=ct, hs=hs, cs=cs, hd=hd: e.matmul(
                    out=B[3][ps_, hd * 64:(hd + 1) * 64], lhsT=Ht[AT_[ct]][hs, cs], rhs=stR[hs, ct, :],
                    start=True, stop=False), reads=[HK[AT_[ct]], 'stR'], writes=['B3'])
                P.pe(lambda e, ps_=ps_, hd=hd, col=col, b2=b2: e.matmul(
                    out=B[3][ps_, hd * 64:(hd + 1) * 64], lhsT=GAK[ps_, col:col + 64],
                    rhs=vtok2[ps_, b2, hd * 64:(hd + 1) * 64], start=False, stop=True),
                    reads=['gm2', 'vtok2'], writes=['B3'])
            P.act(lambda e, ps_=ps_: e.copy(out=GX[ps_, 0:256], in_=B[3][ps_, 0:256]), reads=['B3'], writes=['gm6'])
            for hd in range(4):
                col = b2 * 256 + hd * 64
                P.pe(lambda e, ps_=ps_, hd=hd, col=col: e.matmul(
                    out=B[4][ps_, hd * 64:(hd + 1) * 64], lhsT=GP[ps_, col:col + 64],
                    rhs=GX[ps_, hd * 64:(hd + 1) * 64], start=True, stop=True), reads=['gm5', 'gm6'], writes=['B4'])
            P.act(lambda e, ps_=ps_: e.copy(out=GU[ps_, 0:256], in_=B[4][ps_, 0:256]), reads=['B4'], writes=['gm7'])
            for hd in range(4):
                ct, hs = hd // 2, slice((hd % 2) * 64, (hd % 2) * 64 + 64)
                col = b2 * 256 + hd * 64
                hcol = slice(hd * 64, (hd + 1) * 64)
                P.pe(lambda e, ps_=ps_, ct=ct, hs=hs, cs=cs: e.matmul(
                    out=B[5][hs, ct * 64:(ct + 1) * 64], lhsT=stR[hs, ct, :], rhs=Ht[RT_[ct]][hs, cs],
                    start=True, stop=False), reads=['stR', HK[RT_[ct]]], writes=['B5'])
                P.pe(lambda e, ps_=ps_, ct=ct, hs=hs, col=col, hcol=hcol: e.matmul(
                    out=B[5][hs, ct * 64:(ct + 1) * 64], lhsT=GU[ps_, hcol], rhs=GRB[ps_, col:col + 64],
                    start=False, stop=False), reads=['gm7', 'gm4'], writes=['B5'])
                P.pe(lambda e, ps_=ps_, ct=ct, hs=hs, col=col, hcol=hcol, b2=b2: e.matmul(
                    out=B[5][hs, ct * 64:(ct + 1) * 64], lhsT=vtok2[ps_, b2, hcol], rhs=GRK[ps_, col:col + 64],
                    start=False, stop=True), reads=['vtok2', 'gm3'], writes=['B5'])
                P.pe(lambda e, ps_=ps_, ct=ct, hs=hs, hcol=hcol, b2=b2: e.matmul(
                    out=B[2][hs, ct * 64:(ct + 1) * 64], lhsT=btok[ps_, b2, hcol], rhs=GU[ps_, hcol],
                    start=True, stop=False), reads=['btok', 'gm7'], writes=['B2'])
                P.pe(lambda e, ps_=ps_, ct=ct, hs=hs, hcol=hcol, b2=b2: e.matmul(
                    out=B[2][hs, ct * 64:(ct + 1) * 64], lhsT=ktok2[ps_, b2, hcol], rhs=vtok2[ps_, b2, hcol],
                    start=False, stop=True), reads=['ktok2', 'vtok2'], writes=['B2'])
            for ct in range(2):
                P.act(lambda e, ps_=ps_, ct=ct, cs=cs: e.copy(out=Ht[YT[ct]][:, cs], in_=B[5][:, ct * 64:(ct + 1) * 64]),
                      reads=['B5'], writes=[HK[YT[ct]]])
                P.dve(lambda e, ps_=ps_, ct=ct: e.tensor_tensor(out=stR[:, ct, :], in0=stR[:, ct, :],
                                                       in1=B[2][:, ct * 64:(ct + 1) * 64], op=ALU.add),
                      reads=['stR', 'B2'], writes=['stR'])
                P.dve(lambda e, ps_=ps_, ct=ct, c=c: e.tensor_scalar(out=stR[:, ct, :], in0=stR[:, ct, :],
                                                            scalar1=Ht[WI[ct]][:, c * 64 + 63:c * 64 + 64],
                                                            scalar2=None, op0=ALU.mult),
                      reads=['stR', HK[WI[ct]]], writes=['stR'])
        if blk == 0 and DBG_VAR == 7:
            for nm, idx in (('lw', LW[0]), ('aa', AA[0]), ('kk', KK[0]), ('kmod', K_[0]), ('gg', GG[0]), ('bon', BON[0]),
                            ('y', YT[0]), ('rt', RT_[0]), ('at', AT_[0]), ('bt', BT_[0]), ('kt', KT_[0]), ('r', R_[0]),
                            ('v', V_[0])):
                dbgdump(nm, H(idx), HK[idx], [128, TBr])
            for nm, gi in (('gz', 0), ('gn', 1), ('gak', 2), ('grk', 3), ('grb', 4), ('gp', 5), ('gx', 6), ('gu', 7)):
                dbgdump(nm, GMS[gi], GKEY[gi], [128, 512])
            dbgdump('vtok', vtok2[:, 0, :], 'vtok2', [128, 256])
            dbgdump('btok', btok[:, 0, :], 'btok', [128, 256])
        for ct in range(2):
            P.pe(lambda e, ct=ct: e.matmul(out=B[0][:, 0:TBr], lhsT=blk64, rhs=H(YT[ct]), start=True, stop=True),
                 reads=['c_f32', HK[YT[ct]]], writes=['B0'])
            P.act(lambda e, ct=ct: e.activation(out=H(T1), in_=H(YT[ct]), func=AF.Square), reads=[HK[YT[ct]]],
                  writes=[HK[T1]])
            P.pe(lambda e: e.matmul(out=B[1][:, 0:TBr], lhsT=blk64, rhs=H(T1), start=True, stop=True),
                 reads=['c_f32', HK[T1]], writes=['B1'])
            P.act(lambda e: e.copy(out=H(T2), in_=B[0][:, 0:TBr]), reads=['B0'], writes=[HK[T2]])
            P.dve(lambda e: e.tensor_tensor(out=H(T1), in0=H(T2), in1=H(T2), op=ALU.mult), reads=[HK[T2]],
                  writes=[HK[T1]])
            P.dve(lambda e: e.tensor_tensor(out=H(T1), in0=B[1][:, 0:TBr], in1=H(T1), op=ALU.subtract),
                  reads=['B1', HK[T1]], writes=[HK[T1]])
            P.dve(lambda e: e.tensor_scalar(out=H(T1), in0=H(T1), scalar1=64e-5, scalar2=None, op0=ALU.add),
                  reads=[HK[T1]], writes=[HK[T1]])
            P.act(lambda e: e.sqrt(out=H(T1), in_=H(T1)), reads=[HK[T1]], writes=[HK[T1]])
            P.dve(lambda e: e.reciprocal(out=H(T1), in_=H(T1)), reads=[HK[T1]], writes=[HK[T1]])
            P.dve(lambda e, ct=ct: e.tensor_tensor(out=H(YT[ct]), in0=H(YT[ct]), in1=H(T2), op=ALU.subtract),
                  reads=[HK[YT[ct]], HK[T2]], writes=[HK[YT[ct]]])
            P.dve(lambda e, ct=ct: e.tensor_tensor(out=H(YT[ct]), in0=H(YT[ct]), in1=H(T1), op=ALU.mult),
                  reads=[HK[YT[ct]], HK[T1]], writes=[HK[YT[ct]]])
            P.act(lambda e, ct=ct: e.activation(out=H(YT[ct]), in_=H(YT[ct]), func=AF.Identity,
                                                scale=cR[:, 17 + ct:18 + ct], bias=cR[:, 19 + ct:20 + ct]),
                  reads=[HK[YT[ct]], 'cR'], writes=[HK[YT[ct]]])
            P.dve(lambda e, ct=ct: e.tensor_tensor(out=H(YT[ct]), in0=H(YT[ct]), in1=H(BON[ct]), op=ALU.add),
                  reads=[HK[YT[ct]], HK[BON[ct]]], writes=[HK[YT[ct]]])
            P.dve(lambda e, ct=ct: e.tensor_tensor(out=H(YT[ct]), in0=H(YT[ct]), in1=H(GG[ct]), op=ALU.mult),
                  reads=[HK[YT[ct]], HK[GG[ct]]], writes=[HK[YT[ct]]])
            P.dma(lambda e, ct=ct: e.dma_start(out=mixT[512 + ct * 128:512 + (ct + 1) * 128, c0:c0 + TBr],
                                               in_=H(YT[ct])), reads=[HK[YT[ct]]], writes=['mixT'])

    def rw_setup(l):
        P.dma(lambda e: e.dma_start(out=cR[:], in_=colsR_in[l]), writes=['cR'])
        P.dma(lambda e: e.dma_start(out=lora[0:32, :], in_=rw_w2[l]), writes=['lora'])
        P.dma(lambda e: e.dma_start(out=lora[32:64, :], in_=rw_a2[l]), writes=['lora'])
        P.dma(lambda e: e.dma_start(out=lora[64:128, :], in_=rw_g2[l]), writes=['lora'])
        for ct in range(2):
            P.dve(lambda e, ct=ct: e.tensor_scalar(out=cR[:, 21 + ct:22 + ct], in0=cR[:, 13 + ct:14 + ct],
                                                   scalar1=-1.0, scalar2=1.0, op0=ALU.mult, op1=ALU.add),
                  reads=['cR'], writes=['cR'])

    def phase_B(l, which=B_WHICH):
        fence()
        P.dma(lambda e: e.dma_start(out=cB[:], in_=colsB[l]), writes=['cB'])
        P.dma(lambda e: e.dma_start(out=wup[:], in_=gla_w_up[l]), writes=['wup'])
        P.dve(lambda e: e.tensor_scalar(out=cB[:, 71:72], in0=cB[:, 68:69], scalar1=-1.0, scalar2=None, op0=ALU.mult),
              reads=['cB'], writes=['cB'])
        P.dve(lambda e: e.tensor_scalar(out=cB[:, 73:74], in0=cB[:, 72:73], scalar1=-1.0, scalar2=None, op0=ALU.mult),
              reads=['cB'], writes=['cB'])
        if 's5' in which:
            s5_setup(l)
        if 'rw' in which:
            rw_setup(l)
            for blk in range(T // TBr):
                rw_block(l, blk)
        for blk in range(T // TBk):
            fence()
            if 's5' in which:
                s5_block(l, blk)
            if 'conv' in which:
                conv_block(l, blk)
            if 'gla' in which:
                gla_block(l, blk)

    for l in range(L):
        src = x if l == 0 else out
        if 'A' in phases:
            phase_A(l, src)
        if 'B' in phases:
            phase_B(l)
        if 'O' in phases:
            phase_outproj(l, src)
        if 'X' in phases:
            phase_xattn(l)
        if 'M' in phases:
            phase_moe(l)
    if 'F' in phases:
        phase_final()

    P.emit()
    return nc, es


def make_consts():
    c = np.zeros((128, 2112), np.float32)
    c[:, 1024:1536] = np.arange(1, 513, dtype=np.float32)[None, :]
    c[0:64, 1536] = 1.0
    c[64:128, 1537] = 1.0
    c[:, 0:128] = np.eye(128, dtype=np.float32)
    p = np.arange(128)[:, None] % 64
    t = np.arange(64)[None, :]
    incl = (p <= t).astype(np.float32)
    strict = (p < t).astype(np.float32)
    for h in range(4):
        c[:, 128 + h * 64:128 + (h + 1) * 64] = incl
        c[:, 512 + h * 64:512 + (h + 1) * 64] = strict
    for h in range(4):
        c[:, 1600 + h * 64:1600 + (h + 1) * 64] = (p > t).astype(np.float32)
        c[:, 1856 + h * 64:1856 + (h + 1) * 64] = (p == t).astype(np.float32)
    q = np.arange(128)
    c[:, 384:512] = (q[:, None] // 64 == q[None, :] // 64).astype(np.float32) / 64.0
    return c


ALL_PHASES = ('A', 'B', 'O', 'X', 'M', 'F')
B_WHICH = ('s5', 'conv', 'gla', 'rw')
DBG_STOP = 99
DBG_VAR = 0


def make_colsB(inputs, L):
    g = lambda k: np.asarray(inputs[k], dtype=np.float32)
    c = np.zeros((L, 128, 80), np.float32)
    cw = g("conv_w")
    for ct in range(2):
        c[:, :, ct * 31:(ct + 1) * 31] = cw[:, :, ct * 128:(ct + 1) * 128].transpose(0, 2, 1)
        c[:, :, 62 + ct] = g("conv_b")[:, ct * 128:(ct + 1) * 128]
        c[:, :, 64 + ct] = g("conv_ln_g")[:, ct * 128:(ct + 1) * 128]
        c[:, :, 66 + ct] = g("conv_ln_b")[:, ct * 128:(ct + 1) * 128]
        c[:, :, 69 + ct] = g("gla_norm_g")[:, ct * 128:(ct + 1) * 128]
    for ct in range(2):
        c[:, :, 74 + ct] = g("s5_d")[:, ct * 128:(ct + 1) * 128]
        c[:, :, 76 + ct] = g("s5_glu_b")[:, ct * 128:(ct + 1) * 128]
    c[:, 0:64, 68] = g("gla_b_up")[:, 0:64]
    c[:, 0:64, 72] = g("gla_b_up")[:, 64:128]
    return c


def make_colsR(inputs, L):
    g = lambda k: np.asarray(inputs[k], dtype=np.float32)
    c = np.zeros((L, 128, 24), np.float32)
    c[:, :, 0:7] = g("rw_mu").reshape(L, 7, 128).transpose(0, 2, 1)
    for i, k in enumerate(("rw_w0", "rw_a0", "rw_k_k", "rw_k_a", "rw_r_k", "rw_ln_g", "rw_ln_b")):
        c[:, :, 7 + 2 * i:9 + 2 * i] = g(k).reshape(L, 2, 128).transpose(0, 2, 1)
    return c


def make_s5(inputs, L):
    g = lambda k: np.asarray(inputs[k], dtype=np.float32)
    pl = lambda a: a.reshape(L, 8, 2, 64).transpose(0, 2, 3, 1).reshape(L, 128, 8)
    ldt = np.broadcast_to(g("s5_log_dt")[:, :, None], (L, 16, 64))
    s5p = np.concatenate([pl(g("s5_lam_re")), pl(g("s5_lam_im")), pl(ldt)], axis=-1)
    Bm = np.zeros((L, 128, 16, 128), np.float32)
    for ri, key in enumerate(("s5_b_re", "s5_b_im")):
        b = g(key)
        for j in range(8):
            for two in range(2):
                r0 = (j % 4) * 32 + two * 16
                Bm[:, r0:r0 + 16, ri * 8 + j, two * 64:(two + 1) * 64] = b[:, 2 * j + two].transpose(0, 2, 1)
    C = np.zeros((L, 128, 2, 8, 16), np.float32)
    for ri, key in enumerate(("s5_c_re", "s5_c_im")):
        c = g(key)
        C[:, :, ri] = c.reshape(L, 8, 2, 16, 64).transpose(0, 2, 4, 1, 3).reshape(L, 128, 8, 16)
    return {"s5p": np.ascontiguousarray(s5p), "s5B": Bm, "s5C": C}


def prep_inputs(inputs, b, L):
    f = lambda k: np.ascontiguousarray(np.asarray(inputs[k], dtype=np.float32))
    m = {
        "x": np.ascontiguousarray(np.asarray(inputs["x"], np.float32)[b]),
        "mem": np.ascontiguousarray(np.asarray(inputs["mem"], np.float32)[b]),
        "consts": make_consts(),
        "norm_mix_g": f("norm_mix_g"), "w_in": f("w_in"), "w_out": f("w_out"),
        "beta_c": np.ascontiguousarray(f("mix_beta").reshape(L, KT, 128).transpose(0, 2, 1)),
        "norm_xattn_g": f("norm_xattn_g"), "norm_mem_g": f("norm_mem_g"),
        "xa_wq": f("xa_wq"), "xa_wk": f("xa_wk"), "xa_wv": f("xa_wv"), "xa_wo": f("xa_wo"),
        "norm_ffn_g": f("norm_ffn_g"),
        "moe_rw": np.ascontiguousarray(np.concatenate([f("moe_group_w"), f("moe_expert_w")], axis=-1)),
        "moe_rb": np.ascontiguousarray(np.concatenate([f("moe_group_b"), f("moe_expert_b")], axis=-1)),
        "moe_w_gate": f("moe_w_gate"), "moe_w_up": f("moe_w_up"), "moe_w_down": f("moe_w_down"),
        "norm_final_g": f("norm_final_g").reshape(1, D),
        "colsB": make_colsB(inputs, L), "gla_w_up": f("gla_w_up"),
        **make_s5(inputs, L), "s5_glu_w": f("s5_glu_w"),
        "colsR": make_colsR(inputs, L), "rw_w2": f("rw_w2"), "rw_a2": f("rw_a2"), "rw_g2": f("rw_g2"),
    }
    return m


def kernel(**inputs):
    x = np.asarray(inputs["x"])
    Bsz, T, _ = x.shape
    L = np.asarray(inputs["w_in"]).shape[0]
    nc, es = build(T, L, dbg=False, phases=ALL_PHASES)
    n_cores = 8
    maps = [prep_inputs(inputs, c % Bsz, L) for c in range(Bsz)]
    in_maps = [maps[c % Bsz] for c in range(n_cores)]
    res = run_bass_kernel_spmd(nc, in_maps, core_ids=list(range(n_cores)))
    outs = [np.asarray(res.results[c]["out"], dtype=np.float32) for c in range(Bsz)]
    return np.stack(outs, axis=0)
```

```python
import numpy as np
from contextlib import ExitStack
import concourse.bass as bass
import concourse.mybir as mybir
from concourse.bass_utils import run_bass_kernel_spmd

F32 = mybir.dt.float32
BF16 = mybir.dt.bfloat16
I32 = mybir.dt.int32
ALU = mybir.AluOpType
AF = mybir.ActivationFunctionType
AX = mybir.AxisListType

D = 1024
KT = D // 128
IN_COLS = 2448
NDMA = 16


class Prog:
    ENG = ['pe', 'act', 'dve', 'pool', 'sp']

    def __init__(self, nc, es):
        self.nc = nc
        self.es = es
        self.ops = []
        self.last_w = {}
        self.readers = {}
        self.eng_cnt = {e: 0 for e in self.ENG}
        self.dma_cnt = [0] * NDMA
        self.dma_rr = 0

    def capture(self):
        self._cap = []
        return self._cap

    def end_capture(self):
        self._cap = None

    def replay_interleaved(self, lists):
        n = max(len(x) for x in lists)
        for i in range(n):
            for x in lists:
                if i < len(x):
                    self.op(*x[i])

    def op(self, eng, fn, reads=(), writes=(), dma=False):
        if getattr(self, '_cap', None) is not None:
            self._cap.append((eng, fn, tuple(reads), tuple(writes), dma))
            return None
        deps = set()
        for k in reads:
            if k in self.last_w:
                deps.add(self.last_w[k])
        for k in writes:
            if k in self.last_w:
                deps.add(self.last_w[k])
            deps.update(self.readers.get(k, ()))
        if dma:
            s = self.dma_rr
            self.dma_rr = (s + 1) % NDMA
            prev = self.dma_cnt[s]
            self.dma_cnt[s] += 16
            if prev > 0:
                deps.add(('d%d' % s, prev))
            tok = ('d%d' % s, prev + 16)
        else:
            self.eng_cnt[eng] += 1
            tok = (eng, self.eng_cnt[eng])
        if eng == 'pe':
            deps = {d for d in deps if d[0] != 'pe'}
        self.ops.append((eng, fn, deps, tok, dma))
        for k in writes:
            self.last_w[k] = tok
            self.readers[k] = []
        for k in reads:
            if k not in writes:
                self.readers.setdefault(k, []).append(tok)
        return tok

    def pe(self, fn, reads=(), writes=()):
        return self.op('pe', fn, reads, writes)

    def act(self, fn, reads=(), writes=()):
        return self.op('act', fn, reads, writes)

    def dve(self, fn, reads=(), writes=()):
        return self.op('dve', fn, reads, writes)

    def pool(self, fn, reads=(), writes=()):
        return self.op('pool', fn, reads, writes)

    def dma(self, fn, reads=(), writes=(), q='sp'):
        return self.op(q, fn, reads, writes, dma=True)

    def emit(self):
        nc = self.nc
        sems = {}
        for e in self.ENG:
            sems[e] = self.es.enter_context(nc.semaphore('s_' + e))
        for i in range(NDMA):
            sems['d%d' % i] = self.es.enter_context(nc.semaphore('s_d%d' % i))
        final_waits = [('d%d' % i, self.dma_cnt[i]) for i in range(NDMA) if self.dma_cnt[i] > 0]
        final_waits += [(e, self.eng_cnt[e]) for e in self.ENG if self.eng_cnt[e] > 0]
        by_eng = {e: [o for o in self.ops if o[0] == e] for e in self.ENG}

        class PEProxy:
            def __init__(self, eng, waited):
                self.eng, self.waited, self.last, self.before = eng, waited, None, 0

            def _sync(self, ap):
                rows = ap.partition_size()
                rg = (ap.base_partition(), 32 if rows <= 32 else (64 if rows <= 64 else 128))
                if self.last is not None and rg != self.last and self.before > 0 \
                        and self.waited.get('pe', 0) < self.before:
                    self.eng.wait_ge(sems['pe'], self.before)
                    self.waited['pe'] = self.before
                self.last = rg

            def matmul(self, **kw):
                self._sync(kw['lhsT'])
                return self.eng.matmul(**kw)

            def transpose(self, **kw):
                self._sync(kw['in_'])
                return self.eng.transpose(**kw)

        def run(eng_name, eng):
            waited = {}
            proxy = PEProxy(eng, waited) if eng_name == 'pe' else None
            for (_, fn, deps, tok, dma) in by_eng[eng_name]:
                for (s, v) in sorted(deps):
                    if waited.get(s, 0) < v:
                        eng.wait_ge(sems[s], v)
                        waited[s] = v
                if eng_name == 'pe':
                    proxy.before = tok[1] - 1
                    ins = fn(proxy)
                else:
                    ins = fn(eng)
                ins.then_inc(sems[tok[0]], 16 if dma else 1)
            if eng_name == 'sp':
                for (s, v) in final_waits:
                    if waited.get(s, 0) < v:
                        eng.wait_ge(sems[s], v)

        with nc.Block() as block:
            @block.tensor
            def _(e):
                run('pe', e)

            @block.scalar
            def _(e):
                run('act', e)

            @block.vector
            def _(e):
                run('dve', e)

            @block.gpsimd
            def _(e):
                run('pool', e)

            @block.sync
            def _(e):
                run('sp', e)


class Ctx:
    pass


def build(T, L, dbg=False, phases=('A',)):
    nc = bass.Bass("TRN2", target_bir_lowering=False)
    es = ExitStack()
    P = Prog(nc, es)
    NT = T // 128

    def din(name, shape, dt=F32):
        return nc.dram_tensor(name, list(shape), dt, kind="ExternalInput").ap()

    def dscr(name, shape, dt=F32):
        kind = "ExternalOutput" if dbg else "Internal"
        return nc.dram_tensor(name, list(shape), dt, kind=kind).ap()

    def sb(name, shape, dt=F32):
        return es.enter_context(nc.sbuf_tensor(name, list(shape), dt))

    def ps(name, shape, dt=F32):
        return es.enter_context(nc.psum_tensor(name, list(shape), dt))

    x = din("x", [T, D])
    consts = din("consts", [128, 2112])
    norm_mix_g = din("norm_mix_g", [L, D])
    w_in = din("w_in", [L, D, IN_COLS])
    out = nc.dram_tensor("out", [T, D], F32, kind="ExternalOutput").ap()
    pT = dscr("pT", [IN_COLS, T])
    mixT = (din if 'Ctest' in phases else dscr)("mixT", [D, T])
    memx = din("mem", [256, D])
    w_out = din("w_out", [L, D, D])
    beta_c = din("beta_c", [L, 128, KT])
    norm_xattn_g = din("norm_xattn_g", [L, D])
    norm_mem_g = din("norm_mem_g", [L, D])
    xa_wq = din("xa_wq", [L, D, D]); xa_wk = din("xa_wk", [L, D, D])
    xa_wv = din("xa_wv", [L, D, D]); xa_wo = din("xa_wo", [L, D, D])
    norm_ffn_g = din("norm_ffn_g", [L, D])
    moe_rw = din("moe_rw", [L, D, 36])
    moe_rb = din("moe_rb", [L, 36])
    moe_wg = din("moe_w_gate", [L, 32, D, 512]); moe_wu = din("moe_w_up", [L, 32, D, 512])
    moe_wd = din("moe_w_down", [L, 32, 512, D])
    norm_final_g = din("norm_final_g", [1, D])

    c_f32 = sb("c_f32", [128, 2112])
    ident = sb("ident", [128, 128], BF16)
    ones_bf = sb("ones_bf", [128, 128], BF16)
    P.dma(lambda e: e.dma_start(out=c_f32[:], in_=consts[:, :]), writes=['c_f32'])
    P.act(lambda e: e.copy(out=ident[:], in_=c_f32[:, 0:128]), reads=['c_f32'], writes=['ident'])
    P.dve(lambda e: e.memset(ones_bf[:], 1.0), writes=['ones_bf'])
    identf = c_f32[:, 0:128]

    hb = [sb("hb%d" % i, [128, D]) for i in range(2)]
    ss = [sb("ss%d" % i, [128, 1]) for i in range(2)]
    rstd = [sb("rstd%d" % i, [128, 1]) for i in range(2)]
    xn = [sb("xn%d" % i, [128, D], BF16) for i in range(2)]
    xnf = [sb("xnf%d" % i, [128, D]) for i in range(2)]
    gb = sb("gb", [128, D])
    NTC = min(4, NT)
    TC = NTC * 128
    XN = sb("XN", [128, max(2 * KT * TC, 8192)], BF16)
    xnT = [XN[:, i * KT * TC:(i + 1) * KT * TC].rearrange("p (k t) -> p k t", k=KT) for i in range(2)]
    psT = [ps("psT%d" % i, [128, KT, 128], BF16) for i in range(2)]
    B = [ps("B%d" % i, [128, 512]) for i in range(6)]
    wst = [sb("wst%d" % i, [128, 2560]) for i in range(2)]
    WB = sb("WB", [128, 24576], BF16)
    po = [sb("po%d" % i, [128, 512]) for i in range(2)]
    win_bf = WB[:, 0:KT * IN_COLS].rearrange("p (k n) -> p k n", k=KT)

    cnt = {'tile': 0, 'chunk': 0, 'mm': 0, 'w': 0}

    def load_gb(vec_row):
        P.dma(lambda e: e.dma_start(out=gb[:], in_=vec_row.partition_broadcast(128)), writes=['gb'])

    def rms_to_T(src_rows, dstT, dst_key, tcol, src_key=None, dstTf=None, dstf_key=None):
        i = cnt['tile'] % 2
        cnt['tile'] += 1
        P.dma(lambda e: e.dma_start(out=hb[i][:], in_=src_rows), reads=[src_key] if src_key else [],
              writes=['hb%d' % i])
        P.act(lambda e: e.activation(out=xn[i][:], in_=hb[i][:], func=AF.Square, accum_out=ss[i][:]),
              reads=['hb%d' % i], writes=['xn%d' % i, 'ss%d' % i])
        P.dve(lambda e: e.tensor_scalar(out=rstd[i][:], in0=ss[i][:], scalar1=1.0 / D, scalar2=1e-6,
                                        op0=ALU.mult, op1=ALU.add), reads=['ss%d' % i], writes=['rstd%d' % i])
        P.act(lambda e: e.sqrt(out=rstd[i][:], in_=rstd[i][:]), reads=['rstd%d' % i], writes=['rstd%d' % i])
        P.dve(lambda e: e.reciprocal(out=rstd[i][:], in_=rstd[i][:]), reads=['rstd%d' % i], writes=['rstd%d' % i])
        P.dve(lambda e: e.scalar_tensor_tensor(out=xn[i][:], in0=hb[i][:], scalar=rstd[i][:, 0:1], in1=gb[:],
                                               op0=ALU.mult, op1=ALU.mult),
              reads=['hb%d' % i, 'rstd%d' % i, 'gb'], writes=['xn%d' % i])
        for kt in range(KT):
            P.pe(lambda e, kt=kt: e.transpose(out=psT[i][:, kt, :], in_=xn[i][:, kt * 128:(kt + 1) * 128],
                                              identity=ident[:]),
                 reads=['xn%d' % i, 'ident'], writes=['psT%d' % i])
        P.act(lambda e: e.copy(out=dstT[:, :, tcol:tcol + 128], in_=psT[i][:]),
              reads=['psT%d' % i], writes=[dst_key])
        if dstTf is not None:
            P.dve(lambda e: e.scalar_tensor_tensor(out=xnf[i][:], in0=hb[i][:], scalar=rstd[i][:, 0:1], in1=gb[:],
                                                    op0=ALU.mult, op1=ALU.mult),
                   reads=['hb%d' % i, 'rstd%d' % i, 'gb'], writes=['xnf%d' % i])
            for half in range(2):
                bk = 4 + half
                for q in range(4):
                    kt = half * 4 + q
                    P.pe(lambda e, kt=kt, q=q, bk=bk: e.transpose(out=B[bk][:, q * 128:(q + 1) * 128],
                                                                 in_=xnf[i][:, kt * 128:(kt + 1) * 128],
                                                                 identity=identf),
                         reads=['xnf%d' % i, 'c_f32'], writes=['B%d' % bk])
                P.act(lambda e, half=half, bk=bk: e.copy(
                    out=dstTf[:, half * 4:half * 4 + 4, 0:128],
                    in_=B[bk][:].rearrange("p (q t) -> p q t", q=4)),
                    reads=['B%d' % bk], writes=[dstf_key])

    def load_w_bf(dst3, dst_key, src2d, nk, ncols, scale_col=None, scale_key=None):
        if scale_col is None:
            step = max(1, nk // 2)
            for k in range(0, nk, step):
                n = min(step, nk - k)
                P.dma(lambda e, k=k, n=n: e.dma_start(
                    out=dst3[:, k:k + n, 0:ncols],
                    in_=src2d[k * 128:(k + n) * 128, :].rearrange("(k p) n -> p k n", p=128)),
                    writes=[dst_key], q='pool')
            return
        rows_per = max(1, 2560 // ncols)
        k = 0
        while k < nk:
            n = min(rows_per, nk - k)
            j = cnt['w'] % 2
            cnt['w'] += 1
            st = wst[j][:, 0:n * ncols].rearrange("p (k n) -> p k n", k=n)
            P.dma(lambda e, st=st, k=k, n=n: e.dma_start(
                out=st, in_=src2d[k * 128:(k + n) * 128, :].rearrange("(k p) n -> p k n", p=128)),
                writes=['wst%d' % j], q='sp')
            for kk in range(n):
                P.pool(lambda e, st=st, k=k, kk=kk: e.tensor_scalar(
                    out=dst3[:, k + kk, 0:ncols], in0=st[:, kk, :], scalar1=scale_col[:, k + kk:k + kk + 1],
                    scalar2=None, op0=ALU.mult), reads=['wst%d' % j, scale_key], writes=[dst_key])
            k += n

    fz = sb("fz", [128, 1])
    ALIAS = ['ktok', 'vtok', 'ark', 'zT', 's5i', 'WB', 'EW0', 'EW1', 'xnT0', 'xnT1', 'xfT', 'mst', 'qT_bf', 'oT_bf',
             'macc', 'mxb', 'kT_bf', 'v_bf', 'mnT', 'eT_bf', 'rden', 'xfTf', 'hidT', 'sg0', 'sg1', 'btok', 'ktok2',
             'vtok2'] + ['gm%d' % i for i in range(8)]

    def fence():
        P.dve(lambda e: e.memset(fz[:], 0.0), reads=[], writes=ALIAS + ['fz'])

    def phase_A(l, src):
        fence()
        load_w_bf(win_bf, 'WB', w_in[l], KT, IN_COLS)
        load_gb(norm_mix_g[l:l + 1, :])
        NF = (IN_COLS + 127) // 128
        for c in range(T // TC):
            ci = cnt['chunk'] % 2
            cnt['chunk'] += 1
            for tt in range(NTC):
                r0 = c * TC + tt * 128
                rms_to_T(src[r0:r0 + 128, :], xnT[ci], 'xnT%d' % ci, tt * 128, src_key=('h', r0 // 128))
            for f in range(NF):
                fw = min(128, IN_COLS - f * 128)
                m = cnt['mm'] % 2
                cnt['mm'] += 1
                for kt in range(KT):
                    P.pe(lambda e, f=f, fw=fw, kt=kt, m=m, ci=ci: e.matmul(
                        out=B[m][0:fw, 0:TC], lhsT=win_bf[:, kt, f * 128:f * 128 + fw],
                        rhs=xnT[ci][:, kt, :], start=(kt == 0), stop=(kt == KT - 1)),
                        reads=['WB', 'xnT%d' % ci], writes=['B%d' % m])
                P.act(lambda e, fw=fw, m=m: e.copy(out=po[m][0:fw, 0:TC], in_=B[m][0:fw, 0:TC]),
                      reads=['B%d' % m], writes=['po%d' % m])
                P.dma(lambda e, f=f, fw=fw, m=m, c=c: e.dma_start(
                    out=pT[f * 128:f * 128 + fw, c * TC:(c + 1) * TC], in_=po[m][0:fw, 0:TC]),
                    reads=['po%d' % m], writes=['pT'])

    colv = sb("colv", [128, 64])
    R32 = sb("R32", [128, 8192])
    mst = R32[:, 0:KT * TC].rearrange("p (k t) -> p k t", k=KT)
    PX = sb("PX", [128, 6144])
    mxb = PX[:, 0:2048].bitcast(BF16)[:, 0:KT * TC].rearrange("p (k t) -> p k t", k=KT)
    hacc = [sb("hacc%d" % i, [128, D]) for i in range(2)]
    W2 = WB[:, 0:2 * KT * D].rearrange("p (w k n) -> p w k n", w=2, k=KT)

    def add_to_h(l, src, tok_tile, banks, first_src_x):
        i = cnt['tile'] % 2
        cnt['tile'] += 1
        r0 = tok_tile * 128
        P.dma(lambda e: e.dma_start(out=hacc[i][:], in_=src[r0:r0 + 128, :]), reads=[('h', tok_tile)],
              writes=['hacc%d' % i])
        for half, bk in enumerate(banks):
            P.dve(lambda e, half=half, bk=bk: e.tensor_tensor(
                out=hacc[i][:, half * 512:(half + 1) * 512], in0=hacc[i][:, half * 512:(half + 1) * 512],
                in1=B[bk][:], op=ALU.add), reads=['B%d' % bk, 'hacc%d' % i], writes=['hacc%d' % i])
        P.dma(lambda e: e.dma_start(out=out[r0:r0 + 128, :], in_=hacc[i][:]), reads=['hacc%d' % i],
              writes=[('h', tok_tile)])

    def phase_outproj(l, src):
        fence()
        P.dma(lambda e: e.dma_start(out=colv[:, 0:KT], in_=beta_c[l]), writes=['colv'])
        load_w_bf(W2[:, 0], 'WB', w_out[l], KT, D, scale_col=colv, scale_key='colv')
        for c in range(T // TC):
            P.dma(lambda e, c=c: e.dma_start(
                out=mst[:], in_=mixT[:, c * TC:(c + 1) * TC].rearrange("(k p) t -> p k t", p=128)),
                reads=['mixT'], writes=['mst'])
            P.act(lambda e: e.copy(out=mxb[:], in_=mst[:]), reads=['mst'], writes=['mxb'])
            for tt in range(NTC):
                for half in range(2):
                    for kt in range(KT):
                        P.pe(lambda e, tt=tt, half=half, kt=kt: e.matmul(
                            out=B[half][:], lhsT=mxb[:, kt, tt * 128:(tt + 1) * 128],
                            rhs=W2[:, 0, kt, half * 512:(half + 1) * 512], start=(kt == 0), stop=(kt == KT - 1)),
                            reads=['mxb', 'WB'], writes=['B%d' % half])
                add_to_h(l, src, c * NTC + tt, [0, 1], False)

    kT_bf = PX[:, 0:1024].bitcast(BF16).rearrange("p (k t) -> p k t", k=KT)
    v_bf = PX[:, 1024:2048].bitcast(BF16).rearrange("p (k t) -> p k t", k=2)
    mnT = PX[:, 2048:3072].bitcast(BF16).rearrange("p (k t) -> p k t", k=KT)
    qT_bf = R32[:, 4096:6144].bitcast(BF16)[:, 0:KT * TC].rearrange("p (k t) -> p k t", k=KT)
    eT_bf = PX[:, 3072:3584].bitcast(BF16)[:, 0:2 * TC].rearrange("p (k t) -> p k t", k=2)
    oT_bf = R32[:, 6144:8192].bitcast(BF16)[:, 0:KT * TC].rearrange("p (k t) -> p k t", k=KT)
    rden = PX[:, 3584:3584 + TC]

    def phase_xattn(l):
        fence()
        load_gb(norm_mem_g[l:l + 1, :])
        for mt in range(2):
            rms_to_T(memx[mt * 128:(mt + 1) * 128, :], mnT, 'mnT', mt * 128)
        load_w_bf(W2[:, 0], 'WB', xa_wk[l], KT, D)
        load_w_bf(W2[:, 1], 'WB', xa_wv[l], KT, D)
        for f in range(KT):
            m = f % 2
            for kt in range(KT):
                P.pe(lambda e, f=f, kt=kt, m=m: e.matmul(out=B[m][:, 0:256], lhsT=W2[:, 0, kt, f * 128:(f + 1) * 128],
                                                        rhs=mnT[:, kt, :], start=(kt == 0), stop=(kt == KT - 1)),
                     reads=['WB', 'mnT'], writes=['B%d' % m])
            P.act(lambda e, f=f, m=m: e.copy(out=kT_bf[:, f, :], in_=B[m][:, 0:256]), reads=['B%d' % m],
                  writes=['kT_bf'])
        for mt in range(2):
            for half in range(2):
                m = 2 + half
                for kt in range(KT):
                    P.pe(lambda e, mt=mt, half=half, kt=kt, m=m: e.matmul(
                        out=B[m][:], lhsT=mnT[:, kt, mt * 128:(mt + 1) * 128],
                        rhs=W2[:, 1, kt, half * 512:(half + 1) * 512], start=(kt == 0), stop=(kt == KT - 1)),
                        reads=['WB', 'mnT'], writes=['B%d' % m])
                P.act(lambda e, mt=mt, half=half, m=m: e.copy(out=v_bf[:, mt, half * 512:(half + 1) * 512],
                                                             in_=B[m][:]), reads=['B%d' % m], writes=['v_bf'])
        load_w_bf(W2[:, 0], 'WB', xa_wq[l], KT, D)
        load_w_bf(W2[:, 1], 'WB', xa_wo[l], KT, D)
        load_gb(norm_xattn_g[l:l + 1, :])
        for c in range(T // TC):
            ci = cnt['chunk'] % 2
            cnt['chunk'] += 1
            for tt in range(NTC):
                r0 = c * TC + tt * 128
                rms_to_T(out[r0:r0 + 128, :], xnT[ci], 'xnT%d' % ci, tt * 128, src_key=('h', r0 // 128))
            for f in range(KT):
                m = f % 2
                for kt in range(KT):
                    P.pe(lambda e, f=f, kt=kt, m=m, ci=ci: e.matmul(
                        out=B[m][:, 0:TC], lhsT=W2[:, 0, kt, f * 128:(f + 1) * 128], rhs=xnT[ci][:, kt, :],
                        start=(kt == 0), stop=(kt == KT - 1)), reads=['WB', 'xnT%d' % ci], writes=['B%d' % m])
                P.act(lambda e, f=f, m=m: e.copy(out=qT_bf[:, f, :], in_=B[m][:, 0:TC]), reads=['B%d' % m],
                      writes=['qT_bf'])
            for hd in range(4):
                for mt in range(2):
                    m = 2 + mt
                    for ff in range(2):
                        f = hd * 2 + ff
                        P.pe(lambda e, f=f, ff=ff, mt=mt, m=m: e.matmul(
                            out=B[m][:, 0:TC], lhsT=kT_bf[:, f, mt * 128:(mt + 1) * 128], rhs=qT_bf[:, f, :],
                            start=(ff == 0), stop=(ff == 1)), reads=['kT_bf', 'qT_bf'], writes=['B%d' % m])
                    P.act(lambda e, mt=mt, m=m: e.activation(out=eT_bf[:, mt, :], in_=B[m][:, 0:TC], func=AF.Exp,
                                                            scale=1.0 / 16.0), reads=['B%d' % m], writes=['eT_bf'])
                for mt in range(2):
                    P.pe(lambda e, mt=mt: e.matmul(out=B[4][:, 0:TC], lhsT=ones_bf[:], rhs=eT_bf[:, mt, :],
                                                   start=(mt == 0), stop=(mt == 1)),
                         reads=['ones_bf', 'eT_bf'], writes=['B4'])
                P.dve(lambda e: e.reciprocal(out=rden[:], in_=B[4][:, 0:TC]), reads=['B4'], writes=['rden'])
                for ff in range(2):
                    f = hd * 2 + ff
                    for mt in range(2):
                        P.pe(lambda e, f=f, mt=mt: e.matmul(out=B[5][:, 0:TC], lhsT=v_bf[:, mt, f * 128:(f + 1) * 128],
                                                            rhs=eT_bf[:, mt, :], start=(mt == 0), stop=(mt == 1)),
                             reads=['v_bf', 'eT_bf'], writes=['B5'])
                    P.dve(lambda e, f=f: e.tensor_tensor(out=oT_bf[:, f, :], in0=B[5][:, 0:TC], in1=rden[:],
                                                         op=ALU.mult), reads=['B5', 'rden'], writes=['oT_bf'])
            for tt in range(NTC):
                for half in range(2):
                    for f in range(KT):
                        P.pe(lambda e, tt=tt, half=half, f=f: e.matmul(
                            out=B[half][:], lhsT=oT_bf[:, f, tt * 128:(tt + 1) * 128],
                            rhs=W2[:, 1, f, half * 512:(half + 1) * 512], start=(f == 0), stop=(f == KT - 1)),
                            reads=['oT_bf', 'WB'], writes=['B%d' % half])
                add_to_h(l, out, c * NTC + tt, [0, 1], False)

    TM = min(T, 2 * TC)
    NTM = TM // 128
    TBH = min(512, TM)
    xfT = XN[:, 0:KT * TM].rearrange("p (k t) -> p k t", k=KT)
    xfTf = PX[:, 3072:4096].rearrange("p (k t) -> p k t", k=KT)
    rw = sb("rw", [128, KT, 36])
    rbb = sb("rbb", [128, 36])
    lg = sb("lg", [128, 36])
    gates = sb("gates", [128, NTM, 32])
    rt = sb("rt", [128, 8, 32])
    r1 = sb("r1", [128, 16])
    macc = R32[:, 0:NTM * D].rearrange("p (t d) -> p t d", t=NTM)
    hidT = PX[:, 0:2048].bitcast(BF16)[:, 0:4 * TM].rearrange("p (k t) -> p k t", k=4)
    sg = [PX[:, 2048 + i * 512:2560 + i * 512] for i in range(2)]
    EWS = []
    for i_ in range(2):
        EW = WB[:, i_ * 12288:(i_ + 1) * 12288].rearrange("p (w n) -> p w n", w=3)
        EWS.append((EW[:, 0].rearrange("p (k n) -> p k n", k=8), EW[:, 1].rearrange("p (k n) -> p k n", k=8),
                    EW[:, 2].rearrange("p (k n) -> p k n", k=4)))
    BIG = 1.0e4
    dbg_g = dscr("dbg_g", [128, 32]); dbg_r1 = dscr("dbg_r1", [128, 16]); dbg_lg = dscr("dbg_lg", [128, 36])

    def route(tt):
        g = lambda i: rt[:, i, :]
        dv = lambda fn, r, w: P.dve(fn, reads=r, writes=w)
        dv(lambda e: e.tensor_reduce(out=r1[:, 0:1], in_=lg[:, 0:4], axis=AX.X, op=ALU.max), ['lg'], ['r1'])
        dv(lambda e: e.tensor_scalar(out=rt[:, 0, 0:4], in0=lg[:, 0:4], scalar1=r1[:, 0:1], scalar2=None,
                                     op0=ALU.is_equal), ['lg', 'r1'], ['rt'])
        dv(lambda e: e.tensor_scalar(out=rt[:, 1, 0:4], in0=lg[:, 0:4], scalar1=r1[:, 0:1], scalar2=None,
                                     op0=ALU.subtract), ['lg', 'r1'], ['rt'])
        P.act(lambda e: e.activation(out=rt[:, 1, 0:4], in_=rt[:, 1, 0:4], func=AF.Exp, accum_out=r1[:, 1:2]),
              ['rt'], ['rt', 'r1'])
        dv(lambda e: e.reciprocal(out=r1[:, 2:3], in_=r1[:, 1:2]), ['r1'], ['r1'])
        dv(lambda e: e.tensor_scalar(out=rt[:, 0, 0:4], in0=rt[:, 0, 0:4], scalar1=-1.0, scalar2=BIG,
                                     op0=ALU.add, op1=ALU.mult), ['rt'], ['rt'])
        dv(lambda e: e.tensor_tensor(out=rt[:, 2, :].rearrange("p (g x) -> p g x", g=4),
                                     in0=lg[:, 4:36].rearrange("p (g x) -> p g x", g=4),
                                     in1=rt[:, 0, 0:4].unsqueeze(2).to_broadcast([128, 4, 8]), op=ALU.add),
           ['lg', 'rt'], ['rt'])
        dv(lambda e: e.tensor_reduce(out=r1[:, 3:4], in_=g(2), axis=AX.X, op=ALU.max), ['rt'], ['r1'])
        dv(lambda e: e.tensor_scalar(out=g(3), in0=g(2), scalar1=r1[:, 3:4], scalar2=None, op0=ALU.is_equal),
           ['rt', 'r1'], ['rt'])
        dv(lambda e: e.scalar_tensor_tensor(out=g(4), in0=g(3), scalar=-BIG, in1=g(2), op0=ALU.mult, op1=ALU.add),
           ['rt'], ['rt'])
        dv(lambda e: e.tensor_reduce(out=r1[:, 4:5], in_=g(4), axis=AX.X, op=ALU.max), ['rt'], ['r1'])
        dv(lambda e: e.tensor_scalar(out=g(5), in0=g(4), scalar1=r1[:, 4:5], scalar2=None, op0=ALU.is_equal),
           ['rt', 'r1'], ['rt'])
        dv(lambda e: e.tensor_tensor(out=r1[:, 5:6], in0=r1[:, 4:5], in1=r1[:, 3:4], op=ALU.subtract),
           ['r1'], ['r1'])
        P.act(lambda e: e.activation(out=r1[:, 6:7], in_=r1[:, 5:6], func=AF.Exp), ['r1'], ['r1'])
        dv(lambda e: e.tensor_scalar(out=r1[:, 7:8], in0=r1[:, 6:7], scalar1=1.0, scalar2=None, op0=ALU.add),
           ['r1'], ['r1'])
        dv(lambda e: e.reciprocal(out=r1[:, 7:8], in_=r1[:, 7:8]), ['r1'], ['r1'])
        dv(lambda e: e.tensor_tensor(out=r1[:, 8:9], in0=r1[:, 7:8], in1=r1[:, 2:3], op=ALU.mult), ['r1'], ['r1'])
        dv(lambda e: e.tensor_tensor(out=r1[:, 9:10], in0=r1[:, 2:3], in1=r1[:, 8:9], op=ALU.subtract),
           ['r1'], ['r1'])
        dv(lambda e: e.tensor_scalar(out=g(6), in0=g(3), scalar1=r1[:, 8:9], scalar2=None, op0=ALU.mult),
           ['rt', 'r1'], ['rt'])
        dv(lambda e: e.scalar_tensor_tensor(out=gates[:, tt, :], in0=g(5), scalar=r1[:, 9:10], in1=g(6),
                                            op0=ALU.mult, op1=ALU.add), ['rt', 'r1'], ['gates'])

    def phase_moe(l):
        fence()
        load_gb(norm_ffn_g[l:l + 1, :])
        P.dma(lambda e: e.dma_start(out=rw[:], in_=moe_rw[l].rearrange("(k p) n -> p k n", p=128)), writes=['rw'])
        P.dma(lambda e: e.dma_start(out=rbb[:], in_=moe_rb[l:l + 1, :].partition_broadcast(128)), writes=['rbb'])
        for c in range(T // TM):
            for tt in range(NTM):
                r0 = c * TM + tt * 128
                rms_to_T(out[r0:r0 + 128, :], xfT, 'xfT', tt * 128, src_key=('h', r0 // 128),
                         dstTf=xfTf, dstf_key='xfTf')
                for kt in range(KT):
                    P.pe(lambda e, kt=kt: e.matmul(out=B[3][:, 0:36], lhsT=xfTf[:, kt, :], rhs=rw[:, kt, :],
                                                   start=(kt == 0), stop=(kt == KT - 1)),
                         reads=['xfTf', 'rw'], writes=['B3'])
                P.dve(lambda e: e.tensor_tensor(out=lg[:], in0=B[3][:, 0:36], in1=rbb[:], op=ALU.add),
                      reads=['B3', 'rbb'], writes=['lg'])
                route(tt)
            if dbg:
                P.dma(lambda e: e.dma_start(out=dbg_g[:, :], in_=gates[:, 0, :]), reads=['gates'], writes=['dbg_g'])
                P.dma(lambda e: e.dma_start(out=dbg_r1[:, :], in_=r1[:, :]), reads=['r1'], writes=['dbg_r1'])
                P.dma(lambda e: e.dma_start(out=dbg_lg[:, :], in_=lg[:, :]), reads=['lg'], writes=['dbg_lg'])
            for ex in range(32):
                wg_bf, wu_bf, wd_bf = EWS[ex % 2]
                ewk = 'EW%d' % (ex % 2)
                load_w_bf(wg_bf, ewk, moe_wg[l, ex], 8, 512)
                load_w_bf(wu_bf, ewk, moe_wu[l, ex], 8, 512)
                load_w_bf(wd_bf, ewk, moe_wd[l, ex], 4, D)
                for tb in range(TM // TBH):
                    for mt in range(4):
                        for kt in range(KT):
                            P.pe(lambda e, mt=mt, kt=kt, tb=tb, wg_bf=wg_bf: e.matmul(
                                out=B[0][:, 0:TBH], lhsT=wg_bf[:, kt, mt * 128:(mt + 1) * 128],
                                rhs=xfT[:, kt, tb * TBH:(tb + 1) * TBH], start=(kt == 0), stop=(kt == KT - 1)),
                                reads=[ewk, 'xfT'], writes=['B0'])
                        for kt in range(KT):
                            P.pe(lambda e, mt=mt, kt=kt, tb=tb, wu_bf=wu_bf: e.matmul(
                                out=B[1][:, 0:TBH], lhsT=wu_bf[:, kt, mt * 128:(mt + 1) * 128],
                                rhs=xfT[:, kt, tb * TBH:(tb + 1) * TBH], start=(kt == 0), stop=(kt == KT - 1)),
                                reads=[ewk, 'xfT'], writes=['B1'])
                        j = cnt['mm'] % 2
                        cnt['mm'] += 1
                        P.act(lambda e, j=j: e.activation(out=sg[j][:, 0:TBH], in_=B[0][:, 0:TBH], func=AF.Silu),
                              reads=['B0'], writes=['sg%d' % j])
                        P.dve(lambda e, j=j, mt=mt, tb=tb: e.tensor_tensor(
                            out=hidT[:, mt, tb * TBH:(tb + 1) * TBH], in0=sg[j][:, 0:TBH], in1=B[1][:, 0:TBH], op=ALU.mult),
                            reads=['sg%d' % j, 'B1'], writes=['hidT'])
                for tt in range(NTM):
                    for half in range(2):
                        bk = 2 + half
                        for kt in range(4):
                            P.pe(lambda e, tt=tt, half=half, kt=kt, bk=bk, wd_bf=wd_bf: e.matmul(
                                out=B[bk][:], lhsT=hidT[:, kt, tt * 128:(tt + 1) * 128],
                                rhs=wd_bf[:, kt, half * 512:(half + 1) * 512], start=(kt == 0), stop=(kt == 3)),
                                reads=['hidT', ewk], writes=['B%d' % bk])
                        sl = macc[:, tt, half * 512:(half + 1) * 512]
                        if ex == 0:
                            P.dve(lambda e, sl=sl, tt=tt, bk=bk, ex=ex: e.tensor_scalar(
                                out=sl, in0=B[bk][:], scalar1=gates[:, tt, ex:ex + 1], scalar2=None, op0=ALU.mult),
                                reads=['B%d' % bk, 'gates'], writes=['macc'])
                        else:
                            P.dve(lambda e, sl=sl, tt=tt, bk=bk, ex=ex: e.scalar_tensor_tensor(
                                out=sl, in0=B[bk][:], scalar=gates[:, tt, ex:ex + 1], in1=sl, op0=ALU.mult,
                                op1=ALU.add), reads=['B%d' % bk, 'gates', 'macc'], writes=['macc'])
            for tt in range(NTM):
                i = cnt['tile'] % 2
                cnt['tile'] += 1
                r0 = c * TM + tt * 128
                tk = r0 // 128
                P.dma(lambda e, i=i, r0=r0: e.dma_start(out=hacc[i][:], in_=out[r0:r0 + 128, :]),
                      reads=[('h', tk)], writes=['hacc%d' % i])
                P.pool(lambda e, i=i, tt=tt: e.tensor_tensor(out=hacc[i][:], in0=hacc[i][:], in1=macc[:, tt, :],
                                                             op=ALU.add), reads=['hacc%d' % i, 'macc'],
                       writes=['hacc%d' % i])
                P.dma(lambda e, i=i, r0=r0: e.dma_start(out=out[r0:r0 + 128, :], in_=hacc[i][:]),
                      reads=['hacc%d' % i], writes=[('h', tk)])

    def phase_final():
        load_gb(norm_final_g[0:1, :])
        for tk in range(NT):
            i = cnt['tile'] % 2
            cnt['tile'] += 1
            r0 = tk * 128
            P.dma(lambda e, i=i, r0=r0: e.dma_start(out=hb[i][:], in_=out[r0:r0 + 128, :]), reads=[('h', tk)],
                  writes=['hb%d' % i])
            P.act(lambda e, i=i: e.activation(out=xn[i][:], in_=hb[i][:], func=AF.Square, accum_out=ss[i][:]),
                  reads=['hb%d' % i], writes=['xn%d' % i, 'ss%d' % i])
            P.dve(lambda e, i=i: e.tensor_scalar(out=rstd[i][:], in0=ss[i][:], scalar1=1.0 / D, scalar2=1e-6,
                                                 op0=ALU.mult, op1=ALU.add), reads=['ss%d' % i], writes=['rstd%d' % i])
            P.act(lambda e, i=i: e.sqrt(out=rstd[i][:], in_=rstd[i][:]), reads=['rstd%d' % i], writes=['rstd%d' % i])
            P.dve(lambda e, i=i: e.reciprocal(out=rstd[i][:], in_=rstd[i][:]), reads=['rstd%d' % i],
                  writes=['rstd%d' % i])
            P.dve(lambda e, i=i: e.scalar_tensor_tensor(out=xnf[i][:], in0=hb[i][:], scalar=rstd[i][:, 0:1], in1=gb[:],
                                                        op0=ALU.mult, op1=ALU.mult),
                  reads=['hb%d' % i, 'rstd%d' % i, 'gb'], writes=['xnf%d' % i])
            P.dma(lambda e, i=i, r0=r0: e.dma_start(out=out[r0:r0 + 128, :], in_=xnf[i][:]), reads=['xnf%d' % i],
                  writes=[('h', tk)])

    colsB = din("colsB", [L, 128, 80])
    gla_w_up = din("gla_w_up", [L, 16, 128])
    TBk = min(512, T)
    Wt = [R32[:, i * 512:(i + 1) * 512] for i in range(16)] + \
         [XN[:, i * 1024:(i + 1) * 1024].bitcast(F32) for i in range(8)]
    WK = ['Wt%d' % i for i in range(24)]
    ALIAS.extend(WK)
    cB = sb("cB", [128, 80])
    onesm = sb("onesm", [128, 128])
    ones64 = sb("ones64", [128, 64])
    P.dve(lambda e: e.memset(onesm[:], 1.0 / 256.0), writes=['onesm'])
    P.dve(lambda e: e.memset(ones64[:], 1.0), writes=['ones64'])
    ubuf = [sb("ubuf%d" % i, [128, 30 + TBk]) for i in range(2)]
    stG = sb("stG", [64, 128])
    wup = sb("wup", [16, 128])
    zT = PX[0:16, 1792:1792 + TBk]
    ktok = PX[:, 0:512].rearrange("p (b c) -> p b c", b=4)
    vtok = PX[:, 512:1536].rearrange("p (b c) -> p b c", b=4)
    ark = PX[:, 1536:1792]
    mask_incl = c_f32[:, 128:384]
    blk64 = c_f32[:, 384:512]

    dbg_outs = {}

    def dbgdump(name, ap, key, shape):
        if not dbg:
            return
        t = nc.dram_tensor("dbg_" + name, list(shape), F32, kind="ExternalOutput").ap()
        P.dma(lambda e: e.dma_start(out=t, in_=ap), reads=[key], writes=['dbg_' + name])

    def ldrow(w, row0, c0, n=128, cols=None):
        cols = TBk if cols is None else cols
        P.dma(lambda e: e.dma_start(out=Wt[w][0:n, 0:cols], in_=pT[row0:row0 + n, c0:c0 + cols]),
              reads=['pT'], writes=[WK[w]])

    def strow(w, row0, c0):
        P.dma(lambda e: e.dma_start(out=mixT[row0:row0 + 128, c0:c0 + TBk], in_=Wt[w][:, 0:TBk]),
              reads=[WK[w]], writes=['mixT'])

    def conv_block(l, blk):
        c0 = blk * TBk
        for ct in range(2):
            ub = ubuf[ct]
            uk = 'ubuf%d' % ct
            if blk == 0:
                P.pool(lambda e, ub=ub: e.memset(ub[:, 0:30], 0.0), writes=[uk])
            else:
                P.act(lambda e, ub=ub: e.copy(out=ub[:, 0:30], in_=ub[:, TBk:TBk + 30]), reads=[uk], writes=[uk])
            ldrow(0, 1936 + ct * 128, c0)
            ldrow(1, 2192 + ct * 128, c0)
            P.act(lambda e: e.activation(out=Wt[1][:, 0:TBk], in_=Wt[1][:, 0:TBk], func=AF.Sigmoid),
                  reads=[WK[1]], writes=[WK[1]])
            P.pool(lambda e, ub=ub: e.tensor_tensor(out=ub[:, 30:30 + TBk], in0=Wt[0][:, 0:TBk], in1=Wt[1][:, 0:TBk],
                                                    op=ALU.mult), reads=[WK[0], WK[1], uk], writes=[uk])
            acc = 2 + ct
            P.dve(lambda e, ub=ub, ct=ct, acc=acc: e.tensor_scalar(
                out=Wt[acc][:, 0:TBk], in0=ub[:, 0:TBk], scalar1=cB[:, ct * 31:ct * 31 + 1], scalar2=None,
                op0=ALU.mult), reads=[uk, 'cB'], writes=[WK[acc]])
            for k in range(1, 31):
                P.dve(lambda e, ub=ub, ct=ct, acc=acc, k=k: e.scalar_tensor_tensor(
                    out=Wt[acc][:, 0:TBk], in0=ub[:, k:k + TBk], scalar=cB[:, ct * 31 + k:ct * 31 + k + 1],
                    in1=Wt[acc][:, 0:TBk], op0=ALU.mult, op1=ALU.add), reads=[uk, 'cB', WK[acc]], writes=[WK[acc]])
            P.act(lambda e, ct=ct, acc=acc: e.activation(out=Wt[acc][:, 0:TBk], in_=Wt[acc][:, 0:TBk],
                                                         func=AF.Identity, bias=cB[:, 62 + ct:63 + ct]),
                  reads=[WK[acc], 'cB'], writes=[WK[acc]])
            P.act(lambda e, ct=ct, acc=acc: e.activation(out=Wt[4 + ct][:, 0:TBk], in_=Wt[acc][:, 0:TBk],
                                                         func=AF.Square), reads=[WK[acc]], writes=[WK[4 + ct]])
        for ct in range(2):
            P.pe(lambda e, ct=ct: e.matmul(out=B[0][:, 0:TBk], lhsT=onesm[:], rhs=Wt[2 + ct][:, 0:TBk],
                                           start=(ct == 0), stop=(ct == 1)), reads=['onesm', WK[2 + ct]], writes=['B0'])
        for ct in range(2):
            P.pe(lambda e, ct=ct: e.matmul(out=B[1][:, 0:TBk], lhsT=onesm[:], rhs=Wt[4 + ct][:, 0:TBk],
                                           start=(ct == 0), stop=(ct == 1)), reads=['onesm', WK[4 + ct]], writes=['B1'])
        P.act(lambda e: e.copy(out=Wt[6][:, 0:TBk], in_=B[0][:, 0:TBk]), reads=['B0'], writes=[WK[6]])
        P.dve(lambda e: e.tensor_tensor(out=Wt[7][:, 0:TBk], in0=Wt[6][:, 0:TBk], in1=Wt[6][:, 0:TBk], op=ALU.mult),
              reads=[WK[6]], writes=[WK[7]])
        P.dve(lambda e: e.tensor_tensor(out=Wt[7][:, 0:TBk], in0=B[1][:, 0:TBk], in1=Wt[7][:, 0:TBk],
                                        op=ALU.subtract), reads=['B1', WK[7]], writes=[WK[7]])
        P.dve(lambda e: e.tensor_scalar(out=Wt[7][:, 0:TBk], in0=Wt[7][:, 0:TBk], scalar1=1e-5, scalar2=None,
                                        op0=ALU.add), reads=[WK[7]], writes=[WK[7]])
        P.act(lambda e: e.sqrt(out=Wt[7][:, 0:TBk], in_=Wt[7][:, 0:TBk]), reads=[WK[7]], writes=[WK[7]])
        P.dve(lambda e: e.reciprocal(out=Wt[7][:, 0:TBk], in_=Wt[7][:, 0:TBk]), reads=[WK[7]], writes=[WK[7]])
        for ct in range(2):
            acc = 2 + ct
            P.dve(lambda e, acc=acc: e.tensor_tensor(out=Wt[acc][:, 0:TBk], in0=Wt[acc][:, 0:TBk],
                                                     in1=Wt[6][:, 0:TBk], op=ALU.subtract),
                  reads=[WK[acc], WK[6]], writes=[WK[acc]])
            P.dve(lambda e, acc=acc: e.tensor_tensor(out=Wt[acc][:, 0:TBk], in0=Wt[acc][:, 0:TBk],
                                                     in1=Wt[7][:, 0:TBk], op=ALU.mult),
                  reads=[WK[acc], WK[7]], writes=[WK[acc]])
            P.act(lambda e, acc=acc, ct=ct: e.activation(out=Wt[acc][:, 0:TBk], in_=Wt[acc][:, 0:TBk], func=AF.Silu,
                                                         scale=cB[:, 64 + ct:65 + ct], bias=cB[:, 66 + ct:67 + ct]),
                  reads=[WK[acc], 'cB'], writes=[WK[acc]])
            strow(acc, 768 + ct * 128, c0)

    def gla_block(l, blk):
        c0 = blk * TBk
        NCH = TBk // 64
        QT, KTt, LA, BC, EB, ENB, RT, KTT = (0, 1), (2, 3), (4, 5), (6, 7), (8, 9), (10, 11), (12, 13), (14, 15)
        V0, V1, G0, G1, OB0, OB1, SQ, TMP = 16, 17, 18, 19, 20, 21, 22, 23
        for hf in range(2):
            ldrow(QT[hf], 256 + hf * 64, c0, n=64); ldrow(KTt[hf], 384 + hf * 64, c0, n=64)
        ldrow(V0, 512, c0); ldrow(V1, 640, c0)
        ldrow(G0, 768, c0); ldrow(G1, 896, c0)
        P.dma(lambda e: e.dma_start(out=zT[:, :], in_=pT[1024:1040, c0:c0 + TBk]), reads=['pT'], writes=['zT'])
        if blk == 0:
            P.pool(lambda e: e.memset(stG[:], 0.0), writes=['stG'])
        for hf in range(2):
            la, bc, eb, enb, rt, ktt = Wt[LA[hf]], Wt[BC[hf]], Wt[EB[hf]], Wt[ENB[hf]], Wt[RT[hf]], Wt[KTT[hf]]
            kla, kbc, keb, kenb, krt, kktt = (WK[LA[hf]], WK[BC[hf]], WK[EB[hf]], WK[ENB[hf]], WK[RT[hf]],
                                              WK[KTT[hf]])
            P.pe(lambda e, hf=hf: e.matmul(out=B[0][0:64, 0:TBk], lhsT=wup[:, hf * 64:(hf + 1) * 64], rhs=zT[:, :],
                                           start=True, stop=True), reads=['wup', 'zT'], writes=['B0'])
            P.act(lambda e, la=la, hf=hf: e.activation(out=la[0:64, 0:TBk], in_=B[0][0:64, 0:TBk], func=AF.Exp,
                                                       scale=-1.0, bias=cB[0:64, 71 + 2 * hf:72 + 2 * hf]),
                  reads=['B0', 'cB'], writes=[kla])
            P.act(lambda e, la=la: e.activation(out=la[0:64, 0:TBk], in_=la[0:64, 0:TBk], func=AF.Ln, bias=1.0),
                  reads=[kla], writes=[kla])
            P.dve(lambda e, la=la: e.tensor_scalar(out=la[0:64, 0:TBk], in0=la[0:64, 0:TBk], scalar1=-1.0 / 16.0,
                                                   scalar2=None, op0=ALU.mult), reads=[kla], writes=[kla])
            for c in range(NCH):
                P.dve(lambda e, c=c, la=la, bc=bc: e.tensor_tensor_scan(
                    out=bc[0:64, c * 64:(c + 1) * 64], data0=ones64[0:64, :], data1=la[0:64, c * 64:(c + 1) * 64],
                    initial=0.0, op0=ALU.mult, op1=ALU.add), reads=['ones64', kla], writes=[kbc])
            P.act(lambda e, bc=bc, eb=eb: e.activation(out=eb[0:64, 0:TBk], in_=bc[0:64, 0:TBk], func=AF.Exp),
                  reads=[kbc], writes=[keb])
            P.act(lambda e, bc=bc, enb=enb: e.activation(out=enb[0:64, 0:TBk], in_=bc[0:64, 0:TBk], func=AF.Exp,
                                                         scale=-1.0), reads=[kbc], writes=[kenb])
            P.dve(lambda e, hf=hf, rt=rt, eb=eb: e.scalar_tensor_tensor(
                out=rt[0:64, 0:TBk], in0=Wt[QT[hf]][0:64, 0:TBk], scalar=32.0 ** -0.5, in1=eb[0:64, 0:TBk],
                op0=ALU.mult, op1=ALU.mult), reads=[WK[QT[hf]], keb], writes=[krt])
            P.pool(lambda e, hf=hf, ktt=ktt, enb=enb: e.tensor_tensor(
                out=ktt[0:64, 0:TBk], in0=Wt[KTt[hf]][0:64, 0:TBk], in1=enb[0:64, 0:TBk], op=ALU.mult),
                reads=[WK[KTt[hf]], kenb], writes=[kktt])
        if DBG_STOP <= 1:
            return
        for b4 in range(TBk // 128):
            sl = slice(b4 * 128, (b4 + 1) * 128)
            for hf in range(2):
                P.pe(lambda e, sl=sl, hf=hf: e.transpose(out=B[1][:, hf * 64:(hf + 1) * 64],
                                                         in_=Wt[KTT[hf]][0:64, sl], identity=identf[0:64, 0:64]),
                     reads=[WK[KTT[hf]], 'c_f32'], writes=['B1'])
            P.pe(lambda e, sl=sl: e.transpose(out=B[1][:, 128:256], in_=Wt[V0][:, sl], identity=identf),
                 reads=[WK[V0], 'c_f32'], writes=['B1'])
            P.pe(lambda e, sl=sl: e.transpose(out=B[1][:, 256:384], in_=Wt[V1][:, sl], identity=identf),
                 reads=[WK[V1], 'c_f32'], writes=['B1'])
            P.act(lambda e, b4=b4: e.copy(out=ktok[:, b4, :], in_=B[1][:, 0:128]), reads=['B1'], writes=['ktok'])
            P.act(lambda e, b4=b4: e.copy(out=vtok[:, b4, :], in_=B[1][:, 128:384]), reads=['B1'], writes=['vtok'])
        if DBG_STOP <= 2:
            return
        for c in range(NCH):
            b4, pb = c // 2, (c % 2) * 64
            cs = slice(c * 64, (c + 1) * 64)
            for hd in range(4):
                hf = hd // 2
                hs = slice((hd % 2) * 32, (hd % 2) * 32 + 32)
                P.pe(lambda e, hd=hd, hf=hf, hs=hs, cs=cs, pb=pb: e.matmul(
                    out=B[2][pb:pb + 64, hd * 64:(hd + 1) * 64], lhsT=Wt[KTT[hf]][hs, cs], rhs=Wt[RT[hf]][hs, cs],
                    start=True, stop=True), reads=[WK[KTT[hf]], WK[RT[hf]]], writes=['B2'])
            if DBG_STOP == 3 and DBG_VAR == 1:
                continue
            P.dve(lambda e, pb=pb: e.tensor_tensor(out=ark[pb:pb + 64, :], in0=B[2][pb:pb + 64, 0:256],
                                                   in1=mask_incl[pb:pb + 64, :], op=ALU.mult),
                  reads=['B2', 'c_f32'], writes=['ark'])
            if DBG_STOP <= 3:
                continue
            for hd in range(4):
                hf = hd // 2
                hs = slice((hd % 2) * 32, (hd % 2) * 32 + 32)
                vt, vb = hd // 2, (hd % 2) * 64
                P.pe(lambda e, hf=hf, hs=hs, cs=cs, vt=vt, vb=vb: e.matmul(
                    out=B[3][vb:vb + 64, vt * 64:(vt + 1) * 64], lhsT=stG[hs, hf * 64:(hf + 1) * 64],
                    rhs=Wt[RT[hf]][hs, cs], start=True, stop=False), reads=['stG', WK[RT[hf]]], writes=['B3'])
                P.pe(lambda e, hd=hd, b4=b4, pb=pb, vt=vt, vb=vb: e.matmul(
                    out=B[3][vb:vb + 64, vt * 64:(vt + 1) * 64], lhsT=vtok[pb:pb + 64, b4, hd * 64:(hd + 1) * 64],
                    rhs=ark[pb:pb + 64, hd * 64:(hd + 1) * 64], start=False, stop=True),
                    reads=['vtok', 'ark'], writes=['B3'])
            P.act(lambda e, cs=cs: e.copy(out=Wt[OB0][:, cs], in_=B[3][:, 0:64]), reads=['B3'], writes=[WK[OB0]])
            P.act(lambda e, cs=cs: e.copy(out=Wt[OB1][:, cs], in_=B[3][:, 64:128]), reads=['B3'], writes=[WK[OB1]])
            if DBG_STOP <= 4:
                continue
            for hd in range(4):
                hf = hd // 2
                hs = slice((hd % 2) * 32, (hd % 2) * 32 + 32)
                P.pe(lambda e, hd=hd, hf=hf, hs=hs, b4=b4, pb=pb: e.matmul(
                    out=B[4][hs, hf * 64:(hf + 1) * 64], lhsT=ktok[pb:pb + 64, b4, hd * 32:(hd + 1) * 32],
                    rhs=vtok[pb:pb + 64, b4, hd * 64:(hd + 1) * 64], start=True, stop=True),
                    reads=['ktok', 'vtok'], writes=['B4'])
            P.dve(lambda e: e.tensor_tensor(out=stG[0:64, :], in0=B[4][0:64, 0:128], in1=stG[0:64, :], op=ALU.add),
                  reads=['B4', 'stG'], writes=['stG'])
            for hf in range(2):
                P.dve(lambda e, c=c, hf=hf: e.tensor_scalar(
                    out=stG[0:64, hf * 64:(hf + 1) * 64], in0=stG[0:64, hf * 64:(hf + 1) * 64],
                    scalar1=Wt[EB[hf]][0:64, c * 64 + 63:c * 64 + 64], scalar2=None, op0=ALU.mult),
                    reads=['stG', WK[EB[hf]]], writes=['stG'])
        if DBG_STOP <= 5:
            return
        for vt, (OB, G) in enumerate(((OB0, G0), (OB1, G1))):
            P.act(lambda e, OB=OB: e.activation(out=Wt[SQ][:, 0:TBk], in_=Wt[OB][:, 0:TBk], func=AF.Square),
                  reads=[WK[OB]], writes=[WK[SQ]])
            P.pe(lambda e: e.matmul(out=B[5][:, 0:TBk], lhsT=blk64, rhs=Wt[SQ][:, 0:TBk], start=True, stop=True),
                 reads=['c_f32', WK[SQ]], writes=['B5'])
            P.dve(lambda e: e.tensor_scalar(out=Wt[TMP][:, 0:TBk], in0=B[5][:, 0:TBk], scalar1=1e-6, scalar2=None,
                                            op0=ALU.add), reads=['B5'], writes=[WK[TMP]])
            P.act(lambda e: e.sqrt(out=Wt[TMP][:, 0:TBk], in_=Wt[TMP][:, 0:TBk]), reads=[WK[TMP]], writes=[WK[TMP]])
            P.dve(lambda e: e.reciprocal(out=Wt[TMP][:, 0:TBk], in_=Wt[TMP][:, 0:TBk]), reads=[WK[TMP]],
                  writes=[WK[TMP]])
            P.dve(lambda e, OB=OB, vt=vt: e.scalar_tensor_tensor(
                out=Wt[OB][:, 0:TBk], in0=Wt[OB][:, 0:TBk], scalar=cB[:, 69 + vt:70 + vt], in1=Wt[TMP][:, 0:TBk],
                op0=ALU.mult, op1=ALU.mult), reads=[WK[OB], WK[TMP], 'cB'], writes=[WK[OB]])
            P.act(lambda e, G=G: e.activation(out=Wt[G][:, 0:TBk], in_=Wt[G][:, 0:TBk], func=AF.Silu),
                  reads=[WK[G]], writes=[WK[G]])
            P.pool(lambda e, OB=OB, G=G: e.tensor_tensor(out=Wt[OB][:, 0:TBk], in0=Wt[OB][:, 0:TBk],
                                                         in1=Wt[G][:, 0:TBk], op=ALU.mult),
                   reads=[WK[OB], WK[G]], writes=[WK[OB]])
            strow(OB, 256 + vt * 128, c0)

    s5p_in = din("s5p", [L, 128, 24])
    s5B_in = din("s5B", [L, 128, 16, 128])
    s5C_in = din("s5C", [L, 128, 2, 8, 16])
    s5_glu_w = din("s5_glu_w", [L, 256, 256])
    s5p = sb("s5p_sb", [128, 24])
    s5t = sb("s5t", [128, 16, 8])
    s5C = sb("s5C_sb", [128, 2, 8, 16])
    s5Cp = sb("s5Cp", [128, 2, 8, 16])
    gluw = sb("gluw", [128, 2, 256])
    carry = sb("carry", [128, 2, 8])
    WBf = WB[:].bitcast(F32)
    sinT = [WBf[:, j * 512:(j + 1) * 512] for j in range(8)]
    cosT = [WBf[:, (8 + j) * 512:(9 + j) * 512] for j in range(8)]
    Bm = wst[0][:, 0:2048].rearrange("p (m c) -> p m c", m=16)
    CL = wst[1][:, 0:2048].rearrange("p (m c) -> p m c", m=16)
    iota_t = c_f32[:, 1024:1536]
    mcol = c_f32[:, 1536:1538]
    PI = float(np.pi)

    s5i = PX[:, 2304:2816].bitcast(I32)

    def sin_red(dst, x, n, rk, wk, add=0.0):
        q, m = Wt[22][:, 0:n], Wt[23][:, 0:n]
        qi = s5i[:, 0:n]
        kq, km = WK[22], WK[23]
        P.dve(lambda e: e.tensor_scalar(out=dst, in0=x, scalar1=add, scalar2=None, op0=ALU.add), reads=rk, writes=wk)
        P.dve(lambda e: e.tensor_scalar(out=q, in0=dst, scalar1=1.0 / (2 * PI), scalar2=None, op0=ALU.mult),
              reads=wk, writes=[kq])
        P.dve(lambda e: e.tensor_copy(out=qi, in_=q), reads=[kq], writes=['s5i'])
        P.dve(lambda e: e.tensor_copy(out=q, in_=qi), reads=['s5i'], writes=[kq])
        P.dve(lambda e: e.scalar_tensor_tensor(out=dst, in0=q, scalar=-2 * PI, in1=dst, op0=ALU.mult, op1=ALU.add),
              reads=[kq] + wk, writes=wk)
        P.dve(lambda e: e.tensor_scalar(out=m, in0=dst, scalar1=PI, scalar2=None, op0=ALU.is_gt), reads=wk, writes=[km])
        P.dve(lambda e: e.scalar_tensor_tensor(out=dst, in0=m, scalar=-2 * PI, in1=dst, op0=ALU.mult, op1=ALU.add),
              reads=[km] + wk, writes=wk)
        P.dve(lambda e: e.tensor_scalar(out=m, in0=dst, scalar1=-PI, scalar2=None, op0=ALU.is_lt), reads=wk, writes=[km])
        P.dve(lambda e: e.scalar_tensor_tensor(out=dst, in0=m, scalar=2 * PI, in1=dst, op0=ALU.mult, op1=ALU.add),
              reads=[km] + wk, writes=wk)
        P.act(lambda e: e.activation(out=dst, in_=dst, func=AF.Sin), reads=wk, writes=wk)

    def s5_setup(l):
        t = lambda i: s5t[:, i, :]
        dv = lambda fn: P.dve(fn, reads=['s5t', 's5p'], writes=['s5t'])
        P.dma(lambda e: e.dma_start(out=s5p[:], in_=s5p_in[l]), writes=['s5p'])
        P.dma(lambda e: e.dma_start(out=Bm, in_=s5B_in[l]), writes=['wst0'])
        P.dma(lambda e: e.dma_start(out=s5C[:], in_=s5C_in[l]), writes=['s5C'])
        P.dma(lambda e: e.dma_start(out=gluw[:], in_=s5_glu_w[l].rearrange("(k p) n -> p k n", p=128)),
              writes=['gluw'])
        P.pool(lambda e: e.memset(carry[:], 0.0), writes=['carry%d' % j_ for j_ in range(8)])
        lr, li, ldt = s5p[:, 0:8], s5p[:, 8:16], s5p[:, 16:24]
        P.act(lambda e: e.activation(out=t(0), in_=ldt, func=AF.Exp), reads=['s5p'], writes=['s5t'])
        dv(lambda e: e.tensor_tensor(out=t(1), in0=lr, in1=t(0), op=ALU.mult))
        dv(lambda e: e.tensor_tensor(out=t(2), in0=li, in1=t(0), op=ALU.mult))
        P.act(lambda e: e.activation(out=t(3), in_=t(1), func=AF.Exp), reads=['s5t'], writes=['s5t'])
        sin_red(t(4), t(2), 8, ['s5t'], ['s5t'])
        sin_red(t(5), t(2), 8, ['s5t'], ['s5t'], add=0.5 * PI)
        dv(lambda e: e.tensor_tensor(out=t(6), in0=t(3), in1=t(5), op=ALU.mult))
        dv(lambda e: e.tensor_scalar(out=t(6), in0=t(6), scalar1=-1.0, scalar2=None, op0=ALU.add))
        dv(lambda e: e.tensor_tensor(out=t(7), in0=t(3), in1=t(4), op=ALU.mult))
        dv(lambda e: e.tensor_tensor(out=t(8), in0=lr, in1=lr, op=ALU.mult))
        dv(lambda e: e.tensor_tensor(out=t(9), in0=li, in1=li, op=ALU.mult))
        dv(lambda e: e.tensor_tensor(out=t(8), in0=t(8), in1=t(9), op=ALU.add))
        dv(lambda e: e.reciprocal(out=t(8), in_=t(8)))
        dv(lambda e: e.tensor_tensor(out=t(9), in0=t(6), in1=lr, op=ALU.mult))
        dv(lambda e: e.tensor_tensor(out=t(10), in0=t(7), in1=li, op=ALU.mult))
        dv(lambda e: e.tensor_tensor(out=t(9), in0=t(9), in1=t(10), op=ALU.add))
        dv(lambda e: e.tensor_tensor(out=t(9), in0=t(9), in1=t(8), op=ALU.mult))
        dv(lambda e: e.tensor_tensor(out=t(10), in0=t(7), in1=lr, op=ALU.mult))
        dv(lambda e: e.tensor_tensor(out=t(11), in0=t(6), in1=li, op=ALU.mult))
        dv(lambda e: e.tensor_tensor(out=t(10), in0=t(10), in1=t(11), op=ALU.subtract))
        dv(lambda e: e.tensor_tensor(out=t(10), in0=t(10), in1=t(8), op=ALU.mult))
        bc = lambda i: s5t[:, i, :].unsqueeze(2).to_broadcast([128, 8, 16])
        cr, ci = s5C[:, 0], s5C[:, 1]
        d2 = lambda fn: P.dve(fn, reads=['s5t', 's5C', 's5Cp'], writes=['s5Cp'])
        d2(lambda e: e.tensor_tensor(out=s5Cp[:, 0], in0=cr, in1=bc(9), op=ALU.mult))
        d2(lambda e: e.tensor_tensor(out=s5Cp[:, 1], in0=ci, in1=bc(10), op=ALU.mult))
        d2(lambda e: e.tensor_tensor(out=s5Cp[:, 0], in0=s5Cp[:, 0], in1=s5Cp[:, 1], op=ALU.subtract))
        d2(lambda e: e.tensor_tensor(out=s5Cp[:, 1], in0=cr, in1=bc(10), op=ALU.mult))
        P.dve(lambda e: e.tensor_tensor(out=s5C[:, 0], in0=ci, in1=bc(9), op=ALU.mult), reads=['s5t', 's5C'],
              writes=['s5C'])
        d2(lambda e: e.tensor_tensor(out=s5Cp[:, 1], in0=s5Cp[:, 1], in1=s5C[:, 0], op=ALU.add))
        P.pool(lambda e: e.memset(wst[1][:, 0:2048], 0.0), writes=['wst1'])
        P.dve(lambda e: e.tensor_scalar(out=s5t[:, 12, 0:2], in0=mcol, scalar1=-1.0, scalar2=None, op0=ALU.mult),
              reads=['c_f32'], writes=['s5t'])
        for j in range(8):
            for two in range(2):
                c0 = (j % 4) * 32 + two * 16
                P.dve(lambda e, j=j, two=two, c0=c0: e.tensor_scalar(
                    out=CL[:, j, c0:c0 + 16], in0=s5Cp[:, 0, j, :], scalar1=mcol[:, two:two + 1], scalar2=None,
                    op0=ALU.mult), reads=['s5Cp', 'c_f32'], writes=['wst1'])
                P.dve(lambda e, j=j, two=two, c0=c0: e.tensor_scalar(
                    out=CL[:, 8 + j, c0:c0 + 16], in0=s5Cp[:, 1, j, :], scalar1=s5t[:, 12, two:two + 1],
                    scalar2=None, op0=ALU.mult), reads=['s5Cp', 's5t'], writes=['wst1'])
            for tab, off in ((sinT[j], 0.0), (cosT[j], 0.5 * PI)):
                P.dve(lambda e, j=j, tab=tab: e.tensor_scalar(
                    out=tab, in0=iota_t, scalar1=s5t[:, 2, j:j + 1], scalar2=None, op0=ALU.mult),
                    reads=['s5t', 'c_f32'], writes=['WB'])
                sin_red(tab, tab, 512, ['WB'], ['WB'], add=off)

    def s5_block(l, blk):
        c0 = blk * TBk
        U = (0, 1)
        GT0, GT1 = 22, 23
        ldrow(U[0], 0, c0); ldrow(U[1], 128, c0)
        W = lambda i: Wt[i][:, 0:TBk]
        caps = []
        for j in range(8):
            caps.append(P.capture())
            ct = j // 4
            bA, bB = (B[0], B[1]) if j % 2 == 0 else (B[4], B[5])
            kbA, kbB = ('B0', 'B1') if j % 2 == 0 else ('B4', 'B5')
            BR, BI, T1, T2, T3, T4, ZR, ZI, XR, XI = range(2 + (j % 2) * 10, 12 + (j % 2) * 10)
            ck = 'carry%d' % j
            wBR, kBR, tBR = W(BR), WK[BR], Wt[BR]
            wBI, kBI, tBI = W(BI), WK[BI], Wt[BI]
            wT1, kT1, tT1 = W(T1), WK[T1], Wt[T1]
            wT2, kT2, tT2 = W(T2), WK[T2], Wt[T2]
            wT3, kT3, tT3 = W(T3), WK[T3], Wt[T3]
            wT4, kT4, tT4 = W(T4), WK[T4], Wt[T4]
            wZR, kZR, tZR = W(ZR), WK[ZR], Wt[ZR]
            wZI, kZI, tZI = W(ZI), WK[ZI], Wt[ZI]
            wXR, kXR, tXR = W(XR), WK[XR], Wt[XR]
            wXI, kXI, tXI = W(XI), WK[XI], Wt[XI]
            P.pe(lambda e, j=j, ct=ct, wBR=wBR, wBI=wBI, wT1=wT1, wT2=wT2, wT3=wT3, wT4=wT4, wZR=wZR, wZI=wZI, wXR=wXR, wXI=wXI, tXR=tXR, tXI=tXI, bA=bA, bB=bB: e.matmul(out=bA[:, 0:TBk], lhsT=Bm[:, j, :], rhs=W(U[ct]), start=True,
                                                stop=True), reads=['wst0', WK[U[ct]]], writes=[kbA])
            P.pe(lambda e, j=j, ct=ct, wBR=wBR, wBI=wBI, wT1=wT1, wT2=wT2, wT3=wT3, wT4=wT4, wZR=wZR, wZI=wZI, wXR=wXR, wXI=wXI, tXR=tXR, tXI=tXI, bA=bA, bB=bB: e.matmul(out=bB[:, 0:TBk], lhsT=Bm[:, 8 + j, :], rhs=W(U[ct]), start=True,
                                                stop=True), reads=['wst0', WK[U[ct]]], writes=[kbB])
            P.act(lambda e, wBR=wBR, wBI=wBI, wT1=wT1, wT2=wT2, wT3=wT3, wT4=wT4, wZR=wZR, wZI=wZI, wXR=wXR, wXI=wXI, tXR=tXR, tXI=tXI, bA=bA, bB=bB: e.copy(out=wBR, in_=bA[:, 0:TBk]), reads=[kbA], writes=[kBR])
            P.act(lambda e, wBR=wBR, wBI=wBI, wT1=wT1, wT2=wT2, wT3=wT3, wT4=wT4, wZR=wZR, wZI=wZI, wXR=wXR, wXI=wXI, tXR=tXR, tXI=tXI, bA=bA, bB=bB: e.copy(out=wBI, in_=bB[:, 0:TBk]), reads=[kbB], writes=[kBI])
            sn, cs_ = sinT[j][:, 0:TBk], cosT[j][:, 0:TBk]
            P.dve(lambda e, cs_=cs_, wBR=wBR, wBI=wBI, wT1=wT1, wT2=wT2, wT3=wT3, wT4=wT4, wZR=wZR, wZI=wZI, wXR=wXR, wXI=wXI, tXR=tXR, tXI=tXI, bA=bA, bB=bB: e.tensor_tensor(out=wT1, in0=wBR, in1=cs_, op=ALU.mult),
                  reads=[kBR, 'WB'], writes=[kT1])
            P.pool(lambda e, sn=sn, wBR=wBR, wBI=wBI, wT1=wT1, wT2=wT2, wT3=wT3, wT4=wT4, wZR=wZR, wZI=wZI, wXR=wXR, wXI=wXI, tXR=tXR, tXI=tXI, bA=bA, bB=bB: e.tensor_tensor(out=wT2, in0=wBI, in1=sn, op=ALU.mult),
                   reads=[kBI, 'WB'], writes=[kT2])
            P.dve(lambda e, cs_=cs_, wBR=wBR, wBI=wBI, wT1=wT1, wT2=wT2, wT3=wT3, wT4=wT4, wZR=wZR, wZI=wZI, wXR=wXR, wXI=wXI, tXR=tXR, tXI=tXI, bA=bA, bB=bB: e.tensor_tensor(out=wT3, in0=wBI, in1=cs_, op=ALU.mult),
                  reads=[kBI, 'WB'], writes=[kT3])
            P.pool(lambda e, sn=sn, wBR=wBR, wBI=wBI, wT1=wT1, wT2=wT2, wT3=wT3, wT4=wT4, wZR=wZR, wZI=wZI, wXR=wXR, wXI=wXI, tXR=tXR, tXI=tXI, bA=bA, bB=bB: e.tensor_tensor(out=wT4, in0=wBR, in1=sn, op=ALU.mult),
                   reads=[kBR, 'WB'], writes=[kT4])
            P.dve(lambda e, wBR=wBR, wBI=wBI, wT1=wT1, wT2=wT2, wT3=wT3, wT4=wT4, wZR=wZR, wZI=wZI, wXR=wXR, wXI=wXI, tXR=tXR, tXI=tXI, bA=bA, bB=bB: e.tensor_tensor(out=wT1, in0=wT1, in1=wT2, op=ALU.add),
                  reads=[kT1, kT2], writes=[kT1])
            P.dve(lambda e, wBR=wBR, wBI=wBI, wT1=wT1, wT2=wT2, wT3=wT3, wT4=wT4, wZR=wZR, wZI=wZI, wXR=wXR, wXI=wXI, tXR=tXR, tXI=tXI, bA=bA, bB=bB: e.tensor_tensor(out=wT3, in0=wT3, in1=wT4, op=ALU.subtract),
                  reads=[kT3, kT4], writes=[kT3])
            rb = s5t[:, 3, j:j + 1].to_broadcast([128, TBk])
            P.dve(lambda e, j=j, rb=rb, wBR=wBR, wBI=wBI, wT1=wT1, wT2=wT2, wT3=wT3, wT4=wT4, wZR=wZR, wZI=wZI, wXR=wXR, wXI=wXI, tXR=tXR, tXI=tXI, bA=bA, bB=bB: e.tensor_tensor_scan(out=wZR, data0=rb, data1=wT1,
                                                             initial=carry[:, 0, j:j + 1], op0=ALU.mult, op1=ALU.add),
                  reads=['s5t', kT1, ck], writes=[kZR])
            P.dve(lambda e, j=j, rb=rb, wBR=wBR, wBI=wBI, wT1=wT1, wT2=wT2, wT3=wT3, wT4=wT4, wZR=wZR, wZI=wZI, wXR=wXR, wXI=wXI, tXR=tXR, tXI=tXI, bA=bA, bB=bB: e.tensor_tensor_scan(out=wZI, data0=rb, data1=wT3,
                                                             initial=carry[:, 1, j:j + 1], op0=ALU.mult, op1=ALU.add),
                  reads=['s5t', kT3, ck], writes=[kZI])
            P.dve(lambda e, sn=sn, wBR=wBR, wBI=wBI, wT1=wT1, wT2=wT2, wT3=wT3, wT4=wT4, wZR=wZR, wZI=wZI, wXR=wXR, wXI=wXI, tXR=tXR, tXI=tXI, bA=bA, bB=bB: e.tensor_tensor(out=wT2, in0=wZI, in1=sn, op=ALU.mult),
                   reads=[kZI, 'WB'], writes=[kT2])
            P.dve(lambda e, sn=sn, wBR=wBR, wBI=wBI, wT1=wT1, wT2=wT2, wT3=wT3, wT4=wT4, wZR=wZR, wZI=wZI, wXR=wXR, wXI=wXI, tXR=tXR, tXI=tXI, bA=bA, bB=bB: e.tensor_tensor(out=wT4, in0=wZR, in1=sn, op=ALU.mult),
                   reads=[kZR, 'WB'], writes=[kT4])
            P.dve(lambda e, cs_=cs_, wBR=wBR, wBI=wBI, wT1=wT1, wT2=wT2, wT3=wT3, wT4=wT4, wZR=wZR, wZI=wZI, wXR=wXR, wXI=wXI, tXR=tXR, tXI=tXI, bA=bA, bB=bB: e.tensor_tensor(out=wXR, in0=wZR, in1=cs_, op=ALU.mult),
                  reads=[kZR, 'WB'], writes=[kXR])
            P.dve(lambda e, wBR=wBR, wBI=wBI, wT1=wT1, wT2=wT2, wT3=wT3, wT4=wT4, wZR=wZR, wZI=wZI, wXR=wXR, wXI=wXI, tXR=tXR, tXI=tXI, bA=bA, bB=bB: e.tensor_tensor(out=wXR, in0=wXR, in1=wT2, op=ALU.subtract),
                  reads=[kXR, kT2], writes=[kXR])
            P.dve(lambda e, cs_=cs_, wBR=wBR, wBI=wBI, wT1=wT1, wT2=wT2, wT3=wT3, wT4=wT4, wZR=wZR, wZI=wZI, wXR=wXR, wXI=wXI, tXR=tXR, tXI=tXI, bA=bA, bB=bB: e.tensor_tensor(out=wXI, in0=wZI, in1=cs_, op=ALU.mult),
                  reads=[kZI, 'WB'], writes=[kXI])
            P.dve(lambda e, wBR=wBR, wBI=wBI, wT1=wT1, wT2=wT2, wT3=wT3, wT4=wT4, wZR=wZR, wZI=wZI, wXR=wXR, wXI=wXI, tXR=tXR, tXI=tXI, bA=bA, bB=bB: e.tensor_tensor(out=wXI, in0=wXI, in1=wT4, op=ALU.add),
                  reads=[kXI, kT4], writes=[kXI])
            P.act(lambda e, j=j, wBR=wBR, wBI=wBI, wT1=wT1, wT2=wT2, wT3=wT3, wT4=wT4, wZR=wZR, wZI=wZI, wXR=wXR, wXI=wXI, tXR=tXR, tXI=tXI, bA=bA, bB=bB: e.copy(out=carry[:, 0, j:j + 1], in_=tXR[:, TBk - 1:TBk]), reads=[kXR],
                  writes=[ck])
            P.act(lambda e, j=j, wBR=wBR, wBI=wBI, wT1=wT1, wT2=wT2, wT3=wT3, wT4=wT4, wZR=wZR, wZI=wZI, wXR=wXR, wXI=wXI, tXR=tXR, tXI=tXI, bA=bA, bB=bB: e.copy(out=carry[:, 1, j:j + 1], in_=tXI[:, TBk - 1:TBk]), reads=[kXI],
                  writes=[ck])
            P.pe(lambda e, j=j, ct=ct, wBR=wBR, wBI=wBI, wT1=wT1, wT2=wT2, wT3=wT3, wT4=wT4, wZR=wZR, wZI=wZI, wXR=wXR, wXI=wXI, tXR=tXR, tXI=tXI, bA=bA, bB=bB: e.matmul(out=B[2 + ct][:, 0:TBk], lhsT=CL[:, j, :], rhs=wXR,
                                                start=(j % 4 == 0), stop=False), reads=['wst1', kXR],
                 writes=['B%d' % (2 + ct)])
            P.pe(lambda e, j=j, ct=ct, wBR=wBR, wBI=wBI, wT1=wT1, wT2=wT2, wT3=wT3, wT4=wT4, wZR=wZR, wZI=wZI, wXR=wXR, wXI=wXI, tXR=tXR, tXI=tXI, bA=bA, bB=bB: e.matmul(out=B[2 + ct][:, 0:TBk], lhsT=CL[:, 8 + j, :], rhs=wXI,
                                                start=False, stop=(j % 4 == 3)), reads=['wst1', kXI],
                 writes=['B%d' % (2 + ct)])
            P.end_capture()
        for jj in range(0, 8, 2):
            P.replay_interleaved([caps[jj], caps[jj + 1]])
        for ct, GT in enumerate((GT0, GT1)):
            P.dve(lambda e, ct=ct, GT=GT: e.scalar_tensor_tensor(
                out=W(GT), in0=W(U[ct]), scalar=cB[:, 74 + ct:75 + ct], in1=B[2 + ct][:, 0:TBk], op0=ALU.mult,
                op1=ALU.add), reads=[WK[U[ct]], 'cB', 'B%d' % (2 + ct)], writes=[WK[GT]])
            P.act(lambda e, GT=GT: e.activation(out=W(GT), in_=W(GT), func=AF.Gelu), reads=[WK[GT]], writes=[WK[GT]])
        T1, T2 = 2, 3
        for ot, GT in enumerate((GT0, GT1)):
            for kt, GK in enumerate((GT0, GT1)):
                P.pe(lambda e, ot=ot, kt=kt, GK=GK: e.matmul(out=B[4][:, 0:TBk], lhsT=gluw[:, kt, ot * 128:(ot + 1) * 128],
                                                            rhs=W(GK), start=(kt == 0), stop=(kt == 1)),
                     reads=['gluw', WK[GK]], writes=['B4'])
            P.act(lambda e, ot=ot: e.activation(out=W(T1), in_=B[4][:, 0:TBk], func=AF.Sigmoid,
                                                bias=cB[:, 76 + ot:77 + ot]), reads=['B4', 'cB'], writes=[WK[T1]])
            P.pool(lambda e, GT=GT: e.tensor_tensor(out=W(T2), in0=W(GT), in1=W(T1), op=ALU.mult),
                   reads=[WK[GT], WK[T1]], writes=[WK[T2]])
            strow(T2, ot * 128, c0)

    colsR_in = din("colsR", [L, 128, 24])
    rw_w2 = din("rw_w2", [L, 32, 256]); rw_a2 = din("rw_a2", [L, 32, 256]); rw_g2 = din("rw_g2", [L, 64, 256])
    TBr = min(256, T)
    NCr = TBr // 64
    Ht = [Wt[i // 2][:, (i % 2) * 256:(i % 2) * 256 + 256] for i in range(48)]
    HK = ['Ht%d' % i for i in range(48)]
    ALIAS.extend(HK)
    cR = sb("cR", [128, 24])
    lora = sb("lora", [128, 256])
    stR = sb("stR", [128, 2, 64])
    btok, ktok2, vtok2 = [PX[:, 4096 + i * 512:4608 + i * 512].rearrange("p (b c) -> p b c", b=2) for i in range(3)]
    GMS = [PX[:, i * 512:(i + 1) * 512] for i in range(8)]
    GZ, GN, GAK, GRK, GRB, GP, GX, GU = GMS
    GKEY = ['gm%d' % i for i in range(8)]

    def gkey(ap):
        for i, g_ in enumerate(GMS):
            if g_ is ap:
                return GKEY[i]
        raise KeyError
    mask_su = c_f32[:, 512:768]
    mask_sl = c_f32[:, 1600:1856]
    I4 = c_f32[:, 1856:2112]
    EM05 = float(np.exp(-0.5))

    def rw_block(l, blk):
        if blk == 0:
            fence()
        c0 = blk * TBr
        H = lambda i: Ht[i][:, 0:TBr]
        X = list(range(0, 7))
        XP = 7
        LW, AA, GG, KK = (8, 9), (10, 11), (12, 13), (14, 15)
        LC, WI, WN, WE = (16, 17), (18, 19), (20, 21), (22, 23)
        RT_, AT_, BT_, KT_ = (24, 25), (26, 27), (28, 29), (30, 31)
        YT, BON, T1, T2 = (32, 33), (34, 35), 36, 37
        R_, K_, V_ = (0, 1), (2, 3), (4, 5)
        Zt = 6
        if blk == 0:
            P.pool(lambda e: e.memset(stR[:], 0.0), writes=['stR'])
        for i in range(7):
            r0 = 1040 + i * 128
            P.dma(lambda e, i=i, r0=r0: e.dma_start(out=H(X[i]), in_=pT[r0:r0 + 128, c0:c0 + TBr]), reads=['pT'],
                  writes=[HK[X[i]]])
            if blk == 0:
                P.pool(lambda e: e.memset(Ht[XP][:, 0:1], 0.0), writes=[HK[XP]])
                P.dma(lambda e, r0=r0: e.dma_start(out=Ht[XP][:, 1:TBr], in_=pT[r0:r0 + 128, 0:TBr - 1]),
                      reads=['pT'], writes=[HK[XP]])
            else:
                P.dma(lambda e, r0=r0: e.dma_start(out=H(XP), in_=pT[r0:r0 + 128, c0 - 1:c0 - 1 + TBr]),
                      reads=['pT'], writes=[HK[XP]])
            P.dve(lambda e, i=i: e.tensor_tensor(out=H(XP), in0=H(XP), in1=H(X[i]), op=ALU.subtract),
                  reads=[HK[XP], HK[X[i]]], writes=[HK[XP]])
            P.dve(lambda e, i=i: e.scalar_tensor_tensor(out=H(X[i]), in0=H(XP), scalar=cR[:, i:i + 1], in1=H(X[i]),
                                                        op0=ALU.mult, op1=ALU.add),
                  reads=[HK[XP], HK[X[i]], 'cR'], writes=[HK[X[i]]])
        P.act(lambda e: e.activation(out=Ht[Zt][0:32, 0:TBr], in_=Ht[Zt][0:32, 0:TBr], func=AF.Tanh),
              reads=[HK[Zt]], writes=[HK[Zt]])
        P.act(lambda e: e.activation(out=Ht[Zt][64:128, 0:TBr], in_=Ht[Zt][64:128, 0:TBr], func=AF.Sigmoid),
              reads=[HK[Zt]], writes=[HK[Zt]])
        for ct in range(2):
            cs2 = slice(ct * 128, (ct + 1) * 128)
            P.pe(lambda e, cs2=cs2: e.matmul(out=B[0][:, 0:TBr], lhsT=lora[0:32, cs2], rhs=Ht[Zt][0:32, 0:TBr],
                                             start=True, stop=True), reads=['lora', HK[Zt]], writes=['B0'])
            P.act(lambda e, ct=ct: e.activation(out=H(LW[ct]), in_=B[0][:, 0:TBr], func=AF.Sigmoid,
                                                bias=cR[:, 7 + ct:8 + ct]), reads=['B0', 'cR'], writes=[HK[LW[ct]]])
            P.dve(lambda e, ct=ct: e.tensor_scalar(out=H(LW[ct]), in0=H(LW[ct]), scalar1=-EM05, scalar2=None,
                                                   op0=ALU.mult), reads=[HK[LW[ct]]], writes=[HK[LW[ct]]])
            P.pe(lambda e, cs2=cs2: e.matmul(out=B[1][:, 0:TBr], lhsT=lora[32:64, cs2], rhs=Ht[Zt][32:64, 0:TBr],
                                             start=True, stop=True), reads=['lora', HK[Zt]], writes=['B1'])
            P.act(lambda e, ct=ct: e.activation(out=H(AA[ct]), in_=B[1][:, 0:TBr], func=AF.Sigmoid,
                                                bias=cR[:, 9 + ct:10 + ct]), reads=['B1', 'cR'], writes=[HK[AA[ct]]])
            P.pe(lambda e, cs2=cs2: e.matmul(out=B[2][:, 0:TBr], lhsT=lora[64:128, cs2], rhs=Ht[Zt][64:128, 0:TBr],
                                             start=True, stop=True), reads=['lora', HK[Zt]], writes=['B2'])
            P.act(lambda e, ct=ct: e.copy(out=H(GG[ct]), in_=B[2][:, 0:TBr]), reads=['B2'], writes=[HK[GG[ct]]])
            P.dve(lambda e, ct=ct: e.tensor_scalar(out=H(KK[ct]), in0=H(K_[ct]), scalar1=cR[:, 11 + ct:12 + ct],
                                                   scalar2=None, op0=ALU.mult), reads=[HK[K_[ct]], 'cR'],
                  writes=[HK[KK[ct]]])
            P.act(lambda e, ct=ct: e.activation(out=H(T1), in_=H(KK[ct]), func=AF.Square), reads=[HK[KK[ct]]],
                  writes=[HK[T1]])
            P.pe(lambda e: e.matmul(out=B[3][:, 0:TBr], lhsT=blk64, rhs=H(T1), start=True, stop=True),
                 reads=['c_f32', HK[T1]], writes=['B3'])
            P.act(lambda e: e.activation(out=H(T1), in_=B[3][:, 0:TBr], func=AF.Sqrt, scale=64.0), reads=['B3'],
                  writes=[HK[T1]])
            P.dve(lambda e: e.tensor_scalar(out=H(T1), in0=H(T1), scalar1=1e-12, scalar2=None, op0=ALU.max),
                  reads=[HK[T1]], writes=[HK[T1]])
            P.dve(lambda e: e.reciprocal(out=H(T1), in_=H(T1)), reads=[HK[T1]], writes=[HK[T1]])
            P.dve(lambda e, ct=ct: e.tensor_tensor(out=H(KK[ct]), in0=H(KK[ct]), in1=H(T1), op=ALU.mult),
                  reads=[HK[KK[ct]], HK[T1]], writes=[HK[KK[ct]]])
            P.dve(lambda e, ct=ct: e.tensor_scalar(out=H(T1), in0=H(AA[ct]), scalar1=cR[:, 13 + ct:14 + ct],
                                                   scalar2=cR[:, 21 + ct:22 + ct], op0=ALU.mult, op1=ALU.add),
                  reads=[HK[AA[ct]], 'cR'], writes=[HK[T1]])
            P.dve(lambda e, ct=ct: e.tensor_tensor(out=H(K_[ct]), in0=H(K_[ct]), in1=H(T1), op=ALU.mult),
                  reads=[HK[K_[ct]], HK[T1]], writes=[HK[K_[ct]]])
            P.dve(lambda e, ct=ct: e.scalar_tensor_tensor(out=H(T1), in0=H(R_[ct]), scalar=cR[:, 15 + ct:16 + ct],
                                                          in1=H(K_[ct]), op0=ALU.mult, op1=ALU.mult),
                  reads=[HK[R_[ct]], HK[K_[ct]], 'cR'], writes=[HK[T1]])
            P.pe(lambda e: e.matmul(out=B[3][:, 0:TBr], lhsT=blk64, rhs=H(T1), start=True, stop=True),
                 reads=['c_f32', HK[T1]], writes=['B3'])
            P.dve(lambda e, ct=ct: e.scalar_tensor_tensor(out=H(BON[ct]), in0=B[3][:, 0:TBr], scalar=64.0,
                                                          in1=H(V_[ct]), op0=ALU.mult, op1=ALU.mult),
                  reads=['B3', HK[V_[ct]]], writes=[HK[BON[ct]]])
            for c in range(NCr):
                cs = slice(c * 64, (c + 1) * 64)
                P.dve(lambda e, ct=ct, cs=cs: e.tensor_tensor_scan(out=Ht[LC[ct]][:, cs], data0=ones64[:],
                                                                   data1=Ht[LW[ct]][:, cs], initial=0.0,
                                                                   op0=ALU.mult, op1=ALU.add),
                      reads=['ones64', HK[LW[ct]]], writes=[HK[LC[ct]]])
            P.act(lambda e, ct=ct: e.activation(out=H(WI[ct]), in_=H(LC[ct]), func=AF.Exp), reads=[HK[LC[ct]]],
                  writes=[HK[WI[ct]]])
            P.act(lambda e, ct=ct: e.activation(out=H(WN[ct]), in_=H(LC[ct]), func=AF.Exp, scale=-1.0),
                  reads=[HK[LC[ct]]], writes=[HK[WN[ct]]])
            P.dve(lambda e, ct=ct: e.tensor_tensor(out=H(WE[ct]), in0=H(LC[ct]), in1=H(LW[ct]), op=ALU.subtract),
                  reads=[HK[LC[ct]], HK[LW[ct]]], writes=[HK[WE[ct]]])
            P.act(lambda e, ct=ct: e.activation(out=H(WE[ct]), in_=H(WE[ct]), func=AF.Exp), reads=[HK[WE[ct]]],
                  writes=[HK[WE[ct]]])
            P.dve(lambda e, ct=ct: e.tensor_tensor(out=H(RT_[ct]), in0=H(R_[ct]), in1=H(WI[ct]), op=ALU.mult),
                  reads=[HK[R_[ct]], HK[WI[ct]]], writes=[HK[RT_[ct]]])
            P.dve(lambda e, ct=ct: e.scalar_tensor_tensor(out=H(AT_[ct]), in0=H(KK[ct]), scalar=-1.0, in1=H(WE[ct]),
                                                          op0=ALU.mult, op1=ALU.mult),
                  reads=[HK[KK[ct]], HK[WE[ct]]], writes=[HK[AT_[ct]]])
            P.pool(lambda e, ct=ct: e.tensor_tensor(out=H(BT_[ct]), in0=H(KK[ct]), in1=H(AA[ct]), op=ALU.mult),
                   reads=[HK[KK[ct]], HK[AA[ct]]], writes=[HK[BT_[ct]]])
            P.pool(lambda e, ct=ct: e.tensor_tensor(out=H(BT_[ct]), in0=H(BT_[ct]), in1=H(WN[ct]), op=ALU.mult),
                   reads=[HK[BT_[ct]], HK[WN[ct]]], writes=[HK[BT_[ct]]])
            P.pool(lambda e, ct=ct: e.tensor_tensor(out=H(KT_[ct]), in0=H(K_[ct]), in1=H(WN[ct]), op=ALU.mult),
                   reads=[HK[K_[ct]], HK[WN[ct]]], writes=[HK[KT_[ct]]])
        for b2 in range(TBr // 128):
            sl = slice(b2 * 128, (b2 + 1) * 128)
            for (srcs, dst, dk, bk) in ((BT_, btok, 'btok', 0), (KT_, ktok2, 'ktok2', 1), (V_, vtok2, 'vtok2', 2)):
                for ct in range(2):
                    P.pe(lambda e, srcs=srcs, ct=ct, sl=sl, bk=bk: e.transpose(
                        out=B[bk][:, ct * 128:(ct + 1) * 128], in_=Ht[srcs[ct]][:, sl], identity=identf),
                        reads=[HK[srcs[ct]], 'c_f32'], writes=['B%d' % bk])
                P.act(lambda e, dst=dst, b2=b2, bk=bk: e.copy(out=dst[:, b2, :], in_=B[bk][:, 0:256]),
                      reads=['B%d' % bk], writes=[dk])
        def gram(bank, lt, rt, dst, dkey, mask):
            for par in range(2):
                for c in range(NCr):
                    b2, pb = c // 2, (c % 2) * 64
                    cs = slice(c * 64, (c + 1) * 64)
                    for hd in (par, par + 2):
                        ct, hs = hd // 2, slice((hd % 2) * 64, (hd % 2) * 64 + 64)
                        col = b2 * 256 + hd * 64
                        P.pe(lambda e, ct=ct, hs=hs, cs=cs, pb=pb, col=col: e.matmul(
                            out=B[bank][pb:pb + 64, col:col + 64], lhsT=Ht[lt[ct]][hs, cs], rhs=Ht[rt[ct]][hs, cs],
                            start=True, stop=True), reads=[HK[lt[ct]], HK[rt[ct]]], writes=['B%d' % bank])
            nb = TBr // 128
            P.dve(lambda e: e.tensor_tensor(
                out=dst[:, 0:nb * 256].rearrange("p (b c) -> p b c", b=nb),
                in0=B[bank][:, 0:nb * 256].rearrange("p (b c) -> p b c", b=nb),
                in1=mask.unsqueeze(1).to_broadcast([128, nb, 256]), op=ALU.mult),
                reads=['B%d' % bank, 'c_f32'], writes=[dkey])
        gram(0, BT_, AT_, GZ, 'gm0', mask_su)
        gram(1, AT_, BT_, GN, 'gm1', mask_sl)
        gram(2, KT_, AT_, GAK, 'gm2', mask_su)
        gram(3, KT_, RT_, GRK, 'gm3', mask_incl)
        gram(4, BT_, RT_, GRB, 'gm4', mask_incl)
        nb = TBr // 128
        NW = nb * 256
        v3 = lambda ap: ap[:, 0:NW].rearrange("p (b c) -> p b c", b=nb)
        P.dve(lambda e: e.tensor_tensor(out=v3(GP), in0=v3(GZ), in1=I4.unsqueeze(1).to_broadcast([128, nb, 256]),
                                        op=ALU.add), reads=['gm0', 'c_f32'], writes=['gm5'])
        Zc, Zt_ = GZ, GN
        Za, Zb = GX, GU
        for j in range(1, 6):
            def allblk(fn):
                for par in range(2):
                    for c in range(par, NCr, 2):
                        b2, pb = c // 2, (c % 2) * 64
                        for hd in range(4):
                            col = b2 * 256 + hd * 64
                            fn(pb, col)
            allblk(lambda pb, col, Zc=Zc, Zt_=Zt_: P.pe(lambda e: e.matmul(
                out=B[0][pb:pb + 64, col:col + 64], lhsT=Zt_[pb:pb + 64, col:col + 64], rhs=Zc[pb:pb + 64, col:col + 64],
                start=True, stop=True), reads=[gkey(Zc),
                                               gkey(Zt_)], writes=['B0']))
            allblk(lambda pb, col, Zc=Zc, Zt_=Zt_: P.pe(lambda e: e.matmul(
                out=B[1][pb:pb + 64, col:col + 64], lhsT=Zc[pb:pb + 64, col:col + 64], rhs=Zt_[pb:pb + 64, col:col + 64],
                start=True, stop=True), reads=[gkey(Zc),
                                               gkey(Zt_)], writes=['B1']))
            nZ, nZt = (Za, Zb) if j % 2 == 1 else (GZ, GN)
            kZ = gkey(nZ)
            kZt = gkey(nZt)
            P.act(lambda e, nZ=nZ: e.copy(out=nZ[:, 0:NW], in_=B[0][:, 0:NW]), reads=['B0'], writes=[kZ])
            P.act(lambda e, nZt=nZt: e.copy(out=nZt[:, 0:NW], in_=B[1][:, 0:NW]), reads=['B1'], writes=[kZt])
            Zc, Zt_ = nZ, nZt
            allblk(lambda pb, col, Zt_=Zt_, kZt=kZt: P.pe(lambda e: e.matmul(
                out=B[2][pb:pb + 64, col:col + 64], lhsT=Zt_[pb:pb + 64, col:col + 64], rhs=GP[pb:pb + 64, col:col + 64],
                start=True, stop=True), reads=[kZt, 'gm5'], writes=['B2']))
            P.dve(lambda e: e.tensor_tensor(out=GP[:, 0:NW], in0=GP[:, 0:NW], in1=B[2][:, 0:NW], op=ALU.add),
                  reads=['gm5', 'B2'], writes=['gm5'])
        for c in range(NCr):
            b2, pb = c // 2, (c % 2) * 64
            cs = slice(c * 64, (c + 1) * 64)
            ps_ = slice(pb, pb + 64)
            for hd in range(4):
                ct, hs = hd // 2, slice((hd % 2) * 64, (hd % 2) * 64 + 64)
                col = b2 * 256 + hd * 64
                P.pe(lambda e, ps_=ps_, ct=ct, hs=hs, cs=cs, hd=hd: e.matmul(
                    out=B[3][ps_, hd * 64:(hd + 1) * 64], lhsT=Ht[AT_[ct]][hs, cs], rhs=stR[hs, ct, :],
                    start=True, stop=False), reads=[HK[AT_[ct]], 'stR'], writes=['B3'])
                P.pe(lambda e, ps_=ps_, hd=hd, col=col, b2=b2: e.matmul(
                    out=B[3][ps_, hd * 64:(hd + 1) * 64], lhsT=GAK[ps_, col:col + 64],
                    rhs=vtok2[ps_, b2, hd * 64:(hd + 1) * 64], start=False, stop=True),
                    reads=['gm2', 'vtok2'], writes=['B3'])
            P.act(lambda e, ps_=ps_: e.copy(out=GX[ps_, 0:256], in_=B[3][ps_, 0:256]), reads=['B3'], writes=['gm6'])
            for hd in range(4):
                col = b2 * 256 + hd * 64
                P.pe(lambda e, ps_=ps_, hd=hd, col=col: e.matmul(
                    out=B[4][ps_, hd * 64:(hd + 1) * 64], lhsT=GP[ps_, col:col + 64],
                    rhs=GX[ps_, hd * 64:(hd + 1) * 64], start=True, stop=True), reads=['gm5', 'gm6'], writes=['B4'])
            P.act(lambda e, ps_=ps_: e.copy(out=GU[ps_, 0:256], in_=B[4][ps_, 0:256]), reads=['B4'], writes=['gm7'])
            for hd in range(4):
                ct, hs = hd // 2, slice((hd % 2) * 64, (hd % 2) * 64 + 64)
                col = b2 * 256 + hd * 64
                hcol = slice(hd * 64, (hd + 1) * 64)
                P.pe(lambda e, ps_=ps_, ct=ct, hs=hs, cs=cs: e.matmul(
                    out=B[5][hs, ct * 64:(ct + 1) * 64], lhsT=stR[hs, ct, :], rhs=Ht[RT_[ct]][hs, cs],
                    start=True, stop=False), reads=['stR', HK[RT_[ct]]], writes=['B5'])
                P.pe(lambda e, ps_=ps_, ct=ct, hs=hs, col=col, hcol=hcol: e.matmul(
                    out=B[5][hs, ct * 64:(ct + 1) * 64], lhsT=GU[ps_, hcol], rhs=GRB[ps_, col:col + 64],
                    start=False, stop=False), reads=['gm7', 'gm4'], writes=['B5'])
                P.pe(lambda e, ps_=ps_, ct=ct, hs=hs, col=col, hcol=hcol, b2=b2: e.matmul(
                    out=B[5][hs, ct * 64:(ct + 1) * 64], lhsT=vtok2[ps_, b2, hcol], rhs=GRK[ps_, col:col + 64],
                    start=False, stop=True), reads=['vtok2', 'gm3'], writes=['B5'])
                P.pe(lambda e, ps_=ps_, ct=ct, hs=hs, hcol=hcol, b2=b2: e.matmul(
                    out=B[2][hs, ct * 64:(ct + 1) * 64], lhsT=btok[ps_, b2, hcol], rhs=GU[ps_, hcol],
                    start=True, stop=False), reads=['btok', 'gm7'], writes=['B2'])
                P.pe(lambda e, ps_=ps_, ct=ct, hs=hs, hcol=hcol, b2=b2: e.matmul(
                    out=B[2][hs, ct * 64:(ct + 1) * 64], lhsT=ktok2[ps_, b2, hcol], rhs=vtok2[ps_, b2, hcol],
                    start=False, stop=True), reads=['ktok2', 'vtok2'], writes=['B2'])
            for ct in range(2):
                P.act(lambda e, ps_=ps_, ct=ct, cs=cs: e.copy(out=Ht[YT[ct]][:, cs], in_=B[5][:, ct * 64:(ct + 1) * 64]),
                      reads=['B5'], writes=[HK[YT[ct]]])
                P.dve(lambda e, ps_=ps_, ct=ct: e.tensor_tensor(out=stR[:, ct, :], in0=stR[:, ct, :],
                                                       in1=B[2][:, ct * 64:(ct + 1) * 64], op=ALU.add),
                      reads=['stR', 'B2'], writes=['stR'])
                P.dve(lambda e, ps_=ps_, ct=ct, c=c: e.tensor_scalar(out=stR[:, ct, :], in0=stR[:, ct, :],
                                                            scalar1=Ht[WI[ct]][:, c * 64 + 63:c * 64 + 64],
                                                            scalar2=None, op0=ALU.mult),
                      reads=['stR', HK[WI[ct]]], writes=['stR'])
        if blk == 0 and DBG_VAR == 7:
            for nm, idx in (('lw', LW[0]), ('aa', AA[0]), ('kk', KK[0]), ('kmod', K_[0]), ('gg', GG[0]), ('bon', BON[0]),
                            ('y', YT[0]), ('rt', RT_[0]), ('at', AT_[0]), ('bt', BT_[0]), ('kt', KT_[0]), ('r', R_[0]),
                            ('v', V_[0])):
                dbgdump(nm, H(idx), HK[idx], [128, TBr])
            for nm, gi in (('gz', 0), ('gn', 1), ('gak', 2), ('grk', 3), ('grb', 4), ('gp', 5), ('gx', 6), ('gu', 7)):
                dbgdump(nm, GMS[gi], GKEY[gi], [128, 512])
            dbgdump('vtok', vtok2[:, 0, :], 'vtok2', [128, 256])
            dbgdump('btok', btok[:, 0, :], 'btok', [128, 256])
        for ct in range(2):
            P.pe(lambda e, ct=ct: e.matmul(out=B[0][:, 0:TBr], lhsT=blk64, rhs=H(YT[ct]), start=True, stop=True),
                 reads=['c_f32', HK[YT[ct]]], writes=['B0'])
            P.act(lambda e, ct=ct: e.activation(out=H(T1), in_=H(YT[ct]), func=AF.Square), reads=[HK[YT[ct]]],
                  writes=[HK[T1]])
            P.pe(lambda e: e.matmul(out=B[1][:, 0:TBr], lhsT=blk64, rhs=H(T1), start=True, stop=True),
                 reads=['c_f32', HK[T1]], writes=['B1'])
            P.act(lambda e: e.copy(out=H(T2), in_=B[0][:, 0:TBr]), reads=['B0'], writes=[HK[T2]])
            P.dve(lambda e: e.tensor_tensor(out=H(T1), in0=H(T2), in1=H(T2), op=ALU.mult), reads=[HK[T2]],
                  writes=[HK[T1]])
            P.dve(lambda e: e.tensor_tensor(out=H(T1), in0=B[1][:, 0:TBr], in1=H(T1), op=ALU.subtract),
                  reads=['B1', HK[T1]], writes=[HK[T1]])
            P.dve(lambda e: e.tensor_scalar(out=H(T1), in0=H(T1), scalar1=64e-5, scalar2=None, op0=ALU.add),
                  reads=[HK[T1]], writes=[HK[T1]])
            P.act(lambda e: e.sqrt(out=H(T1), in_=H(T1)), reads=[HK[T1]], writes=[HK[T1]])
            P.dve(lambda e: e.reciprocal(out=H(T1), in_=H(T1)), reads=[HK[T1]], writes=[HK[T1]])
            P.dve(lambda e, ct=ct: e.tensor_tensor(out=H(YT[ct]), in0=H(YT[ct]), in1=H(T2), op=ALU.subtract),
                  reads=[HK[YT[ct]], HK[T2]], writes=[HK[YT[ct]]])
            P.dve(lambda e, ct=ct: e.tensor_tensor(out=H(YT[ct]), in0=H(YT[ct]), in1=H(T1), op=ALU.mult),
                  reads=[HK[YT[ct]], HK[T1]], writes=[HK[YT[ct]]])
            P.act(lambda e, ct=ct: e.activation(out=H(YT[ct]), in_=H(YT[ct]), func=AF.Identity,
                                                scale=cR[:, 17 + ct:18 + ct], bias=cR[:, 19 + ct:20 + ct]),
                  reads=[HK[YT[ct]], 'cR'], writes=[HK[YT[ct]]])
            P.dve(lambda e, ct=ct: e.tensor_tensor(out=H(YT[ct]), in0=H(YT[ct]), in1=H(BON[ct]), op=ALU.add),
                  reads=[HK[YT[ct]], HK[BON[ct]]], writes=[HK[YT[ct]]])
            P.dve(lambda e, ct=ct: e.tensor_tensor(out=H(YT[ct]), in0=H(YT[ct]), in1=H(GG[ct]), op=ALU.mult),
                  reads=[HK[YT[ct]], HK[GG[ct]]], writes=[HK[YT[ct]]])
            P.dma(lambda e, ct=ct: e.dma_start(out=mixT[512 + ct * 128:512 + (ct + 1) * 128, c0:c0 + TBr],
                                               in_=H(YT[ct])), reads=[HK[YT[ct]]], writes=['mixT'])

    def rw_setup(l):
        P.dma(lambda e: e.dma_start(out=cR[:], in_=colsR_in[l]), writes=['cR'])
        P.dma(lambda e: e.dma_start(out=lora[0:32, :], in_=rw_w2[l]), writes=['lora'])
        P.dma(lambda e: e.dma_start(out=lora[32:64, :], in_=rw_a2[l]), writes=['lora'])
        P.dma(lambda e: e.dma_start(out=lora[64:128, :], in_=rw_g2[l]), writes=['lora'])
        for ct in range(2):
            P.dve(lambda e, ct=ct: e.tensor_scalar(out=cR[:, 21 + ct:22 + ct], in0=cR[:, 13 + ct:14 + ct],
                                                   scalar1=-1.0, scalar2=1.0, op0=ALU.mult, op1=ALU.add),
                  reads=['cR'], writes=['cR'])

    def phase_B(l, which=B_WHICH):
        fence()
        P.dma(lambda e: e.dma_start(out=cB[:], in_=colsB[l]), writes=['cB'])
        P.dma(lambda e: e.dma_start(out=wup[:], in_=gla_w_up[l]), writes=['wup'])
        P.dve(lambda e: e.tensor_scalar(out=cB[:, 71:72], in0=cB[:, 68:69], scalar1=-1.0, scalar2=None, op0=ALU.mult),
              reads=['cB'], writes=['cB'])
        P.dve(lambda e: e.tensor_scalar(out=cB[:, 73:74], in0=cB[:, 72:73], scalar1=-1.0, scalar2=None, op0=ALU.mult),
              reads=['cB'], writes=['cB'])
        if 's5' in which:
            s5_setup(l)
        if 'rw' in which:
            rw_setup(l)
            for blk in range(T // TBr):
                rw_block(l, blk)
        for blk in range(T // TBk):
            if blk == 0:
                fence()
            if 's5' in which:
                s5_block(l, blk)
            if 'conv' in which:
                conv_block(l, blk)
            if 'gla' in which:
                gla_block(l, blk)

    for l in range(L):
        src = x if l == 0 else out
        if 'A' in phases:
            phase_A(l, src)
        if 'B' in phases:
            phase_B(l)
        if 'O' in phases:
            phase_outproj(l, src)
        if 'X' in phases:
            phase_xattn(l)
        if 'M' in phases:
            phase_moe(l)
    if 'F' in phases:
        phase_final()

    P.emit()
    return nc, es


def make_consts():
    c = np.zeros((128, 2112), np.float32)
    c[:, 1024:1536] = np.arange(1, 513, dtype=np.float32)[None, :]
    c[0:64, 1536] = 1.0
    c[64:128, 1537] = 1.0
    c[:, 0:128] = np.eye(128, dtype=np.float32)
    p = np.arange(128)[:, None] % 64
    t = np.arange(64)[None, :]
    incl = (p <= t).astype(np.float32)
    strict = (p < t).astype(np.float32)
    for h in range(4):
        c[:, 128 + h * 64:128 + (h + 1) * 64] = incl
        c[:, 512 + h * 64:512 + (h + 1) * 64] = strict
    for h in range(4):
        c[:, 1600 + h * 64:1600 + (h + 1) * 64] = (p > t).astype(np.float32)
        c[:, 1856 + h * 64:1856 + (h + 1) * 64] = (p == t).astype(np.float32)
    q = np.arange(128)
    c[:, 384:512] = (q[:, None] // 64 == q[None, :] // 64).astype(np.float32) / 64.0
    return c


ALL_PHASES = ('A', 'B', 'O', 'X', 'M', 'F')
B_WHICH = ('s5', 'conv', 'gla', 'rw')
DBG_STOP = 99
DBG_VAR = 0


def make_colsB(inputs, L):
    g = lambda k: np.asarray(inputs[k], dtype=np.float32)
    c = np.zeros((L, 128, 80), np.float32)
    cw = g("conv_w")
    for ct in range(2):
        c[:, :, ct * 31:(ct + 1) * 31] = cw[:, :, ct * 128:(ct + 1) * 128].transpose(0, 2, 1)
        c[:, :, 62 + ct] = g("conv_b")[:, ct * 128:(ct + 1) * 128]
        c[:, :, 64 + ct] = g("conv_ln_g")[:, ct * 128:(ct + 1) * 128]
        c[:, :, 66 + ct] = g("conv_ln_b")[:, ct * 128:(ct + 1) * 128]
        c[:, :, 69 + ct] = g("gla_norm_g")[:, ct * 128:(ct + 1) * 128]
    for ct in range(2):
        c[:, :, 74 + ct] = g("s5_d")[:, ct * 128:(ct + 1) * 128]
        c[:, :, 76 + ct] = g("s5_glu_b")[:, ct * 128:(ct + 1) * 128]
    c[:, 0:64, 68] = g("gla_b_up")[:, 0:64]
    c[:, 0:64, 72] = g("gla_b_up")[:, 64:128]
    return c


def make_colsR(inputs, L):
    g = lambda k: np.asarray(inputs[k], dtype=np.float32)
    c = np.zeros((L, 128, 24), np.float32)
    c[:, :, 0:7] = g("rw_mu").reshape(L, 7, 128).transpose(0, 2, 1)
    for i, k in enumerate(("rw_w0", "rw_a0", "rw_k_k", "rw_k_a", "rw_r_k", "rw_ln_g", "rw_ln_b")):
        c[:, :, 7 + 2 * i:9 + 2 * i] = g(k).reshape(L, 2, 128).transpose(0, 2, 1)
    return c


def make_s5(inputs, L):
    g = lambda k: np.asarray(inputs[k], dtype=np.float32)
    pl = lambda a: a.reshape(L, 8, 2, 64).transpose(0, 2, 3, 1).reshape(L, 128, 8)
    ldt = np.broadcast_to(g("s5_log_dt")[:, :, None], (L, 16, 64))
    s5p = np.concatenate([pl(g("s5_lam_re")), pl(g("s5_lam_im")), pl(ldt)], axis=-1)
    Bm = np.zeros((L, 128, 16, 128), np.float32)
    for ri, key in enumerate(("s5_b_re", "s5_b_im")):
        b = g(key)
        for j in range(8):
            for two in range(2):
                r0 = (j % 4) * 32 + two * 16
                Bm[:, r0:r0 + 16, ri * 8 + j, two * 64:(two + 1) * 64] = b[:, 2 * j + two].transpose(0, 2, 1)
    C = np.zeros((L, 128, 2, 8, 16), np.float32)
    for ri, key in enumerate(("s5_c_re", "s5_c_im")):
        c = g(key)
        C[:, :, ri] = c.reshape(L, 8, 2, 16, 64).transpose(0, 2, 4, 1, 3).reshape(L, 128, 8, 16)
    return {"s5p": np.ascontiguousarray(s5p), "s5B": Bm, "s5C": C}


def prep_inputs(inputs, b, L):
    f = lambda k: np.ascontiguousarray(np.asarray(inputs[k], dtype=np.float32))
    m = {
        "x": np.ascontiguousarray(np.asarray(inputs["x"], np.float32)[b]),
        "mem": np.ascontiguousarray(np.asarray(inputs["mem"], np.float32)[b]),
        "consts": make_consts(),
        "norm_mix_g": f("norm_mix_g"), "w_in": f("w_in"), "w_out": f("w_out"),
        "beta_c": np.ascontiguousarray(f("mix_beta").reshape(L, KT, 128).transpose(0, 2, 1)),
        "norm_xattn_g": f("norm_xattn_g"), "norm_mem_g": f("norm_mem_g"),
        "xa_wq": f("xa_wq"), "xa_wk": f("xa_wk"), "xa_wv": f("xa_wv"), "xa_wo": f("xa_wo"),
        "norm_ffn_g": f("norm_ffn_g"),
        "moe_rw": np.ascontiguousarray(np.concatenate([f("moe_group_w"), f("moe_expert_w")], axis=-1)),
        "moe_rb": np.ascontiguousarray(np.concatenate([f("moe_group_b"), f("moe_expert_b")], axis=-1)),
        "moe_w_gate": f("moe_w_gate"), "moe_w_up": f("moe_w_up"), "moe_w_down": f("moe_w_down"),
        "norm_final_g": f("norm_final_g").reshape(1, D),
        "colsB": make_colsB(inputs, L), "gla_w_up": f("gla_w_up"),
        **make_s5(inputs, L), "s5_glu_w": f("s5_glu_w"),
        "colsR": make_colsR(inputs, L), "rw_w2": f("rw_w2"), "rw_a2": f("rw_a2"), "rw_g2": f("rw_g2"),
    }
    return m


def kernel(**inputs):
    x = np.asarray(inputs["x"])
    Bsz, T, _ = x.shape
    L = np.asarray(inputs["w_in"]).shape[0]
    nc, es = build(T, L, dbg=False, phases=ALL_PHASES)
    n_cores = 8
    maps = [prep_inputs(inputs, c % Bsz, L) for c in range(Bsz)]
    in_maps = [maps[c % Bsz] for c in range(n_cores)]
    res = run_bass_kernel_spmd(nc, in_maps, core_ids=list(range(n_cores)))
    outs = [np.asarray(res.results[c]["out"], dtype=np.float32) for c in range(Bsz)]
    return np.stack(outs, axis=0)
```
